# Optimizing a Trainium2 kernel written in Bass

```python
import math
import jax, jax.numpy as jnp
from jax import lax
import numpy as np

D_MODEL = 2048
BATCH = 4
SEQ = 2048
DEPTH = 4

N_A = DEPTH // 2
N_B = DEPTH - N_A
HGRN_HEADS = D_MODEL // 128
HGRN_DK = 128
HGRN_DV = D_MODEL // HGRN_HEADS
HGRN_CHUNK = 32
ATTN_HEAD_DIM = 128
ATTN_HEADS = D_MODEL // ATTN_HEAD_DIM
MOBA_BLOCK = 256
MOBA_TOPK = 3
MOBA_QUERY_BLOCK = 4
D_FF = 4 * D_MODEL
EPS = 1e-6
NEG_BIG = -1e30
LB_FLOOR = 1e-30

kernel_name = "hgrn2_moba_yoco_hybrid"

F32 = jnp.float32


def rms_norm(x, g):
    xf = x.astype(F32)
    return xf * lax.rsqrt(jnp.mean(xf * xf, axis=-1, keepdims=True) + EPS) * g.astype(F32)


def chunked_gla(q, k, v, log_f):
    B, S, H, DK = q.shape
    DV = v.shape[-1]
    C = HGRN_CHUNK
    NC = S // C

    def to_chunks(t):
        return t.reshape(B, NC, C, H, t.shape[-1]).transpose(1, 0, 3, 2, 4)

    causal = jnp.tril(jnp.ones((C, C), dtype=bool))[:, :, None]

    def step(state, inp):
        qc, kc, vc, gc = inp
        b = jnp.cumsum(gc, axis=-2)
        inter = jnp.einsum('bhtd,bhde->bhte', qc * jnp.exp(b), state)
        diff = b[:, :, :, None, :] - b[:, :, None, :, :]
        decay = jnp.where(causal, jnp.exp(jnp.where(causal, diff, 0.0)), 0.0)
        scores = jnp.einsum('bhtd,bhsd,bhtsd->bhts', qc, kc, decay)
        intra = jnp.einsum('bhts,bhse->bhte', scores, vc)
        b_last = b[:, :, -1]
        new_state = jnp.exp(b_last)[..., None] * state + jnp.einsum(
            'bhsd,bhse->bhde', kc * jnp.exp(b_last[:, :, None] - b), vc)
        return new_state, inter + intra

    state0 = jnp.zeros((B, H, DK, DV), F32)
    _, out = lax.scan(step, state0, (to_chunks(q), to_chunks(k), to_chunks(v), to_chunks(log_f)))
    return out.transpose(1, 0, 3, 2, 4).reshape(B, S, H, DV)


def hgrn2_mixer(hn, w_in, lb, head_gain, w_out):
    B, S, _ = hn.shape
    proj = hn @ w_in
    q, f_raw, i, g = jnp.split(proj.astype(F32), 4, axis=-1)
    q = jax.nn.silu(q)
    lb = lb.astype(F32)
    log_f = jnp.logaddexp(jnp.log(jnp.maximum(lb, LB_FLOOR)), jnp.log1p(-lb) + jax.nn.log_sigmoid(f_raw))
    k = (1.0 - lb) * jax.nn.sigmoid(-f_raw)
    heads = lambda t, d: t.reshape(B, S, HGRN_HEADS, d)
    o = chunked_gla(heads(q, HGRN_DK), heads(k, HGRN_DK), heads(i, HGRN_DV), heads(log_f, HGRN_DK))
    o = rms_norm(o, head_gain.reshape(HGRN_HEADS, HGRN_DV)).reshape(B, S, D_MODEL)
    o = o * jax.nn.silu(g)
    return o @ w_out


def shared_kv(h, kv_norm, w_kv, k_norm):
    B, S, _ = h.shape
    hn = rms_norm(h, kv_norm)
    k, v = jnp.split(hn @ w_kv, 2, axis=-1)
    k = rms_norm(k.reshape(B, S, ATTN_HEADS, ATTN_HEAD_DIM), k_norm)
    v = v.reshape(B, S, ATTN_HEADS, ATTN_HEAD_DIM).astype(F32)
    NB = -(-S // MOBA_BLOCK)
    pad = NB * MOBA_BLOCK - S

    def blocks(t):
        t = jnp.pad(t.transpose(0, 2, 1, 3), ((0, 0), (0, 0), (0, pad), (0, 0)))
        return t.reshape(B, ATTN_HEADS, NB, MOBA_BLOCK, ATTN_HEAD_DIM)

    k_blocks, v_blocks = blocks(k), blocks(v)
    counts = jnp.clip(S - jnp.arange(NB) * MOBA_BLOCK, 1, MOBA_BLOCK).astype(F32)
    k_mean = jnp.sum(k_blocks, axis=3) / counts[:, None]
    return k_blocks, v_blocks, k_mean


def moba_mixer(hn, w_q, q_norm, k_blocks, v_blocks, k_mean, w_o):
    B, S, _ = hn.shape
    H, dh = ATTN_HEADS, ATTN_HEAD_DIM
    NB = k_blocks.shape[2]
    q = rms_norm((hn @ w_q).reshape(B, S, H, dh), q_norm).transpose(0, 2, 1, 3)
    pos = jnp.arange(S)
    qblk = pos // MOBA_BLOCK
    gate = jnp.einsum('bhtd,bhnd->bhtn', q, k_mean)
    past = jnp.arange(NB)[None, :] < qblk[:, None]
    gate = jnp.where(past, gate, NEG_BIG)
    _, sel = lax.top_k(gate, min(MOBA_TOPK, NB))
    valid = sel < qblk[:, None]
    own = jnp.broadcast_to(qblk[:, None], (B, H, S, 1))
    sel = jnp.concatenate([sel, own], axis=-1).astype(jnp.int32)
    valid = jnp.concatenate([valid, jnp.ones(own.shape, dtype=bool)], axis=-1)

    scale = dh ** -0.5
    slopes = 2.0 ** (-8.0 * jnp.arange(1, H + 1, dtype=F32) / H)
    bi = jnp.arange(B)[:, None, None, None]
    hi = jnp.arange(H)[None, :, None, None]
    key_off = jnp.arange(MOBA_BLOCK)

    def attend(args):
        qc, selc, validc, tq = args
        kg = k_blocks[bi, hi, selc]
        vg = v_blocks[bi, hi, selc]
        kpos = selc[..., None] * MOBA_BLOCK + key_off
        dist = (tq[:, None, None] - kpos).astype(F32)
        logits = jnp.einsum('bhqd,bhqrkd->bhqrk', qc, kg) * scale - slopes[:, None, None, None] * dist
        mask = validc[..., None] & (dist >= 0)
        logits = jnp.where(mask, logits, NEG_BIG)
        p = jax.nn.softmax(logits.reshape(B, H, qc.shape[2], -1), axis=-1).reshape(logits.shape)
        p = jnp.where(mask, p, 0.0)
        return jnp.einsum('bhqrk,bhqrkd->bhqd', p, vg)

    NQ = S // MOBA_QUERY_BLOCK

    def qblocks(t):
        return jnp.moveaxis(t.reshape(B, H, NQ, MOBA_QUERY_BLOCK, *t.shape[3:]), 2, 0)

    out = lax.map(attend, (qblocks(q), qblocks(sel), qblocks(valid), pos.reshape(NQ, MOBA_QUERY_BLOCK)))
    out = jnp.moveaxis(out, 0, 2).reshape(B, H, S, dh).transpose(0, 2, 1, 3).reshape(B, S, D_MODEL)
    return out @ w_o


def sqrelu_mlp(hn, w1, w2):
    return jnp.square(jax.nn.relu(hn @ w1)) @ w2


def setup_inputs(seed: int = 0) -> dict:
    key = jax.random.key(seed)
    ks = jax.random.split(key, 20)
    nrm = lambda k, shape, fan_in: jax.random.normal(k, shape, F32) * (fan_in ** -0.5)
    gain = lambda k, shape: 1.0 + 0.02 * jax.random.normal(k, shape, F32)
    D = D_MODEL
    return {
        "x": jax.random.normal(ks[0], (BATCH, SEQ, D), F32),
        "a_norm": gain(ks[1], (N_A, D)),
        "a_w_in": nrm(ks[2], (N_A, D, 4 * D), D),
        "a_head_norm": gain(ks[3], (N_A, D)),
        "a_w_out": nrm(ks[4], (N_A, D, D), D),
        "lower_bounds": 0.1 * jax.random.normal(ks[5], (N_A, D), F32),
        "kv_norm": gain(ks[6], (D,)),
        "w_kv": nrm(ks[7], (D, 2 * D), D),
        "k_norm": gain(ks[8], (ATTN_HEAD_DIM,)),
        "b_norm": gain(ks[9], (N_B, D)),
        "b_w_q": nrm(ks[10], (N_B, D, D), D),
        "b_q_norm": gain(ks[11], (N_B, ATTN_HEAD_DIM)),
        "b_w_o": nrm(ks[12], (N_B, D, D), D),
        "mlp_norm": gain(ks[13], (DEPTH, D)),
        "mlp_w1": nrm(ks[14], (DEPTH, D, D_FF), D),
        "mlp_w2": nrm(ks[15], (DEPTH, D_FF, D), D_FF),
    }


def reference(x, a_norm, a_w_in, a_head_norm, a_w_out, lower_bounds, kv_norm, w_kv, k_norm,
              b_norm, b_w_q, b_q_norm, b_w_o, mlp_norm, mlp_w1, mlp_w2):
    h = x
    p = jax.nn.softmax(lower_bounds.astype(F32), axis=0)
    lbs = jnp.cumsum(p, axis=0) - p[0]
    k_blocks = v_blocks = k_mean = None
    for l in range(DEPTH):
        if l < N_A:
            mix = hgrn2_mixer(rms_norm(h, a_norm[l]), a_w_in[l], lbs[l], a_head_norm[l], a_w_out[l])
        else:
            if l == N_A:
                k_blocks, v_blocks, k_mean = shared_kv(h, kv_norm, w_kv, k_norm)
            j = l - N_A
            mix = moba_mixer(rms_norm(h, b_norm[j]), b_w_q[j], b_q_norm[j], k_blocks, v_blocks, k_mean, b_w_o[j])
        h = h + mix.astype(h.dtype)
        h = h + sqrelu_mlp(rms_norm(h, mlp_norm[l]), mlp_w1[l], mlp_w2[l]).astype(h.dtype)
    return h
```

```python
from contextlib import ExitStack
import numpy as np
import concourse.bass as bass
import concourse.mybir as mybir

F32 = mybir.dt.float32
BF16 = mybir.dt.bfloat16
AF = mybir.ActivationFunctionType
ALU = mybir.AluOpType
AX = mybir.AxisListType

ENGS = ("pe", "act", "dve", "pool", "sp")


class Buf:
    __slots__ = ("name", "w", "r", "dsem", "dval")

    def __init__(self, name):
        self.name = name
        self.w = {}
        self.r = {}
        self.dsem = None
        self.dval = 0


class Scope:
    uid = 0

    def __init__(self, cx):
        self.cx = cx
        self.es = ExitStack()

    def __enter__(self):
        self.mark = len(self.cx.scope_bufs)
        return self

    def __exit__(self, *a):
        self.cx.barrier()
        for b in self.cx.scope_bufs[self.mark:]:
            self.cx.sempool.append((b.dsem, b.dval))
            self.cx.alldma.remove(b)
            self.cx.final.append((b.dsem, b.dval))
        del self.cx.scope_bufs[self.mark:]
        self.es.close()
        return False

    def sb(self, name, shape, dt):
        Scope.uid += 1
        return self.es.enter_context(self.cx.nc.sbuf_tensor(f"{name}_{Scope.uid}", list(shape), dt))

    def ps(self, name, shape, dt):
        Scope.uid += 1
        return self.es.enter_context(self.cx.nc.psum_tensor(f"{name}_{Scope.uid}", list(shape), dt))


class Ctx:
    def __init__(self, nc):
        self.nc = nc
        self.es = ExitStack()
        self.eng = {"pe": nc.tensor, "act": nc.scalar, "dve": nc.vector, "pool": nc.gpsimd, "sp": nc.sync}
        self.esem = {}
        self.ecnt = {e: 0 for e in ENGS}
        for e in ENGS:
            self.esem[e] = self.es.enter_context(nc.semaphore("es_" + e))
        self.known = {e: {} for e in ENGS}
        self.nsem = 0
        self.final = []
        self.alldma = []
        self.sempool = []
        self.scope_bufs = []

    def scope(self):
        return Scope(self)

    def barrier(self):
        deps = {}
        for e in ENGS:
            if self.ecnt[e] > 0:
                deps[id(self.esem[e])] = (self.esem[e], self.ecnt[e])
        for b in self.alldma:
            if b.dval > 0:
                deps[id(b.dsem)] = (b.dsem, b.dval)
        for e in ENGS:
            self._emit_waits(e, deps)

    def newsem(self, name):
        self.nsem += 1
        return self.es.enter_context(self.nc.semaphore(name))

    def dmabuf(self, name):
        b = Buf(name)
        if self.sempool:
            b.dsem, b.dval = self.sempool.pop()
        else:
            b.dsem = self.newsem("d_%d" % self.nsem)
        self.alldma.append(b)
        self.scope_bufs.append(b)
        return b

    def _collect(self, reads, writes):
        deps = {}
        for b in reads:
            for k, (s, v) in b.w.items():
                if k not in deps or deps[k][1] < v:
                    deps[k] = (s, v)
        for b in writes:
            for d in (b.w, b.r):
                for k, (s, v) in d.items():
                    if k not in deps or deps[k][1] < v:
                        deps[k] = (s, v)
        return deps

    def _emit_waits(self, e, deps, skip_self=False):
        kn = self.known[e]
        for k, (s, v) in deps.items():
            if skip_self and k == id(self.esem[e]):
                continue
            if kn.get(k, 0) >= v:
                continue
            kn[k] = v
            self.eng[e].wait_ge(s, v)

    def op(self, e, fn, reads=(), writes=()):
        deps = self._collect(reads, writes)
        self._emit_waits(e, deps, skip_self=(e == "pe"))
        self.ecnt[e] += 1
        tok = (self.esem[e], self.ecnt[e])
        k = id(self.esem[e])
        fn(self.eng[e]).then_inc(self.esem[e], 1)
        for b in reads:
            b.r[k] = tok
        for b in writes:
            b.w = {k: tok}
            b.r = {}

    def dma(self, q, fn, owner, reads=(), writes=(), serial=True):
        deps = self._collect(reads, writes)
        if owner.dval > 0 and serial:
            deps[id(owner.dsem)] = (owner.dsem, max(owner.dval, deps.get(id(owner.dsem), (None, 0))[1]))
        self._emit_waits(q, deps)
        owner.dval += 16
        tok = (owner.dsem, owner.dval)
        k = id(owner.dsem)
        fn(self.eng[q]).then_inc(owner.dsem, 16)
        for b in reads:
            b.r[k] = tok
        for b in writes:
            b.w = {k: tok}
            b.r = {}
        return tok

    def renew_engine_sems(self):
        self.barrier()
        for e in ENGS:
            self.maxcnt = max(getattr(self, "maxcnt", 0), self.ecnt[e])
            if self.ecnt[e] > 0:
                self.esem[e] = self.es.enter_context(self.nc.semaphore("es%d_%s" % (self.nsem, e)))
                self.nsem += 1
                self.ecnt[e] = 0

    def barrier_on(self, owner):
        deps = {id(owner.dsem): (owner.dsem, owner.dval)}
        for e in ENGS:
            self._emit_waits(e, deps)

    def coll(self, fn, owner):
        deps = {}
        if owner.dval > 0:
            deps[id(owner.dsem)] = (owner.dsem, owner.dval)
        self._emit_waits("pool", deps)
        owner.dval += 1
        fn(self.eng["pool"]).then_inc(owner.dsem)

    def dma_multi(self, q, fn, owner, reads=(), writes_acc=()):
        deps = self._collect(reads, ())
        for b in writes_acc:
            for kk, (s, v) in b.r.items():
                if kk not in deps or deps[kk][1] < v:
                    deps[kk] = (s, v)
        if owner.dval > 0:
            deps[id(owner.dsem)] = (owner.dsem, max(owner.dval, deps.get(id(owner.dsem), (None, 0))[1]))
        self._emit_waits(q, deps)
        owner.dval += 16
        tok = (owner.dsem, owner.dval)
        k = id(owner.dsem)
        fn(self.eng[q]).then_inc(owner.dsem, 16)
        for b in reads:
            b.r[k] = tok
        for b in writes_acc:
            b.w[k] = tok
        return tok

    def finish(self):
        deps = {}
        for e in ENGS:
            if self.ecnt[e] > 0:
                deps[id(self.esem[e])] = (self.esem[e], self.ecnt[e])
        for b in self.alldma:
            if b.dval > 0:
                deps[id(b.dsem)] = (b.dsem, b.dval)
        for (s_, v) in self.final:
            if id(s_) not in deps or deps[id(s_)][1] < v:
                deps[id(s_)] = (s_, v)
        for k, (s, v) in deps.items():
            self.eng["sp"].wait_ge(s, v)
        self.es.close()


EPS = 1e-6


def _load_pieces(cx, pieces, dst_of, owner, buf):
    first = True
    for (cs, ce, sap) in pieces:
        cx.dma("sp", lambda e, dst=dst_of(cs, ce), sap=sap: e.dma_start(out=dst, in_=sap), owner,
               writes=[buf] if first else [], serial=first)
        first = False
    buf.w = {id(owner.dsem): (owner.dsem, owner.dval)}
    buf.r = {}


def emit_linear(cx, xT, Wt, outT, T, Fin, Fout, x_dt, out_dt, consts, gainT=None, resT=None, act=None,
                TBLK=None, tag="lin", xsrc=None, xsrcB=None, selT=None):
    nc = cx.nc
    KC = Fin // 128
    MC = Fout // 128
    if TBLK is None:
        TBLK = min(T, 1024 if KC <= 16 else 512)
    NB = TBLK // 512
    G = max(1, 64 // KC)
    assert MC % G == 0
    if xsrc is None:
        xTv = xT.rearrange("(c p) t -> p c t", p=128)
        xsrc = lambda c0, c1, t0, n: [(c0, c1, xTv[:, c0:c1, t0:t0 + n])]
    with cx.scope() as sc:
        xn = sc.sb("xn", [128, KC, TBLK], BF16)
        xnB = Buf("xn")
        if xsrcB is not None:
            xtm = sc.sb("xtm", [128, KC, TBLK], BF16)
            xtmB = Buf("xtm")
            xtmD = cx.dmabuf(f"{tag}_xtm")
            sel = sc.sb("sel", [128, 2], F32)
            selB = cx.dmabuf(f"{tag}_sel")
            cx.dma("sp", lambda e: e.dma_start(out=sel[:], in_=selT), selB, writes=[selB])
        NW = 3
        wb = [sc.sb(f"w{i}", [128, G, KC * 128], BF16) for i in range(NW)]
        wB = [cx.dmabuf(f"{tag}_w{i}") for i in range(NW)]
        NPS = 4
        pss = [sc.ps(f"ps{i}", [128, 512], F32) for i in range(NPS)]
        psB = [Buf(f"ps{i}") for i in range(NPS)]
        NO = 3
        ost = [sc.sb(f"o{i}", [128, 512], out_dt) for i in range(NO)]
        ostB = [cx.dmabuf(f"{tag}_o{i}") for i in range(NO)]
        if act == "relu2":
            rst = [sc.sb(f"r{i}", [128, 512], F32) for i in range(2)]
            rstB = [Buf(f"r{i}") for i in range(2)]
        if resT is not None:
            rsd = [sc.sb(f"rs{i}", [128, 512], F32) for i in range(3)]
            rsdB = [cx.dmabuf(f"{tag}_rs{i}") for i in range(3)]
        if gainT is not None:
            xs = sc.sb("xs", [128, KC, 512], F32)
            xsB = cx.dmabuf(f"{tag}_xs")
            sqb = sc.sb("sqb", [128, KC, 512], BF16)
            sqB = Buf("sqb")
            gsb = sc.sb("gsb", [128, KC], F32)
            gB = cx.dmabuf(f"{tag}_g")
            cx.dma("sp", lambda e: e.dma_start(out=gsb[:], in_=gainT), gB, writes=[gB])
            pssum = sc.ps("pssum", [128, 512], F32)
            pssB = Buf("pssum")
            tmp = sc.sb("tmp", [128, 512], F32)
            tmpB = Buf("tmp")
            rstd = sc.sb("rstd", [128, 512], F32)
            rstdB = Buf("rstd")
        else:
            xnD = cx.dmabuf(f"{tag}_xn")
        ones_bf, onesB = consts["ones_bf"]
        eps_sb, epsB = consts["eps"]
        cnt = 0
        wcnt = 0
        for tb in range(T // TBLK):
            t0 = tb * TBLK
            if gainT is None:
                for c0 in range(0, KC, 16):
                    _load_pieces(cx, xsrc(c0, c0 + 16, t0, TBLK), lambda cs, ce: xn[:, cs:ce, :], xnD, xnB)
                    if xsrcB is not None:
                        _load_pieces(cx, xsrcB(c0, c0 + 16, t0, TBLK), lambda cs, ce: xtm[:, cs:ce, :], xtmD, xtmB)
                        cx.op("dve", lambda e, c0=c0: e.tensor_scalar(out=xn[:, c0:c0 + 16, :], in0=xn[:, c0:c0 + 16, :],
                                                                      scalar1=sel[:, 0:1], scalar2=None, op0=ALU.mult),
                              reads=[xnB, selB], writes=[xnB])
                        cx.op("dve", lambda e, c0=c0: e.scalar_tensor_tensor(out=xn[:, c0:c0 + 16, :], in0=xtm[:, c0:c0 + 16, :],
                                                                             scalar=sel[:, 1:2], in1=xn[:, c0:c0 + 16, :],
                                                                             op0=ALU.mult, op1=ALU.add),
                              reads=[xtmB, xnB, selB], writes=[xnB])
            else:
                for n in range(NB):
                    ts = t0 + n * 512
                    _load_pieces(cx, xsrc(0, KC, ts, 512), lambda cs, ce: xs[:, cs:ce, :], xsB, xsB)
                    for c in range(KC):
                        cx.op("act", lambda e, c=c: e.activation(out=sqb[:, c, :], in_=xs[:, c, :], func=AF.Square),
                              reads=[xsB], writes=[sqB] if c == 0 else [])
                        if c > 0:
                            sqB.w = {id(cx.esem["act"]): (cx.esem["act"], cx.ecnt["act"])}
                    for c in range(KC):
                        cx.op("pe", lambda e, c=c: e.matmul(pssum[:], lhsT=ones_bf[:], rhs=sqb[:, c, :],
                                                            start=(c == 0), stop=(c == KC - 1)),
                              reads=[sqB, onesB], writes=[pssB])
                    cx.op("act", lambda e: e.activation(out=tmp[:], in_=pssum[:], func=AF.Ln, scale=1.0 / Fin, bias=eps_sb[:]),
                          reads=[pssB, epsB], writes=[tmpB])
                    cx.op("act", lambda e: e.activation(out=rstd[:], in_=tmp[:], func=AF.Exp, scale=-0.5),
                          reads=[tmpB], writes=[rstdB])
                    for c in range(KC):
                        cx.op("dve", lambda e, c=c, n=n: e.scalar_tensor_tensor(
                            out=xn[:, c, n * 512:(n + 1) * 512], in0=xs[:, c, :], scalar=gsb[:, c:c + 1], in1=rstd[:],
                            op0=ALU.mult, op1=ALU.mult),
                            reads=[xsB, gB, rstdB], writes=[xnB] if (c == 0 and n == 0) else [])
                        xnB.w = {id(cx.esem["dve"]): (cx.esem["dve"], cx.ecnt["dve"])}
            NMG = MC // G

            def issue_w(mg):
                wi = (wbase + mg) % NW
                cx.dma("pool", lambda e, wi=wi, mg=mg: e.dma_start(
                    out=wb[wi][:], in_=Wt[mg * G:(mg + 1) * G].rearrange("g p f -> p g f"), max_dma_last_dim=8192),
                    wB[wi], writes=[wB[wi]])

            wbase = wcnt
            wcnt += NMG
            iters = [(m, n) for m in range(MC) for n in range(NB)]

            def issue_res(i):
                m, n = iters[i]
                ri = i % 3
                ts = t0 + n * 512
                cx.dma("sp", lambda e, ri=ri, m=m, ts=ts: e.dma_start(
                    out=rsd[ri][:], in_=resT[m * 128:(m + 1) * 128, ts:ts + 512]), rsdB[ri], writes=[rsdB[ri]])

            for mg in range(min(NW - 1, NMG)):
                issue_w(mg)
            if resT is not None:
                for i in range(min(2, len(iters))):
                    issue_res(i)
            it = 0
            for mg in range(NMG):
                if mg + NW - 1 < NMG:
                    issue_w(mg + NW - 1)
                wi = (wbase + mg) % NW
                for g in range(G):
                    m = mg * G + g
                    for n in range(NB):
                        pi = cnt % NPS
                        oi = cnt % NO
                        cnt += 1
                        for c in range(KC):
                            cx.op("pe", lambda e, pi=pi, wi=wi, g=g, c=c, n=n: e.matmul(
                                pss[pi][:], lhsT=wb[wi][:, g, c * 128:(c + 1) * 128], rhs=xn[:, c, n * 512:(n + 1) * 512],
                                start=(c == 0), stop=(c == KC - 1)),
                                reads=[wB[wi], xnB], writes=[psB[pi]])
                        ts = t0 + n * 512
                        dst = outT[m * 128:(m + 1) * 128, ts:ts + 512]
                        if act == "relu2":
                            ri = cnt % 2
                            cx.op("act", lambda e, pi=pi, ri=ri: e.activation(out=rst[ri][:], in_=pss[pi][:], func=AF.Relu),
                                  reads=[psB[pi]], writes=[rstB[ri]])
                            cx.op("dve", lambda e, ri=ri, oi=oi: e.tensor_tensor(out=ost[oi][:], in0=rst[ri][:], in1=rst[ri][:],
                                                                                 op=ALU.mult),
                                  reads=[rstB[ri]], writes=[ostB[oi]])
                        elif resT is not None:
                            ri = it % 3
                            if it + 2 < len(iters):
                                issue_res(it + 2)
                            cx.op("dve", lambda e, pi=pi, ri=ri, oi=oi: e.tensor_tensor(
                                out=ost[oi][:], in0=pss[pi][:], in1=rsd[ri][:], op=ALU.add),
                                reads=[psB[pi], rsdB[ri]], writes=[ostB[oi]])
                        else:
                            if cnt % 2 == 0:
                                cx.op("act", lambda e, pi=pi, oi=oi: e.activation(out=ost[oi][:], in_=pss[pi][:], func=AF.Copy),
                                      reads=[psB[pi]], writes=[ostB[oi]])
                            else:
                                cx.op("dve", lambda e, pi=pi, oi=oi: e.tensor_copy(out=ost[oi][:], in_=pss[pi][:]),
                                      reads=[psB[pi]], writes=[ostB[oi]])
                        cx.dma("sp", lambda e, oi=oi, dst=dst: e.dma_start(out=dst, in_=ost[oi][:]), ostB[oi], reads=[ostB[oi]])
                        it += 1


import ml_dtypes

NPBF = ml_dtypes.bfloat16
BIGNEG = -30000.0
ATT_SCALE = 128 ** -0.5


def tile_w(W):
    Fin, Fout = W.shape
    KC, MC = Fin // 128, Fout // 128
    return np.ascontiguousarray(W.reshape(KC, 128, MC, 128).transpose(2, 1, 0, 3)).reshape(MC, 128, KC * 128)


def fm(v):
    return np.ascontiguousarray(v.reshape(-1, 128).T)


def _const_tables():
    c = {}
    c["c_ones_bf"] = np.ones((128, 128), NPBF)
    c["c_ident_bf"] = np.eye(128, dtype=np.float32).astype(NPBF)
    c["c_eps"] = np.full((128, 1), EPS, np.float32)
    seg = np.ones((128, 512), np.float32)
    seg[:, ::32] = 0.0
    c["c_segmask"] = seg
    s = np.arange(128)[:, None]
    t = np.arange(128)[None, :]
    c["c_gmaskT"] = ((s <= t) & (s // 32 == t // 32)).astype(np.float32)
    pm = np.zeros((128, 16, 8), np.float32)
    for tile in range(16):
        qb = tile // 2
        pm[:, tile, qb:] = -1e30
    c["c_pastmask"] = pm
    E = np.zeros((8, 8, 128), np.float32)
    for n in range(8):
        E[n, n, :] = 1.0
    c["c_E"] = E.astype(NPBF)
    s = np.arange(128)[:, None]
    t = np.arange(256)[None, :]
    c["c_CM0"] = np.where(s > t, BIGNEG, 0.0).astype(np.float32).astype(NPBF)
    return c


CONST_DT = {"c_ones_bf": BF16, "c_ident_bf": BF16, "c_eps": F32, "c_segmask": F32, "c_gmaskT": F32,
            "c_pastmask": F32, "c_E": BF16, "c_CM0": BF16}


def const_inputs():
    return _const_tables()


def setup_consts(cx, names=None):
    nc = cx.nc
    tabs = _const_tables()
    out = {}
    ld = cx.dmabuf("constld")
    for k, arr in tabs.items():
        d = nc.dram_tensor(k, list(arr.shape), CONST_DT[k], kind="ExternalInput").ap()
        if names is not None and k not in names:
            continue
        t = cx.es.enter_context(nc.sbuf_tensor("sb_" + k, list(arr.shape), CONST_DT[k]))
        b = Buf(k)
        cx.dma("sp", lambda e, t=t, d=d: e.dma_start(out=t[:], in_=d), ld, writes=[b])
        out[k[2:]] = (t, b)
    return out


def emit_gla(cx, projT, lbT, hgT, ogT, NH, S, layer, consts, tag="gla"):
    nc = cx.nc
    NBLK = S // 512
    pv = projT.rearrange("(a j p) t -> p a j t", a=4, j=NH, p=128)
    ones_bf, onesB = consts["ones_bf"]
    ident, identB = consts["ident_bf"]
    eps_sb, epsB = consts["eps"]
    segm, segB = consts["segmask"]
    gmask, gmB = consts["gmaskT"]
    with cx.scope() as sc:
        def T2(name, dt=F32, shape=(128, 512)):
            return [sc.sb(f"{name}{i}", list(shape), dt) for i in range(2)], [Buf(f"{name}{i}") for i in range(2)]
        inb = [sc.sb(f"in{i}", [128, 4, 512], F32) for i in range(2)]
        inB = [cx.dmabuf(f"{tag}_in{i}") for i in range(2)]
        qs, qsB = T2("qs")
        gate, gateB = T2("gate")
        ee, eeB = T2("ee")
        rr, rrB = T2("rr")
        ff, ffB = T2("ff")
        kk, kkB = T2("kk")
        lf, lfB = T2("lf")
        bc, bcB = T2("bc")
        eb, ebB = T2("eb")
        einv, einvB = T2("einv")
        qtb, qtbB = T2("qtb", BF16)
        ktb, ktbB = T2("ktb", BF16)
        khb, khbB = T2("khb", BF16)
        ib, ibB = T2("ib", BF16)
        vsb, vsbB = T2("vsb", BF16, (128, 4, 128))
        v32, v32B = T2("v32", BF16, (32, 16, 128))
        kh32, kh32B = T2("kh32", BF16, (32, 16, 128))
        amb, ambB = T2("amb", BF16, (128, 128))
        sbf, sbfB = T2("sbf", BF16, (128, 128))
        osb, osbB = T2("osb")
        osq, osqB = T2("osq", BF16)
        tmp, tmpB = T2("tmp")
        rstd, rstdB = T2("rstd")
        ogb = [sc.sb(f"ogb{i}", [128, 512], BF16) for i in range(2)]
        ogB = [cx.dmabuf(f"{tag}_og{i}") for i in range(2)]
        s32 = sc.sb("s32", [128, 128], F32)
        s32B = Buf("s32")
        lbr = sc.sb("lbr", [128, 2, NH], F32)
        lbB = cx.dmabuf(f"{tag}_lb")
        hg = sc.sb("hg", [128, NH], F32)
        hgB = cx.dmabuf(f"{tag}_hg")
        lb = sc.sb("lb", [128, NH], F32)
        oml = sc.sb("oml", [128, NH], F32)
        lbcB = Buf("lbc")
        psTr = [sc.ps(f"psTr{i}", [128, 1024], BF16) for i in range(2)]
        psTrB = [Buf(f"psTr{i}") for i in range(2)]
        psA = [sc.ps(f"psA{i}", [128, 512], F32) for i in range(2)]
        psAB = [Buf(f"psA{i}") for i in range(2)]
        psO = [sc.ps(f"psO{i}", [128, 512], F32) for i in range(2)]
        psOB = [Buf(f"psO{i}") for i in range(2)]
        psS = sc.ps("psS", [128, 512], F32)
        psSB = Buf("psS")
        psN = sc.ps("psN", [128, 512], F32)
        psNB = Buf("psN")

        cx.dma("sp", lambda e: e.dma_start(out=lbr[:], in_=lbT), lbB, writes=[lbB])
        cx.dma("sp", lambda e: e.dma_start(out=hg[:], in_=hgT), hgB, writes=[hgB])
        if layer == 0:
            cx.op("dve", lambda e: e.memset(lb[:], 0.0), writes=[lbcB])
            cx.op("dve", lambda e: e.memset(oml[:], 1.0), writes=[])
        else:
            cx.op("dve", lambda e: e.tensor_tensor(out=lb[:], in0=lbr[:, 0, :], in1=lbr[:, 1, :], op=ALU.subtract),
                  reads=[lbB], writes=[lbcB])
            cx.op("act", lambda e: e.activation(out=lb[:], in_=lb[:], func=AF.Exp), reads=[lbcB], writes=[lbcB])
            cx.op("dve", lambda e: e.tensor_scalar(out=lb[:], in0=lb[:], scalar1=1.0, scalar2=None, op0=ALU.add),
                  reads=[lbcB], writes=[lbcB])
            cx.op("dve", lambda e: e.reciprocal(out=lb[:], in_=lb[:]), reads=[lbcB], writes=[lbcB])
            cx.op("dve", lambda e: e.tensor_scalar(out=oml[:], in0=lb[:], scalar1=-1.0, scalar2=1.0, op0=ALU.mult, op1=ALU.add),
                  reads=[lbcB], writes=[])
        lbcB.w = {id(cx.esem["dve"]): (cx.esem["dve"], cx.ecnt["dve"])}

        blocks = [(j, tb) for j in range(NH) for tb in range(NBLK)]

        def issue_in(i):
            j, tb = blocks[i]
            b = i % 2
            cx.dma("sp", lambda e, b=b, j=j, tb=tb: e.dma_start(out=inb[b][:], in_=pv[:, :, j, tb * 512:(tb + 1) * 512]),
                   inB[b], writes=[inB[b]])

        issue_in(0)
        trc = 0
        ac = 0
        sc_i = 0
        for bi, (j, tb) in enumerate(blocks):
            b = bi % 2
            if bi + 1 < len(blocks):
                issue_in(bi + 1)
            t0 = tb * 512
            X = inb[b]
            if tb == 0:
                cx.op("dve", lambda e: e.memset(s32[:], 0.0), writes=[s32B])
                cx.op("dve", lambda e, k=sc_i % 2: e.memset(sbf[k][:], 0.0), writes=[sbfB[sc_i % 2]])
            cx.op("act", lambda e: e.activation(out=qs[b][:], in_=X[:, 0, :], func=AF.Silu), reads=[inB[b]], writes=[qsB[b]])
            cx.op("act", lambda e: e.activation(out=gate[b][:], in_=X[:, 3, :], func=AF.Silu), reads=[inB[b]], writes=[gateB[b]])
            cx.op("act", lambda e: e.activation(out=ee[b][:], in_=X[:, 1, :], func=AF.Exp, scale=-1.0), reads=[inB[b]], writes=[eeB[b]])
            cx.op("act", lambda e: e.activation(out=ib[b][:], in_=X[:, 2, :], func=AF.Copy), reads=[inB[b]], writes=[ibB[b]])
            cx.op("dve", lambda e: e.tensor_scalar(out=ee[b][:], in0=ee[b][:], scalar1=1.0, scalar2=None, op0=ALU.add),
                  reads=[eeB[b]], writes=[eeB[b]])
            cx.op("dve", lambda e: e.reciprocal(out=rr[b][:], in_=ee[b][:]), reads=[eeB[b]], writes=[rrB[b]])
            cx.op("dve", lambda e: e.tensor_scalar(out=ff[b][:], in0=rr[b][:], scalar1=oml[:, j:j + 1], scalar2=lb[:, j:j + 1],
                                                   op0=ALU.mult, op1=ALU.add), reads=[rrB[b], lbcB], writes=[ffB[b]])
            cx.op("pool", lambda e: e.tensor_scalar(out=kk[b][:], in0=ff[b][:], scalar1=-1.0, scalar2=1.0, op0=ALU.mult, op1=ALU.add),
                  reads=[ffB[b]], writes=[kkB[b]])
            cx.op("act", lambda e: e.activation(out=lf[b][:], in_=ff[b][:], func=AF.Ln), reads=[ffB[b]], writes=[lfB[b]])
            cx.op("dve", lambda e: e.tensor_tensor_scan(out=bc[b][:], data0=segm[:], data1=lf[b][:], initial=0.0,
                                                        op0=ALU.mult, op1=ALU.add), reads=[lfB[b], segB], writes=[bcB[b]])
            cx.op("act", lambda e: e.activation(out=eb[b][:], in_=bc[b][:], func=AF.Exp), reads=[bcB[b]], writes=[ebB[b]])
            cx.op("dve", lambda e: e.tensor_tensor(out=qtb[b][:], in0=qs[b][:], in1=eb[b][:], op=ALU.mult),
                  reads=[qsB[b], ebB[b]], writes=[qtbB[b]])
            cx.op("dve", lambda e: e.reciprocal(out=einv[b][:], in_=eb[b][:]), reads=[ebB[b]], writes=[einvB[b]])
            cx.op("dve", lambda e: e.tensor_tensor(out=ktb[b][:], in0=kk[b][:], in1=einv[b][:], op=ALU.mult),
                  reads=[kkB[b], einvB[b]], writes=[ktbB[b]])
            for c in range(16):
                cx.op("pool", lambda e, c=c: e.tensor_scalar(out=khb[b][:, 32 * c:32 * c + 32], in0=ktb[b][:, 32 * c:32 * c + 32],
                                                             scalar1=eb[b][:, 32 * c + 31:32 * c + 32], scalar2=None, op0=ALU.mult),
                      reads=[ktbB[b], ebB[b]], writes=[khbB[b]] if c == 0 else [])
            khbB[b].w = {id(cx.esem["pool"]): (cx.esem["pool"], cx.ecnt["pool"])}
            tr = trc % 2
            trc += 1
            for i in range(4):
                cx.op("pe", lambda e, tr=tr, i=i: e.transpose(out=psTr[tr][:, i * 128:(i + 1) * 128],
                                                              in_=ib[b][:, i * 128:(i + 1) * 128], identity=ident[:]),
                      reads=[ibB[b], identB], writes=[psTrB[tr]])
            cx.op("dve", lambda e, tr=tr: e.tensor_copy(out=vsb[b][:].rearrange("p a b -> p (a b)"), in_=psTr[tr][:, 0:512]),
                  reads=[psTrB[tr]], writes=[vsbB[b]])
            for (src_, srcB, dst, dstB) in ((ib[b], ibB[b], v32[b], v32B[b]), (khb[b], khbB[b], kh32[b], kh32B[b])):
                for h in range(2):
                    tr = trc % 2
                    trc += 1
                    for i in range(8):
                        cc = h * 8 + i
                        cx.op("pe", lambda e, tr=tr, i=i, cc=cc, src_=src_: e.transpose(
                            out=psTr[tr][0:32, i * 128:(i + 1) * 128], in_=src_[:, cc * 32:(cc + 1) * 32], identity=ident[:]),
                            reads=[srcB, identB], writes=[psTrB[tr]])
                    cx.op("act" if h == 0 else "dve", lambda e, tr=tr, dst=dst, h=h: (e.activation(
                        out=dst[:, h * 8:(h + 1) * 8, :].rearrange("p a b -> p (a b)"), in_=psTr[tr][0:32, :], func=AF.Copy) if h == 0 else
                        e.tensor_copy(out=dst[:, h * 8:(h + 1) * 8, :].rearrange("p a b -> p (a b)"), in_=psTr[tr][0:32, :])),
                        reads=[psTrB[tr]], writes=[dstB] if h == 0 else [])
                    if h == 1:
                        dstB.w[id(cx.esem["dve"])] = (cx.esem["dve"], cx.ecnt["dve"])
            po = bi % 2
            for i in range(4):
                a = ac % 2
                ac += 1
                cs = slice(i * 128, (i + 1) * 128)
                cx.op("pe", lambda e, a=a, cs=cs: e.matmul(psA[a][:, 0:128], lhsT=ktb[b][:, cs], rhs=qtb[b][:, cs], start=True, stop=True),
                      reads=[ktbB[b], qtbB[b]], writes=[psAB[a]])
                cx.op("dve", lambda e, a=a: e.tensor_tensor(out=amb[a][:], in0=psA[a][:, 0:128], in1=gmask[:], op=ALU.mult),
                      reads=[psAB[a], gmB], writes=[ambB[a]])
                cx.op("pe", lambda e, a=a, i=i, cs=cs: e.matmul(psO[po][:, cs], lhsT=vsb[b][:, i, :], rhs=amb[a][:], start=True, stop=False),
                      reads=[vsbB[b], ambB[a]], writes=[psOB[po]])
                for c in range(4):
                    k = sc_i % 2
                    c0 = i * 128 + 32 * c
                    cx.op("pe", lambda e, k=k, c0=c0: e.matmul(psO[po][:, c0:c0 + 32], lhsT=sbf[k][:], rhs=qtb[b][:, c0:c0 + 32],
                                                               start=False, stop=(c0 % 128 == 96)),
                          reads=[sbfB[k], qtbB[b]], writes=[psOB[po]])
                    cx.op("pe", lambda e, i=i, c=c: e.matmul(psS[:, 0:128], lhsT=kh32[b][:, 4 * i + c, :],
                                                             rhs=v32[b][:, 4 * i + c, :], start=True, stop=True),
                          reads=[kh32B[b], v32B[b]], writes=[psSB])
                    cx.op("dve", lambda e, c0=c0: e.scalar_tensor_tensor(out=s32[:], in0=s32[:], scalar=eb[b][:, c0 + 31:c0 + 32],
                                                                         in1=psS[:, 0:128], op0=ALU.mult, op1=ALU.add),
                          reads=[s32B, ebB[b], psSB], writes=[s32B])
                    sc_i += 1
                    k2 = sc_i % 2
                    cx.op("act", lambda e, k2=k2: e.activation(out=sbf[k2][:], in_=s32[:], func=AF.Copy),
                          reads=[s32B], writes=[sbfB[k2]])
            cx.op("act", lambda e: e.activation(out=osb[b][:], in_=psO[po][:], func=AF.Copy), reads=[psOB[po]], writes=[osbB[b]])
            cx.op("act", lambda e: e.activation(out=osq[b][:], in_=psO[po][:], func=AF.Square), reads=[psOB[po]], writes=[osqB[b]])
            cx.op("pe", lambda e: e.matmul(psN[:], lhsT=ones_bf[:], rhs=osq[b][:], start=True, stop=True),
                  reads=[onesB, osqB[b]], writes=[psNB])
            cx.op("act", lambda e: e.activation(out=tmp[b][:], in_=psN[:], func=AF.Ln, scale=1.0 / 128, bias=eps_sb[:]),
                  reads=[psNB, epsB], writes=[tmpB[b]])
            cx.op("act", lambda e: e.activation(out=rstd[b][:], in_=tmp[b][:], func=AF.Exp, scale=-0.5), reads=[tmpB[b]], writes=[rstdB[b]])
            cx.op("dve", lambda e: e.tensor_tensor(out=osb[b][:], in0=osb[b][:], in1=rstd[b][:], op=ALU.mult),
                  reads=[osbB[b], rstdB[b]], writes=[osbB[b]])
            cx.op("dve", lambda e: e.scalar_tensor_tensor(out=ogb[b][:], in0=osb[b][:], scalar=hg[:, j:j + 1], in1=gate[b][:],
                                                          op0=ALU.mult, op1=ALU.mult),
                  reads=[osbB[b], hgB, gateB[b]], writes=[ogB[b]])
            cx.dma("sp", lambda e, j=j, t0=t0: e.dma_start(out=ogT[j * 128:(j + 1) * 128, t0:t0 + 512], in_=ogb[b][:]),
                   ogB[b], reads=[ogB[b]])


def moba_tables(heads):
    NH = len(heads)
    H = 16
    slopes = np.array([2.0 ** (-8.0 * (h + 1) / H) for h in heads], np.float64)
    ALA = np.zeros((2, NH, 128), np.float32)
    ALB = np.zeros((2, NH, 256), np.float32)
    bt = np.zeros((128, NH, 16), np.float32)
    for j in range(NH):
        ALA[0, j, :] = 1.0
        ALA[1, j, :] = slopes[j] / ATT_SCALE * np.arange(128)
        ALB[0, j, :] = -slopes[j] / ATT_SCALE * np.arange(256)
        ALB[1, j, :] = 1.0
        for d in range(16):
            bt[:, j, d] = -slopes[j] * 128.0 * (d - 1)
    return ALA, ALB, bt


def emit_moba(cx, qT, kvT, qgT, kgT, alaT, albT, btT, ogT, NH, S, consts, tag="moba"):
    nc = cx.nc
    NT = S // 128
    NQB = S // 256
    assert NQB == 8
    ones_bf, onesB = consts["ones_bf"]
    ident, identB = consts["ident_bf"]
    eps_sb, epsB = consts["eps"]
    pastm, pastB = consts["pastmask"]
    E_sb, EB = consts["E"]
    CM0, CMB = consts["CM0"]
    with cx.scope() as sc:
        raw = [sc.sb(f"raw{i}", [128, S], F32) for i in range(2)]
        rawB = [cx.dmabuf(f"{tag}_raw{i}") for i in range(2)]
        sq = sc.sb("sq", [128, S], BF16); sqB = Buf("sq")
        tmp = sc.sb("tmp", [128, 512], F32); tmpB = Buf("tmp")
        rstd = sc.sb("rstd", [128, S], F32); rstdB = Buf("rstd")
        kn32 = sc.sb("kn32", [128, S], F32); kn32B = Buf("kn32")
        knb = sc.sb("knb", [128, S], BF16); knbB = Buf("knb")
        qnb = sc.sb("qnb", [128, S], BF16); qnbB = Buf("qnb")
        vb = sc.sb("vb", [128, S], BF16); vbB = Buf("vb")
        vsb = sc.sb("vsb", [128, NT, 128], BF16); vsbB = Buf("vsb")
        kms = sc.sb("kms", [128, 8], F32); kmsB = Buf("kms")
        kmb = sc.sb("kmb", [128, 8], BF16); kmbB = Buf("kmb")
        gm = sc.sb("gm", [128, NT, 8], F32); gmB = Buf("gm")
        top8 = sc.sb("top8", [128, NT, 8], F32); top8B = Buf("top8")
        bb = sc.sb("bb", [128, NT, 8], BF16); bbB = Buf("bb")
        bbT = sc.sb("bbT", [8, S], BF16); bbTB = Buf("bbT")
        pT = [sc.sb(f"pT{i}", [128, 256], BF16) for i in range(2)]
        pTB = [Buf(f"pT{i}") for i in range(2)]
        rec = sc.sb("rec", [128, 256], F32); recB = Buf("rec")
        ogb = [sc.sb(f"ogb{i}", [128, 256], BF16) for i in range(2)]
        ogB = [cx.dmabuf(f"{tag}_og{i}") for i in range(2)]
        qg = sc.sb("qg", [128, 1], F32); kg = sc.sb("kg", [128, 1], F32)
        ala = sc.sb("ala", [2, NH, 128], F32); alb = sc.sb("alb", [2, NH, 256], F32)
        bt = sc.sb("bt", [128, NH, 16], F32)
        tabB = cx.dmabuf(f"{tag}_tab")
        for (t_, d_) in ((qg, qgT), (kg, kgT), (ala, alaT), (alb, albT), (bt, btT)):
            cx.dma("sp", lambda e, t_=t_, d_=d_: e.dma_start(out=t_[:], in_=d_), tabB, writes=[tabB])
        psN = sc.ps("psN", [128, 512], F32); psNB = Buf("psN")
        psTr = [sc.ps(f"psTr{i}", [128, 1024], BF16) for i in range(2)]
        psTrB = [Buf(f"psTr{i}") for i in range(2)]
        psG = sc.ps("psG", [128, 512], F32); psGB = Buf("psG")
        psS = [sc.ps(f"psS{i}", [128, 512], F32) for i in range(2)]
        psSB = [Buf(f"psS{i}") for i in range(2)]
        psO = sc.ps("psO", [128, 512], F32); psOB = Buf("psO")
        psR = sc.ps("psR", [128, 512], F32); psRB = Buf("psR")

        rc = 0
        trc = 0
        sc_i = 0
        oc = 0

        def load_raw(src_rows):
            nonlocal rc
            r = rc % 2
            rc += 1
            cx.dma("sp", lambda e, r=r: e.dma_start(out=raw[r][:], in_=src_rows), rawB[r], writes=[rawB[r]])
            return r

        def rms_rstd(r):
            for n in range(S // 512):
                cs = slice(n * 512, (n + 1) * 512)
                cx.op("act", lambda e, cs=cs: e.activation(out=sq[:, cs], in_=raw[r][:, cs], func=AF.Square),
                      reads=[rawB[r]], writes=[sqB] if n == 0 else [])
                sqB.w = {id(cx.esem["act"]): (cx.esem["act"], cx.ecnt["act"])}
                cx.op("pe", lambda e, cs=cs: e.matmul(psN[:], lhsT=ones_bf[:], rhs=sq[:, cs], start=True, stop=True),
                      reads=[sqB, onesB], writes=[psNB])
                cx.op("act", lambda e: e.activation(out=tmp[:], in_=psN[:], func=AF.Ln, scale=1.0 / 128, bias=eps_sb[:]),
                      reads=[psNB, epsB], writes=[tmpB])
                cx.op("act", lambda e, cs=cs: e.activation(out=rstd[:, cs], in_=tmp[:], func=AF.Exp, scale=-0.5),
                      reads=[tmpB], writes=[rstdB] if n == 0 else [])
                rstdB.w = {id(cx.esem["act"]): (cx.esem["act"], cx.ecnt["act"])}

        for j in range(NH):
            r = load_raw(kvT[j * 128:(j + 1) * 128, :])
            rms_rstd(r)
            cx.op("dve", lambda e: e.scalar_tensor_tensor(out=kn32[:], in0=raw[r][:], scalar=kg[:, 0:1], in1=rstd[:],
                                                          op0=ALU.mult, op1=ALU.mult),
                  reads=[rawB[r], tabB, rstdB], writes=[kn32B])
            cx.op("pool", lambda e: e.tensor_copy(out=knb[:], in_=kn32[:]), reads=[kn32B], writes=[knbB])
            cx.op("dve", lambda e: e.tensor_reduce(out=kms[:], in_=kn32[:].rearrange("p (n k) -> p n k", k=256),
                                                   axis=AX.X, op=ALU.add), reads=[kn32B], writes=[kmsB])
            cx.op("dve", lambda e: e.tensor_scalar(out=kmb[:], in0=kms[:], scalar1=1.0 / 256, scalar2=None, op0=ALU.mult),
                  reads=[kmsB], writes=[kmbB])
            r = load_raw(kvT[(NH + j) * 128:(NH + j + 1) * 128, :])
            cx.op("act", lambda e: e.activation(out=vb[:], in_=raw[r][:], func=AF.Copy), reads=[rawB[r]], writes=[vbB])
            for h in range(NT // 8):
                tr = trc % 2
                trc += 1
                for i in range(8):
                    tt = h * 8 + i
                    cx.op("pe", lambda e, tr=tr, i=i, tt=tt: e.transpose(out=psTr[tr][:, i * 128:(i + 1) * 128],
                                                                        in_=vb[:, tt * 128:(tt + 1) * 128], identity=ident[:]),
                          reads=[vbB, identB], writes=[psTrB[tr]])
                cx.op("dve", lambda e, tr=tr, h=h: e.tensor_copy(out=vsb[:, h * 8:(h + 1) * 8, :].rearrange("p a b -> p (a b)"),
                                                                 in_=psTr[tr][:, :]),
                      reads=[psTrB[tr]], writes=[vsbB] if h == 0 else [])
                vsbB.w = {id(cx.esem["dve"]): (cx.esem["dve"], cx.ecnt["dve"])}
            r = load_raw(qT[j * 128:(j + 1) * 128, :])
            rms_rstd(r)
            cx.op("dve", lambda e: e.scalar_tensor_tensor(out=qnb[:], in0=raw[r][:], scalar=qg[:, 0:1], in1=rstd[:],
                                                          op0=ALU.mult, op1=ALU.mult),
                  reads=[rawB[r], tabB, rstdB], writes=[qnbB])
            for tt in range(NT):
                cx.op("pe", lambda e, tt=tt: e.matmul(psG[:, tt * 8:(tt + 1) * 8], lhsT=qnb[:, tt * 128:(tt + 1) * 128], rhs=kmb[:],
                                                      start=True, stop=True),
                      reads=[qnbB, kmbB], writes=[psGB])
            cx.op("dve", lambda e: e.tensor_tensor(out=gm[:].rearrange("p a b -> p (a b)"), in0=psG[:, 0:NT * 8],
                                                   in1=pastm[:].rearrange("p a b -> p (a b)"), op=ALU.add),
                  reads=[psGB, pastB], writes=[gmB])
            for tt in range(NT):
                cx.op("dve", lambda e, tt=tt: e.max(out=top8[:, tt, :], in_=gm[:, tt, :]), reads=[gmB],
                      writes=[top8B] if tt == 0 else [])
            top8B.w = {id(cx.esem["dve"]): (cx.esem["dve"], cx.ecnt["dve"])}
            for tt in range(NT):
                cx.op("dve", lambda e, tt=tt: e.tensor_scalar(out=bb[:, tt, :], in0=gm[:, tt, :], scalar1=top8[:, tt, 2:3],
                                                              scalar2=BIGNEG, op0=ALU.is_lt, op1=ALU.mult),
                      reads=[gmB, top8B], writes=[bbB] if tt == 0 else [])
            bbB.w = {id(cx.esem["dve"]): (cx.esem["dve"], cx.ecnt["dve"])}
            for h in range(NT // 8):
                tr = trc % 2
                trc += 1
                for i in range(8):
                    tt = h * 8 + i
                    cx.op("pe", lambda e, tr=tr, i=i, tt=tt: e.transpose(out=psTr[tr][0:8, i * 128:(i + 1) * 128],
                                                                        in_=bb[:, tt, :], identity=ident[:]),
                          reads=[bbB, identB], writes=[psTrB[tr]])
                cx.op("dve", lambda e, tr=tr, h=h: e.tensor_copy(out=bbT[:, h * 1024:(h + 1) * 1024], in_=psTr[tr][0:8, :]),
                      reads=[psTrB[tr]], writes=[bbTB] if h == 0 else [])
                bbTB.w = {id(cx.esem["dve"]): (cx.esem["dve"], cx.ecnt["dve"])}
            for qb in range(NQB):
                q0 = qb * 256
                subs = [(2 * qb, 0, 256, True), (2 * qb + 1, 128, 128, True)] + [(ks, 0, 256, False) for ks in range(2 * qb)]
                for si, (ks, lo, N, own) in enumerate(subs):
                    s_ = sc_i % 2
                    sc_i += 1
                    qc = slice(q0 + lo, q0 + lo + N)
                    cx.op("pe", lambda e, s_=s_, ks=ks, qc=qc, N=N: e.matmul(psS[s_][:, 0:N], lhsT=knb[:, ks * 128:(ks + 1) * 128],
                                                                             rhs=qnb[:, qc], start=True, stop=False),
                          reads=[knbB, qnbB], writes=[psSB[s_]])
                    cx.op("pe", lambda e, s_=s_, lo=lo, N=N: e.matmul(psS[s_][:, 0:N], lhsT=ala[0:2, j, :], rhs=alb[0:2, j, lo:lo + N],
                                                                      start=False, stop=False),
                          reads=[tabB], writes=[psSB[s_]])
                    if own:
                        cx.op("pe", lambda e, s_=s_, N=N: e.matmul(psS[s_][:, 0:N], lhsT=ident[:], rhs=CM0[:, 0:N],
                                                                   start=False, stop=True),
                              reads=[identB, CMB], writes=[psSB[s_]])
                    else:
                        n = ks // 2
                        cx.op("pe", lambda e, s_=s_, n=n, qc=qc, N=N: e.matmul(psS[s_][:, 0:N], lhsT=E_sb[0:8, n, :], rhs=bbT[0:8, qc],
                                                                               start=False, stop=True),
                              reads=[EB, bbTB], writes=[psSB[s_]])
                    didx = 2 * qb - ks + 1
                    cx.op("act", lambda e, s_=s_, N=N, didx=didx: e.activation(out=pT[s_][:, 0:N], in_=psS[s_][:, 0:N], func=AF.Exp,
                                                                               scale=ATT_SCALE, bias=bt[:, j, didx:didx + 1]),
                          reads=[psSB[s_], tabB], writes=[pTB[s_]])
                    last = (si == len(subs) - 1)
                    cx.op("pe", lambda e, s_=s_, ks=ks, lo=lo, N=N, si=si, last=last: e.matmul(
                        psO[:, lo:lo + N], lhsT=vsb[:, ks, :], rhs=pT[s_][:, 0:N], start=(si == 0), stop=last),
                        reads=[vsbB, pTB[s_]], writes=[psOB])
                    cx.op("pe", lambda e, s_=s_, lo=lo, N=N, si=si, last=last: e.matmul(
                        psR[:, lo:lo + N], lhsT=ones_bf[:], rhs=pT[s_][:, 0:N], start=(si == 0), stop=last),
                        reads=[onesB, pTB[s_]], writes=[psRB])
                o_ = oc % 2
                oc += 1
                cx.op("dve", lambda e: e.reciprocal(out=rec[:], in_=psR[:, 0:256]), reads=[psRB], writes=[recB])
                cx.op("dve", lambda e, o_=o_: e.tensor_tensor(out=ogb[o_][:], in0=psO[:, 0:256], in1=rec[:], op=ALU.mult),
                      reads=[psOB, recB], writes=[ogB[o_]])
                cx.dma("sp", lambda e, o_=o_, q0=q0: e.dma_start(out=ogT[j * 128:(j + 1) * 128, q0:q0 + 256], in_=ogb[o_][:]),
                       ogB[o_], reads=[ogB[o_]])


from concourse.bass_utils import run_bass_kernel_spmd

D_MODEL = 2048
SEQ = 2048
BATCH = 4
NHL = 8
D_FF = 8192
TOWN = 1024
NCORES = 8
_PROGS = {}


_DECL = {}


def _dram(nc, name, shape, dt, kind="ExternalInput"):
    if kind == "ExternalInput":
        _DECL[name] = (tuple(shape), dt)
    return nc.dram_tensor(name, list(shape), dt, kind=kind).ap()


def build_A(layer):
    nc = bass.Bass("TRN2", target_bir_lowering=False)
    cx = Ctx(nc)
    consts = setup_consts(cx)
    hT = _dram(nc, "hT", [D_MODEL, SEQ], F32)
    Wt = _dram(nc, "Wt", [4 * NHL, 128, D_MODEL], F32)
    gT = _dram(nc, "gT", [128, 16], F32)
    lbT = _dram(nc, "lbT", [128, 2, NHL], F32)
    hgT = _dram(nc, "hgT", [128, NHL], F32)
    projT = _dram(nc, "projT", [4 * NHL * 128, SEQ], F32, "Internal")
    ogT = _dram(nc, "ogT", [NHL * 128, SEQ], BF16, "ExternalOutput")
    emit_linear(cx, hT, Wt, projT, SEQ, D_MODEL, 4 * NHL * 128, F32, F32, consts, gainT=gT, tag="win")
    emit_gla(cx, projT, lbT, hgT, ogT, NHL, SEQ, layer, consts)
    cx.finish()
    return nc


def build_C(with_kv):
    nc = bass.Bass("TRN2", target_bir_lowering=False)
    cx = Ctx(nc)
    consts = setup_consts(cx)
    hT = _dram(nc, "hT", [D_MODEL, SEQ], F32)
    Wq = _dram(nc, "Wq", [NHL, 128, D_MODEL], F32)
    gT = _dram(nc, "gT", [128, 16], F32)
    qgT = _dram(nc, "qgT", [128, 1], F32)
    kgT = _dram(nc, "kgT", [128, 1], F32)
    alaT = _dram(nc, "alaT", [2, NHL, 128], F32)
    albT = _dram(nc, "albT", [2, NHL, 256], F32)
    btT = _dram(nc, "btT", [128, NHL, 16], F32)
    qT = _dram(nc, "qT", [NHL * 128, SEQ], F32, "Internal")
    ogT = _dram(nc, "ogT", [NHL * 128, SEQ], BF16, "ExternalOutput")
    if with_kv:
        Wkv = _dram(nc, "Wkv", [2 * NHL, 128, D_MODEL], F32)
        gkvT = _dram(nc, "gkvT", [128, 16], F32)
        kvT = _dram(nc, "kvT", [2 * NHL * 128, SEQ], F32, "ExternalOutput")
        emit_linear(cx, hT, Wkv, kvT, SEQ, D_MODEL, 2 * NHL * 128, F32, F32, consts, gainT=gkvT, tag="wkv")
    else:
        kvT = _dram(nc, "kvT", [2 * NHL * 128, SEQ], F32)
    emit_linear(cx, hT, Wq, qT, SEQ, D_MODEL, NHL * 128, F32, F32, consts, gainT=gT, tag="wq")
    emit_moba(cx, qT, kvT, qgT, kgT, alaT, albT, btT, ogT, NHL, SEQ, consts)
    cx.finish()
    return nc


def build_B():
    nc = bass.Bass("TRN2", target_bir_lowering=False)
    cx = Ctx(nc)
    consts = setup_consts(cx)
    ogT = _dram(nc, "ogT", [D_MODEL, TOWN], BF16)
    hT = _dram(nc, "hT", [D_MODEL, TOWN], F32)
    Wo = _dram(nc, "Wo", [16, 128, D_MODEL], F32)
    gT = _dram(nc, "gT", [128, 16], F32)
    W1 = _dram(nc, "W1", [64, 128, D_MODEL], F32)
    W2 = _dram(nc, "W2", [16, 128, D_FF], F32)
    h1T = _dram(nc, "h1T", [D_MODEL, TOWN], F32, "Internal")
    aT = _dram(nc, "aT", [D_FF, TOWN], BF16, "Internal")
    h2T = _dram(nc, "h2T", [D_MODEL, TOWN], F32, "ExternalOutput")
    emit_linear(cx, ogT, Wo, h1T, TOWN, D_MODEL, D_MODEL, BF16, F32, consts, resT=hT, tag="wo")
    emit_linear(cx, h1T, W1, aT, TOWN, D_MODEL, D_FF, F32, BF16, consts, gainT=gT, act="relu2", tag="w1")
    emit_linear(cx, aT, W2, h2T, TOWN, D_FF, D_MODEL, BF16, F32, consts, resT=h1T, tag="w2")
    cx.finish()
    return nc


PAIRS = [[0, 1], [2, 3], [4, 5], [6, 7]]


def build_fused(layers=(0, 1, 2, 3), info=None):
    nc = bass.Bass("TRN2", target_bir_lowering=False)
    cx = Ctx(nc)
    consts = setup_consts(cx)
    I = lambda n, s, dt=F32: _dram(nc, n, s, dt)
    N = lambda n, s, dt=F32: _dram(nc, n, s, dt, "Internal")
    rsel = I("rsel", [128, 2])
    hall0 = I("hall0", [2 * D_MODEL, TOWN])
    hT0 = I("hT0", [D_MODEL, TOWN])
    hall = N("hall", [2 * D_MODEL, TOWN])
    ogT = N("ogT", [NHL * 128, SEQ], BF16)
    ogall = N("ogall", [2 * NHL * 128, SEQ], BF16)
    projT = N("projT", [4 * NHL * 128, SEQ])
    qT = N("qT", [NHL * 128, SEQ])
    kvT = N("kvT", [2 * NHL * 128, SEQ])
    h1T = N("h1T", [D_MODEL, TOWN])
    aT = N("aT", [D_FF, TOWN], BF16)
    hbuf = [N("hA", [D_MODEL, TOWN]), N("hB", [D_MODEL, TOWN])]
    hout = _dram(nc, "hout", [D_MODEL, TOWN], F32, "ExternalOutput")
    agB = cx.dmabuf("ag")
    qgT = [None] * 2
    ogv = ogall.rearrange("(i r cc p) t -> r i p cc t", i=4, r=2, cc=2, p=128)

    def ogsrc(off):
        def f(c0, c1, t0, n):
            assert c0 == 0 and c1 == 16
            return [(8 * rr + 2 * i, 8 * rr + 2 * i + 2, ogv[rr, i, :, :, off + t0:off + t0 + n])
                    for rr in range(2) for i in range(4)]
        return f
    ogA = ogsrc(0)
    ogBf = ogsrc(TOWN)
    kgT = I("kgT", [128, 1])
    alaT = I("alaT", [2, NHL, 128])
    albT = I("albT", [2, NHL, 256])
    btT = I("btT", [128, NHL, 16])
    lbT = I("lbT", [128, 2, NHL])
    for l in layers:
        if l != layers[0]:
            cx.renew_engine_sems()
        h_cur = hT0 if l == layers[0] else hbuf[(l - 1) % 2]
        if l == layers[0]:
            src_all = hall0
        else:
            for i in range(8):
                cx.coll(lambda e, h_cur=h_cur, i=i: e.collective_compute(
                    "AllGather", ALU.bypass, replica_groups=PAIRS,
                    ins=[h_cur[256 * i:256 * (i + 1), :].opt()], outs=[hall[512 * i:512 * (i + 1), :].opt()]), agB)
            cx.barrier_on(agB)
            src_all = hall
        if l == layers[0]:
            sv0 = src_all.rearrange("(r c p) t -> r p c t", r=2, p=128)
            xs = lambda c0, c1, t0, n, sv0=sv0: [(c0, c1, sv0[t0 // TOWN, :, c0:c1, (t0 % TOWN):(t0 % TOWN) + n])]
        else:
            sv = src_all.rearrange("(i r cc p) t -> r i p cc t", i=8, r=2, cc=2, p=128)
            xs = lambda c0, c1, t0, n, sv=sv: [(2 * i, 2 * i + 2, sv[t0 // TOWN, i, :, :, (t0 % TOWN):(t0 % TOWN) + n])
                                               for i in range(c0 // 2, c1 // 2)]
        gT = I(f"gmix{l}", [128, 16])
        if l < 2:
            Wt = I(f"Win{l}", [4 * NHL, 128, D_MODEL])
            hgT = I(f"hg{l}", [128, NHL])
            emit_linear(cx, None, Wt, projT, SEQ, D_MODEL, 4 * NHL * 128, F32, F32, consts, gainT=gT, tag=f"win{l}", xsrc=xs)
            emit_gla(cx, projT, lbT, hgT, ogT, NHL, SEQ, l, consts, tag=f"gla{l}")
        else:
            Wq = I(f"Wq{l}", [NHL, 128, D_MODEL])
            qg = I(f"qg{l}", [128, 1])
            if l == 2:
                Wkv = I("Wkv", [2 * NHL, 128, D_MODEL])
                gkvT = I("gkv", [128, 16])
                emit_linear(cx, None, Wkv, kvT, SEQ, D_MODEL, 2 * NHL * 128, F32, F32, consts, gainT=gkvT, tag="wkv", xsrc=xs)
            emit_linear(cx, None, Wq, qT, SEQ, D_MODEL, NHL * 128, F32, F32, consts, gainT=gT, tag=f"wq{l}", xsrc=xs)
            emit_moba(cx, qT, kvT, qg, kgT, alaT, albT, btT, ogT, NHL, SEQ, consts, tag=f"moba{l}")
        for i in range(4):
            cx.coll(lambda e, i=i: e.collective_compute(
                "AllGather", ALU.bypass, replica_groups=PAIRS,
                ins=[ogT[256 * i:256 * (i + 1), :].opt()], outs=[ogall[512 * i:512 * (i + 1), :].opt()]), agB)
        cx.barrier_on(agB)
        Wo = I(f"Wo{l}", [16, 128, D_MODEL])
        g2 = I(f"gmlp{l}", [128, 16])
        W1 = I(f"W1_{l}", [64, 128, D_MODEL])
        W2 = I(f"W2_{l}", [16, 128, D_FF])
        h_next = hout if l == layers[-1] else hbuf[l % 2]
        emit_linear(cx, None, Wo, h1T, TOWN, D_MODEL, D_MODEL, BF16, F32, consts, resT=h_cur, tag=f"wo{l}",
                    xsrc=ogA, xsrcB=ogBf, selT=rsel)
        emit_linear(cx, h1T, W1, aT, TOWN, D_MODEL, D_FF, F32, BF16, consts, gainT=g2, act="relu2", tag=f"w1{l}")
        emit_linear(cx, aT, W2, h_next, TOWN, D_FF, D_MODEL, BF16, F32, consts, resT=h1T, tag=f"w2{l}")
    if info is not None:
        info['nsem'] = cx.nsem
        info['ecnt'] = dict(cx.ecnt)
        info['maxcnt'] = getattr(cx, 'maxcnt', 0)
        info['maxdma'] = max([v for (_, v) in cx.final] + [b.dval for b in cx.alldma])
    cx.finish()
    return nc


def _head_cols(r, nparts):
    cols = []
    for a in range(nparts):
        for j in range(NHL):
            h = NHL * r + j
            cols.append(np.arange(a * D_MODEL + h * 128, a * D_MODEL + (h + 1) * 128))
    return np.concatenate(cols)


def kernel(x, a_norm, a_w_in, a_head_norm, a_w_out, lower_bounds, kv_norm, w_kv, k_norm,
           b_norm, b_w_q, b_q_norm, b_w_o, mlp_norm, mlp_w1, mlp_w2):
    f32 = lambda a: np.asarray(a, dtype=np.float32)
    x = f32(x)
    shared = dict(const_inputs())
    shared["kgT"] = f32(k_norm)[:, None].copy()
    for l in range(4):
        shared[f"gmlp{l}"] = fm(f32(mlp_norm[l]))
        shared[f"W1_{l}"] = tile_w(f32(mlp_w1[l]))
        shared[f"W2_{l}"] = tile_w(f32(mlp_w2[l]))
        shared[f"Wo{l}"] = tile_w(f32(a_w_out[l] if l < 2 else b_w_o[l - 2]))
        shared[f"gmix{l}"] = fm(f32(a_norm[l] if l < 2 else b_norm[l - 2]))
        if l >= 2:
            shared[f"qg{l}"] = f32(b_q_norm[l - 2])[:, None].copy()
    shared["gkv"] = fm(f32(kv_norm))
    per_rank = []
    for r in range(2):
        d = {}
        hs = slice(NHL * r * 128, NHL * (r + 1) * 128)
        for l in range(2):
            d[f"Win{l}"] = tile_w(f32(a_w_in[l])[:, _head_cols(r, 4)])
            d[f"hg{l}"] = np.ascontiguousarray(f32(a_head_norm[l])[hs].reshape(NHL, 128).T)
        d["lbT"] = np.ascontiguousarray(f32(lower_bounds)[:, hs].reshape(2, NHL, 128).transpose(2, 0, 1))
        for l in (2, 3):
            d[f"Wq{l}"] = tile_w(f32(b_w_q[l - 2])[:, _head_cols(r, 1)])
        d["Wkv"] = tile_w(f32(w_kv)[:, _head_cols(r, 2)])
        ala, alb, bt = moba_tables(list(range(NHL * r, NHL * (r + 1))))
        d["alaT"], d["albT"], d["btT"] = ala, alb, bt
        rs = np.zeros((128, 2), np.float32)
        rs[:, r] = 1.0
        d["rsel"] = rs
        per_rank.append(d)
    maps = []
    for c in range(NCORES):
        b, r = divmod(c, 2)
        xb = x[b]
        hall0 = np.ascontiguousarray(xb.reshape(2, TOWN, D_MODEL).transpose(0, 2, 1)).reshape(2 * D_MODEL, TOWN)
        m = dict(shared)
        m.update(per_rank[r])
        m["hall0"] = hall0
        m["hT0"] = np.ascontiguousarray(hall0[r * D_MODEL:(r + 1) * D_MODEL])
        maps.append(m)
    nc = build_fused()
    res = run_bass_kernel_spmd(nc, maps, core_ids=list(range(NCORES))).results
    out = np.empty((BATCH, SEQ, D_MODEL), np.float32)
    for c in range(NCORES):
        b, r = divmod(c, 2)
        out[b, r * TOWN:(r + 1) * TOWN, :] = np.asarray(res[c]["hout"]).T
    return out
```

```python
from contextlib import ExitStack
import numpy as np
import concourse.bass as bass
import concourse.mybir as mybir

F32 = mybir.dt.float32
BF16 = mybir.dt.bfloat16
AF = mybir.ActivationFunctionType
ALU = mybir.AluOpType
AX = mybir.AxisListType

ENGS = ("pe", "act", "dve", "pool", "sp")


class Buf:
    __slots__ = ("name", "w", "r", "dsem", "dval")

    def __init__(self, name):
        self.name = name
        self.w = {}
        self.r = {}
        self.dsem = None
        self.dval = 0


class Scope:
    uid = 0

    def __init__(self, cx):
        self.cx = cx
        self.es = ExitStack()

    def __enter__(self):
        self.mark = len(self.cx.scope_bufs)
        return self

    def __exit__(self, *a):
        self.cx.barrier()
        for b in self.cx.scope_bufs[self.mark:]:
            self.cx.sempool.append((b.dsem, b.dval))
            self.cx.alldma.remove(b)
            self.cx.final.append((b.dsem, b.dval))
        del self.cx.scope_bufs[self.mark:]
        self.es.close()
        return False

    def sb(self, name, shape, dt):
        Scope.uid += 1
        return self.es.enter_context(self.cx.nc.sbuf_tensor(f"{name}_{Scope.uid}", list(shape), dt))

    def ps(self, name, shape, dt):
        Scope.uid += 1
        return self.es.enter_context(self.cx.nc.psum_tensor(f"{name}_{Scope.uid}", list(shape), dt))


class Ctx:
    def __init__(self, nc):
        self.nc = nc
        self.es = ExitStack()
        self.eng = {"pe": nc.tensor, "act": nc.scalar, "dve": nc.vector, "pool": nc.gpsimd, "sp": nc.sync}
        self.esem = {}
        self.ecnt = {e: 0 for e in ENGS}
        for e in ENGS:
            self.esem[e] = self.es.enter_context(nc.semaphore("es_" + e))
        self.known = {e: {} for e in ENGS}
        self.nsem = 0
        self.final = []
        self.alldma = []
        self.sempool = []
        self.scope_bufs = []

    def scope(self):
        return Scope(self)

    def barrier(self):
        deps = {}
        for e in ENGS:
            if self.ecnt[e] > 0:
                deps[id(self.esem[e])] = (self.esem[e], self.ecnt[e])
        for b in self.alldma:
            if b.dval > 0:
                deps[id(b.dsem)] = (b.dsem, b.dval)
        for e in ENGS:
            self._emit_waits(e, deps)

    def newsem(self, name):
        self.nsem += 1
        return self.es.enter_context(self.nc.semaphore(name))

    def dmabuf(self, name):
        b = Buf(name)
        if self.sempool:
            b.dsem, b.dval = self.sempool.pop()
        else:
            b.dsem = self.newsem("d_%d" % self.nsem)
        self.alldma.append(b)
        self.scope_bufs.append(b)
        return b

    def _collect(self, reads, writes):
        deps = {}
        for b in reads:
            for k, (s, v) in b.w.items():
                if k not in deps or deps[k][1] < v:
                    deps[k] = (s, v)
        for b in writes:
            for d in (b.w, b.r):
                for k, (s, v) in d.items():
                    if k not in deps or deps[k][1] < v:
                        deps[k] = (s, v)
        return deps

    def _emit_waits(self, e, deps, skip_self=False):
        kn = self.known[e]
        for k, (s, v) in deps.items():
            if skip_self and k == id(self.esem[e]):
                continue
            if kn.get(k, 0) >= v:
                continue
            kn[k] = v
            self.eng[e].wait_ge(s, v)

    def op(self, e, fn, reads=(), writes=()):
        deps = self._collect(reads, writes)
        self._emit_waits(e, deps, skip_self=(e == "pe"))
        self.ecnt[e] += 1
        tok = (self.esem[e], self.ecnt[e])
        k = id(self.esem[e])
        fn(self.eng[e]).then_inc(self.esem[e], 1)
        for b in reads:
            b.r[k] = tok
        for b in writes:
            b.w = {k: tok}
            b.r = {}

    def dma(self, q, fn, owner, reads=(), writes=(), serial=True):
        deps = self._collect(reads, writes)
        if owner.dval > 0 and serial:
            deps[id(owner.dsem)] = (owner.dsem, max(owner.dval, deps.get(id(owner.dsem), (None, 0))[1]))
        self._emit_waits(q, deps)
        owner.dval += 16
        tok = (owner.dsem, owner.dval)
        k = id(owner.dsem)
        fn(self.eng[q]).then_inc(owner.dsem, 16)
        for b in reads:
            b.r[k] = tok
        for b in writes:
            b.w = {k: tok}
            b.r = {}
        return tok

    def renew_engine_sems(self):
        self.barrier()
        for e in ENGS:
            self.maxcnt = max(getattr(self, "maxcnt", 0), self.ecnt[e])
            if self.ecnt[e] > 0:
                self.esem[e] = self.es.enter_context(self.nc.semaphore("es%d_%s" % (self.nsem, e)))
                self.nsem += 1
                self.ecnt[e] = 0

    def barrier_on(self, owner):
        deps = {id(owner.dsem): (owner.dsem, owner.dval)}
        for e in ENGS:
            self._emit_waits(e, deps)

    def coll(self, fn, owner):
        deps = {}
        if owner.dval > 0:
            deps[id(owner.dsem)] = (owner.dsem, owner.dval)
        self._emit_waits("pool", deps)
        owner.dval += 1
        fn(self.eng["pool"]).then_inc(owner.dsem)

    def dma_multi(self, q, fn, owner, reads=(), writes_acc=()):
        deps = self._collect(reads, ())
        for b in writes_acc:
            for kk, (s, v) in b.r.items():
                if kk not in deps or deps[kk][1] < v:
                    deps[kk] = (s, v)
        if owner.dval > 0:
            deps[id(owner.dsem)] = (owner.dsem, max(owner.dval, deps.get(id(owner.dsem), (None, 0))[1]))
        self._emit_waits(q, deps)
        owner.dval += 16
        tok = (owner.dsem, owner.dval)
        k = id(owner.dsem)
        fn(self.eng[q]).then_inc(owner.dsem, 16)
        for b in reads:
            b.r[k] = tok
        for b in writes_acc:
            b.w[k] = tok
        return tok

    def finish(self):
        deps = {}
        for e in ENGS:
            if self.ecnt[e] > 0:
                deps[id(self.esem[e])] = (self.esem[e], self.ecnt[e])
        for b in self.alldma:
            if b.dval > 0:
                deps[id(b.dsem)] = (b.dsem, b.dval)
        for (s_, v) in self.final:
            if id(s_) not in deps or deps[id(s_)][1] < v:
                deps[id(s_)] = (s_, v)
        for k, (s, v) in deps.items():
            self.eng["sp"].wait_ge(s, v)
        self.es.close()


EPS = 1e-6


def _load_pieces(cx, pieces, dst_of, owner, buf):
    first = True
    for (cs, ce, sap) in pieces:
        cx.dma("sp", lambda e, dst=dst_of(cs, ce), sap=sap: e.dma_start(out=dst, in_=sap), owner,
               writes=[buf] if first else [], serial=first)
        first = False
    buf.w = {id(owner.dsem): (owner.dsem, owner.dval)}
    buf.r = {}


def emit_linear(cx, xT, Wt, outT, T, Fin, Fout, x_dt, out_dt, consts, gainT=None, resT=None, act=None,
                TBLK=None, tag="lin", xsrc=None, xsrcB=None, selT=None, NW=3):
    nc = cx.nc
    KC = Fin // 128
    MC = Fout // 128
    if TBLK is None:
        TBLK = min(T, 1024 if KC <= 16 else 512)
    NB = TBLK // 512
    G = max(1, 64 // KC)
    assert MC % G == 0
    if xsrc is None:
        xTv = xT.rearrange("(c p) t -> p c t", p=128)
        xsrc = lambda c0, c1, t0, n: [(c0, c1, xTv[:, c0:c1, t0:t0 + n])]
    with cx.scope() as sc:
        xn = sc.sb("xn", [128, KC, TBLK], BF16)
        xnB = Buf("xn")
        if xsrcB is not None:
            xtm = sc.sb("xtm", [128, KC, TBLK], BF16)
            xtmB = Buf("xtm")
            xtmD = cx.dmabuf(f"{tag}_xtm")
            sel = sc.sb("sel", [128, 2], F32)
            selB = cx.dmabuf(f"{tag}_sel")
            cx.dma("sp", lambda e: e.dma_start(out=sel[:], in_=selT), selB, writes=[selB])
        wb = [sc.sb(f"w{i}", [128, G, KC * 128], BF16) for i in range(NW)]
        wB = [cx.dmabuf(f"{tag}_w{i}") for i in range(NW)]
        NPS = 4
        pss = [sc.ps(f"ps{i}", [128, 512], F32) for i in range(NPS)]
        psB = [Buf(f"ps{i}") for i in range(NPS)]
        NO = 3
        ost = [sc.sb(f"o{i}", [128, 512], out_dt) for i in range(NO)]
        ostB = [cx.dmabuf(f"{tag}_o{i}") for i in range(NO)]
        if act == "relu2":
            rst = [sc.sb(f"r{i}", [128, 512], F32) for i in range(2)]
            rstB = [Buf(f"r{i}") for i in range(2)]
        if resT is not None:
            rsd = [sc.sb(f"rs{i}", [128, 512], F32) for i in range(3)]
            rsdB = [cx.dmabuf(f"{tag}_rs{i}") for i in range(3)]
        if gainT is not None:
            xs = sc.sb("xs", [128, KC, 512], F32)
            xsB = cx.dmabuf(f"{tag}_xs")
            sqb = sc.sb("sqb", [128, KC, 512], BF16)
            sqB = Buf("sqb")
            gsb = sc.sb("gsb", [128, KC], F32)
            gB = cx.dmabuf(f"{tag}_g")
            cx.dma("sp", lambda e: e.dma_start(out=gsb[:], in_=gainT), gB, writes=[gB])
            pssum = sc.ps("pssum", [128, 512], F32)
            pssB = Buf("pssum")
            tmp = sc.sb("tmp", [128, 512], F32)
            tmpB = Buf("tmp")
            rstd = sc.sb("rstd", [128, 512], F32)
            rstdB = Buf("rstd")
        else:
            xnD = cx.dmabuf(f"{tag}_xn")
        ones_bf, onesB = consts["ones_bf"]
        eps_sb, epsB = consts["eps"]
        cnt = 0
        wcnt = 0
        for tb in range(T // TBLK):
            t0 = tb * TBLK
            if gainT is None:
                for c0 in range(0, KC, 16):
                    _load_pieces(cx, xsrc(c0, c0 + 16, t0, TBLK), lambda cs, ce: xn[:, cs:ce, :], xnD, xnB)
                    if xsrcB is not None:
                        _load_pieces(cx, xsrcB(c0, c0 + 16, t0, TBLK), lambda cs, ce: xtm[:, cs:ce, :], xtmD, xtmB)
                        cx.op("dve", lambda e, c0=c0: e.tensor_scalar(out=xn[:, c0:c0 + 16, :], in0=xn[:, c0:c0 + 16, :],
                                                                      scalar1=sel[:, 0:1], scalar2=None, op0=ALU.mult),
                              reads=[xnB, selB], writes=[xnB])
                        cx.op("dve", lambda e, c0=c0: e.scalar_tensor_tensor(out=xn[:, c0:c0 + 16, :], in0=xtm[:, c0:c0 + 16, :],
                                                                             scalar=sel[:, 1:2], in1=xn[:, c0:c0 + 16, :],
                                                                             op0=ALU.mult, op1=ALU.add),
                              reads=[xtmB, xnB, selB], writes=[xnB])
            else:
                for n in range(NB):
                    ts = t0 + n * 512
                    _load_pieces(cx, xsrc(0, KC, ts, 512), lambda cs, ce: xs[:, cs:ce, :], xsB, xsB)
                    for c in range(KC):
                        cx.op("act", lambda e, c=c: e.activation(out=sqb[:, c, :], in_=xs[:, c, :], func=AF.Square),
                              reads=[xsB], writes=[sqB] if c == 0 else [])
                        if c > 0:
                            sqB.w = {id(cx.esem["act"]): (cx.esem["act"], cx.ecnt["act"])}
                    for c in range(KC):
                        cx.op("pe", lambda e, c=c: e.matmul(pssum[:], lhsT=ones_bf[:], rhs=sqb[:, c, :],
                                                            start=(c == 0), stop=(c == KC - 1)),
                              reads=[sqB, onesB], writes=[pssB])
                    cx.op("act", lambda e: e.activation(out=tmp[:], in_=pssum[:], func=AF.Ln, scale=1.0 / Fin, bias=eps_sb[:]),
                          reads=[pssB, epsB], writes=[tmpB])
                    cx.op("act", lambda e: e.activation(out=rstd[:], in_=tmp[:], func=AF.Exp, scale=-0.5),
                          reads=[tmpB], writes=[rstdB])
                    for c in range(KC):
                        cx.op("dve", lambda e, c=c, n=n: e.scalar_tensor_tensor(
                            out=xn[:, c, n * 512:(n + 1) * 512], in0=xs[:, c, :], scalar=gsb[:, c:c + 1], in1=rstd[:],
                            op0=ALU.mult, op1=ALU.mult),
                            reads=[xsB, gB, rstdB], writes=[xnB] if (c == 0 and n == 0) else [])
                        xnB.w = {id(cx.esem["dve"]): (cx.esem["dve"], cx.ecnt["dve"])}
            NMG = MC // G

            def issue_w(mg):
                wi = (wbase + mg) % NW
                cx.dma("pool", lambda e, wi=wi, mg=mg: e.dma_start(
                    out=wb[wi][:], in_=Wt[mg * G:(mg + 1) * G].rearrange("g p f -> p g f"), max_dma_last_dim=8192),
                    wB[wi], writes=[wB[wi]])

            wbase = wcnt
            wcnt += NMG
            iters = [(m, n) for m in range(MC) for n in range(NB)]

            def issue_res(i):
                m, n = iters[i]
                ri = i % 3
                ts = t0 + n * 512
                cx.dma("sp", lambda e, ri=ri, m=m, ts=ts: e.dma_start(
                    out=rsd[ri][:], in_=resT[m * 128:(m + 1) * 128, ts:ts + 512]), rsdB[ri], writes=[rsdB[ri]])

            for mg in range(min(NW - 1, NMG)):
                issue_w(mg)
            if resT is not None:
                for i in range(min(2, len(iters))):
                    issue_res(i)
            it = 0
            for mg in range(NMG):
                if mg + NW - 1 < NMG:
                    issue_w(mg + NW - 1)
                wi = (wbase + mg) % NW
                for g in range(G):
                    m = mg * G + g
                    for n in range(NB):
                        pi = cnt % NPS
                        oi = cnt % NO
                        cnt += 1
                        for c in range(KC):
                            cx.op("pe", lambda e, pi=pi, wi=wi, g=g, c=c, n=n: e.matmul(
                                pss[pi][:], lhsT=wb[wi][:, g, c * 128:(c + 1) * 128], rhs=xn[:, c, n * 512:(n + 1) * 512],
                                start=(c == 0), stop=(c == KC - 1)),
                                reads=[wB[wi], xnB], writes=[psB[pi]])
                        ts = t0 + n * 512
                        dst = outT[m * 128:(m + 1) * 128, ts:ts + 512]
                        if act == "relu2":
                            ri = cnt % 2
                            cx.op("act", lambda e, pi=pi, ri=ri: e.activation(out=rst[ri][:], in_=pss[pi][:], func=AF.Relu),
                                  reads=[psB[pi]], writes=[rstB[ri]])
                            cx.op("dve", lambda e, ri=ri, oi=oi: e.tensor_tensor(out=ost[oi][:], in0=rst[ri][:], in1=rst[ri][:],
                                                                                 op=ALU.mult),
                                  reads=[rstB[ri]], writes=[ostB[oi]])
                        elif resT is not None:
                            ri = it % 3
                            if it + 2 < len(iters):
                                issue_res(it + 2)
                            cx.op("dve", lambda e, pi=pi, ri=ri, oi=oi: e.tensor_tensor(
                                out=ost[oi][:], in0=pss[pi][:], in1=rsd[ri][:], op=ALU.add),
                                reads=[psB[pi], rsdB[ri]], writes=[ostB[oi]])
                        else:
                            if cnt % 2 == 0:
                                cx.op("act", lambda e, pi=pi, oi=oi: e.activation(out=ost[oi][:], in_=pss[pi][:], func=AF.Copy),
                                      reads=[psB[pi]], writes=[ostB[oi]])
                            else:
                                cx.op("dve", lambda e, pi=pi, oi=oi: e.tensor_copy(out=ost[oi][:], in_=pss[pi][:]),
                                      reads=[psB[pi]], writes=[ostB[oi]])
                        cx.dma("sp", lambda e, oi=oi, dst=dst: e.dma_start(out=dst, in_=ost[oi][:]), ostB[oi], reads=[ostB[oi]])
                        it += 1


import ml_dtypes

NPBF = ml_dtypes.bfloat16
BIGNEG = -30000.0
ATT_SCALE = 128 ** -0.5


def tile_w(W):
    Fin, Fout = W.shape
    KC, MC = Fin // 128, Fout // 128
    return np.ascontiguousarray(W.reshape(KC, 128, MC, 128).transpose(2, 1, 0, 3)).reshape(MC, 128, KC * 128)


def fm(v):
    return np.ascontiguousarray(v.reshape(-1, 128).T)


def _const_tables():
    c = {}
    c["c_ones_bf"] = np.ones((128, 128), NPBF)
    c["c_ident_bf"] = np.eye(128, dtype=np.float32).astype(NPBF)
    c["c_eps"] = np.full((128, 1), EPS, np.float32)
    seg = np.ones((128, 512), np.float32)
    seg[:, ::32] = 0.0
    c["c_segmask"] = seg
    s = np.arange(128)[:, None]
    t = np.arange(128)[None, :]
    c["c_gmaskT"] = ((s <= t) & (s // 32 == t // 32)).astype(np.float32)
    pm = np.zeros((128, 16, 8), np.float32)
    for tile in range(16):
        qb = tile // 2
        pm[:, tile, qb:] = -1e30
    c["c_pastmask"] = pm
    E = np.zeros((8, 8, 128), np.float32)
    for n in range(8):
        E[n, n, :] = 1.0
    c["c_E"] = E.astype(NPBF)
    s = np.arange(128)[:, None]
    t = np.arange(256)[None, :]
    c["c_CM0"] = np.where(s > t, BIGNEG, 0.0).astype(np.float32).astype(NPBF)
    return c


CONST_DT = {"c_ones_bf": BF16, "c_ident_bf": BF16, "c_eps": F32, "c_segmask": F32, "c_gmaskT": F32,
            "c_pastmask": F32, "c_E": BF16, "c_CM0": BF16}


def const_inputs():
    return _const_tables()


def setup_consts(cx, names=None):
    nc = cx.nc
    tabs = _const_tables()
    out = {}
    ld = cx.dmabuf("constld")
    for k, arr in tabs.items():
        d = nc.dram_tensor(k, list(arr.shape), CONST_DT[k], kind="ExternalInput").ap()
        if names is not None and k not in names:
            continue
        t = cx.es.enter_context(nc.sbuf_tensor("sb_" + k, list(arr.shape), CONST_DT[k]))
        b = Buf(k)
        cx.dma("sp", lambda e, t=t, d=d: e.dma_start(out=t[:], in_=d), ld, writes=[b])
        out[k[2:]] = (t, b)
    return out


def emit_gla(cx, projT, lbT, hgT, ogT, NH, S, layer, consts, tag="gla"):
    nc = cx.nc
    NBLK = S // 512
    pv = projT.rearrange("(a j p) t -> p a j t", a=4, j=NH, p=128)
    ones_bf, onesB = consts["ones_bf"]
    ident, identB = consts["ident_bf"]
    eps_sb, epsB = consts["eps"]
    segm, segB = consts["segmask"]
    gmask, gmB = consts["gmaskT"]
    with cx.scope() as sc:
        def T2(name, dt=F32, shape=(128, 512)):
            return [sc.sb(f"{name}{i}", list(shape), dt) for i in range(2)], [Buf(f"{name}{i}") for i in range(2)]
        inb = [sc.sb(f"in{i}", [128, 4, 512], F32) for i in range(2)]
        inB = [cx.dmabuf(f"{tag}_in{i}") for i in range(2)]
        qs, qsB = T2("qs")
        gate, gateB = T2("gate")
        ee, eeB = T2("ee")
        rr, rrB = T2("rr")
        ff, ffB = T2("ff")
        kk, kkB = T2("kk")
        lf, lfB = T2("lf")
        bc, bcB = T2("bc")
        eb, ebB = T2("eb")
        einv, einvB = T2("einv")
        qtb, qtbB = T2("qtb", BF16)
        ktb, ktbB = T2("ktb", BF16)
        khb, khbB = T2("khb", BF16)
        ib, ibB = T2("ib", BF16)
        vsb, vsbB = T2("vsb", BF16, (128, 4, 128))
        v32, v32B = T2("v32", BF16, (32, 16, 128))
        kh32, kh32B = T2("kh32", BF16, (32, 16, 128))
        amb, ambB = T2("amb", BF16, (128, 128))
        sbf = [[sc.sb(f"sbf{c_}{i}", [128, 128], BF16) for i in range(2)] for c_ in range(2)]
        sbfB = [[Buf(f"sbf{c_}{i}") for i in range(2)] for c_ in range(2)]
        osb, osbB = T2("osb")
        osq, osqB = T2("osq", BF16)
        tmp, tmpB = T2("tmp")
        rstd, rstdB = T2("rstd")
        ogb = [sc.sb(f"ogb{i}", [128, 512], BF16) for i in range(2)]
        ogB = [cx.dmabuf(f"{tag}_og{i}") for i in range(2)]
        s32 = [sc.sb(f"s32_{i}", [128, 128], F32) for i in range(2)]
        s32B = [Buf(f"s32_{i}") for i in range(2)]
        lbr = sc.sb("lbr", [128, 2, NH], F32)
        lbB = cx.dmabuf(f"{tag}_lb")
        hg = sc.sb("hg", [128, NH], F32)
        hgB = cx.dmabuf(f"{tag}_hg")
        lb = sc.sb("lb", [128, NH], F32)
        oml = sc.sb("oml", [128, NH], F32)
        lbcB = Buf("lbc")
        psTr = [sc.ps(f"psTr{i}", [128, 1024], BF16) for i in range(2)]
        psTrB = [Buf(f"psTr{i}") for i in range(2)]
        psA = [sc.ps(f"psA{i}", [128, 512], F32) for i in range(1)]
        psAB = [Buf(f"psA{i}") for i in range(1)]
        psO = [sc.ps(f"psO{i}", [128, 512], F32) for i in range(2)]
        psOB = [Buf(f"psO{i}") for i in range(2)]
        psS = [sc.ps(f"psS{i}", [128, 512], F32) for i in range(2)]
        psSB = [Buf(f"psS{i}") for i in range(2)]
        psN = sc.ps("psN", [128, 512], F32)
        psNB = Buf("psN")

        cx.dma("sp", lambda e: e.dma_start(out=lbr[:], in_=lbT), lbB, writes=[lbB])
        cx.dma("sp", lambda e: e.dma_start(out=hg[:], in_=hgT), hgB, writes=[hgB])
        if layer == 0:
            cx.op("dve", lambda e: e.memset(lb[:], 0.0), writes=[lbcB])
            cx.op("dve", lambda e: e.memset(oml[:], 1.0), writes=[])
        else:
            cx.op("dve", lambda e: e.tensor_tensor(out=lb[:], in0=lbr[:, 0, :], in1=lbr[:, 1, :], op=ALU.subtract),
                  reads=[lbB], writes=[lbcB])
            cx.op("act", lambda e: e.activation(out=lb[:], in_=lb[:], func=AF.Exp), reads=[lbcB], writes=[lbcB])
            cx.op("dve", lambda e: e.tensor_scalar(out=lb[:], in0=lb[:], scalar1=1.0, scalar2=None, op0=ALU.add),
                  reads=[lbcB], writes=[lbcB])
            cx.op("dve", lambda e: e.reciprocal(out=lb[:], in_=lb[:]), reads=[lbcB], writes=[lbcB])
            cx.op("dve", lambda e: e.tensor_scalar(out=oml[:], in0=lb[:], scalar1=-1.0, scalar2=1.0, op0=ALU.mult, op1=ALU.add),
                  reads=[lbcB], writes=[])
        lbcB.w = {id(cx.esem["dve"]): (cx.esem["dve"], cx.ecnt["dve"])}

        NCH = 2 if NH % 2 == 0 else 1
        trc_ = [0]
        sci = [0, 0]

        def issue_in(j, tb, b):
            cx.dma("sp", lambda e, b=b, j=j, tb=tb: e.dma_start(out=inb[b][:], in_=pv[:, :, j, tb * 512:(tb + 1) * 512]),
                   inB[b], writes=[inB[b]])

        def block_gen(j, tb, b, nxt):
            po = b
            X = inb[b]
            if tb == 0:
                cx.op("dve", lambda e: e.memset(s32[b][:], 0.0), writes=[s32B[b]])
                cx.op("dve", lambda e, k=sci[b] % 2: e.memset(sbf[b][k][:], 0.0), writes=[sbfB[b][sci[b] % 2]])
            t0 = tb * 512
            cx.op("act", lambda e: e.activation(out=qs[b][:], in_=X[:, 0, :], func=AF.Silu), reads=[inB[b]], writes=[qsB[b]])
            cx.op("act", lambda e: e.activation(out=gate[b][:], in_=X[:, 3, :], func=AF.Silu), reads=[inB[b]], writes=[gateB[b]])
            cx.op("act", lambda e: e.activation(out=ee[b][:], in_=X[:, 1, :], func=AF.Exp, scale=-1.0), reads=[inB[b]], writes=[eeB[b]])
            cx.op("act", lambda e: e.activation(out=ib[b][:], in_=X[:, 2, :], func=AF.Copy), reads=[inB[b]], writes=[ibB[b]])
            cx.op("dve", lambda e: e.tensor_scalar(out=ee[b][:], in0=ee[b][:], scalar1=1.0, scalar2=None, op0=ALU.add),
                  reads=[eeB[b]], writes=[eeB[b]])
            cx.op("dve", lambda e: e.reciprocal(out=rr[b][:], in_=ee[b][:]), reads=[eeB[b]], writes=[rrB[b]])
            cx.op("dve", lambda e: e.tensor_scalar(out=ff[b][:], in0=rr[b][:], scalar1=oml[:, j:j + 1], scalar2=lb[:, j:j + 1],
                                                   op0=ALU.mult, op1=ALU.add), reads=[rrB[b], lbcB], writes=[ffB[b]])
            cx.op("pool", lambda e: e.tensor_scalar(out=kk[b][:], in0=ff[b][:], scalar1=-1.0, scalar2=1.0, op0=ALU.mult, op1=ALU.add),
                  reads=[ffB[b]], writes=[kkB[b]])
            cx.op("act", lambda e: e.activation(out=lf[b][:], in_=ff[b][:], func=AF.Ln), reads=[ffB[b]], writes=[lfB[b]])
            cx.op("dve", lambda e: e.tensor_tensor_scan(out=bc[b][:], data0=segm[:], data1=lf[b][:], initial=0.0,
                                                        op0=ALU.mult, op1=ALU.add), reads=[lfB[b], segB], writes=[bcB[b]])
            cx.op("act", lambda e: e.activation(out=eb[b][:], in_=bc[b][:], func=AF.Exp), reads=[bcB[b]], writes=[ebB[b]])
            cx.op("dve", lambda e: e.tensor_tensor(out=qtb[b][:], in0=qs[b][:], in1=eb[b][:], op=ALU.mult),
                  reads=[qsB[b], ebB[b]], writes=[qtbB[b]])
            cx.op("dve", lambda e: e.reciprocal(out=einv[b][:], in_=eb[b][:]), reads=[ebB[b]], writes=[einvB[b]])
            cx.op("dve", lambda e: e.tensor_tensor(out=ktb[b][:], in0=kk[b][:], in1=einv[b][:], op=ALU.mult),
                  reads=[kkB[b], einvB[b]], writes=[ktbB[b]])
            for c in range(16):
                cx.op("pool", lambda e, c=c: e.tensor_scalar(out=khb[b][:, 32 * c:32 * c + 32], in0=ktb[b][:, 32 * c:32 * c + 32],
                                                             scalar1=eb[b][:, 32 * c + 31:32 * c + 32], scalar2=None, op0=ALU.mult),
                      reads=[ktbB[b], ebB[b]], writes=[khbB[b]] if c == 0 else [])
            khbB[b].w = {id(cx.esem["pool"]): (cx.esem["pool"], cx.ecnt["pool"])}
            tr = trc_[0] % 2
            trc_[0] += 1
            for i in range(4):
                cx.op("pe", lambda e, tr=tr, i=i: e.transpose(out=psTr[tr][:, i * 128:(i + 1) * 128],
                                                              in_=ib[b][:, i * 128:(i + 1) * 128], identity=ident[:]),
                      reads=[ibB[b], identB], writes=[psTrB[tr]])
            cx.op("dve", lambda e, tr=tr: e.tensor_copy(out=vsb[b][:].rearrange("p a b -> p (a b)"), in_=psTr[tr][:, 0:512]),
                  reads=[psTrB[tr]], writes=[vsbB[b]])
            for (src_, srcB, dst, dstB) in ((ib[b], ibB[b], v32[b], v32B[b]), (khb[b], khbB[b], kh32[b], kh32B[b])):
                for h in range(2):
                    tr = trc_[0] % 2
                    trc_[0] += 1
                    for i in range(8):
                        cc = h * 8 + i
                        cx.op("pe", lambda e, tr=tr, i=i, cc=cc, src_=src_: e.transpose(
                            out=psTr[tr][0:32, i * 128:(i + 1) * 128], in_=src_[:, cc * 32:(cc + 1) * 32], identity=ident[:]),
                            reads=[srcB, identB], writes=[psTrB[tr]])
                    cx.op("act" if h == 0 else "dve", lambda e, tr=tr, dst=dst, h=h: (e.activation(
                        out=dst[:, h * 8:(h + 1) * 8, :].rearrange("p a b -> p (a b)"), in_=psTr[tr][0:32, :], func=AF.Copy) if h == 0 else
                        e.tensor_copy(out=dst[:, h * 8:(h + 1) * 8, :].rearrange("p a b -> p (a b)"), in_=psTr[tr][0:32, :])),
                        reads=[psTrB[tr]], writes=[dstB] if h == 0 else [])
                    if h == 1:
                        dstB.w[id(cx.esem["dve"])] = (cx.esem["dve"], cx.ecnt["dve"])
            if nxt is not None:
                issue_in(nxt[0], nxt[1], b)
            yield
            for i in range(4):
                a = 0
                cs = slice(i * 128, (i + 1) * 128)
                cx.op("pe", lambda e, a=a, cs=cs: e.matmul(psA[a][:, 0:128], lhsT=ktb[b][:, cs], rhs=qtb[b][:, cs], start=True, stop=True),
                      reads=[ktbB[b], qtbB[b]], writes=[psAB[a]])
                cx.op("dve", lambda e, a=a: e.tensor_tensor(out=amb[a][:], in0=psA[a][:, 0:128], in1=gmask[:], op=ALU.mult),
                      reads=[psAB[a], gmB], writes=[ambB[a]])
                cx.op("pe", lambda e, a=a, i=i, cs=cs: e.matmul(psO[po][:, cs], lhsT=vsb[b][:, i, :], rhs=amb[a][:], start=True, stop=False),
                      reads=[vsbB[b], ambB[a]], writes=[psOB[po]])
                for c in range(4):
                    k = sci[b] % 2
                    c0 = i * 128 + 32 * c
                    cx.op("pe", lambda e, k=k, c0=c0: e.matmul(psO[po][:, c0:c0 + 32], lhsT=sbf[b][k][:], rhs=qtb[b][:, c0:c0 + 32],
                                                               start=False, stop=(c0 % 128 == 96)),
                          reads=[sbfB[b][k], qtbB[b]], writes=[psOB[po]])
                    cx.op("pe", lambda e, i=i, c=c: e.matmul(psS[b][:, 0:128], lhsT=kh32[b][:, 4 * i + c, :],
                                                             rhs=v32[b][:, 4 * i + c, :], start=True, stop=True),
                          reads=[kh32B[b], v32B[b]], writes=[psSB[b]])
                    sci[b] += 1
                    k2 = sci[b] % 2
                    cx.op("dve", lambda e, c0=c0, k2=k2: e.scalar_tensor_tensor(out=sbf[b][k2][:], in0=s32[b][:], scalar=eb[b][:, c0 + 31:c0 + 32],
                                                                               in1=psS[b][:, 0:128], op0=ALU.mult, op1=ALU.add),
                          reads=[s32B[b], ebB[b], psSB[b]], writes=[sbfB[b][k2]])
                    cx.op("dve", lambda e, c0=c0: e.scalar_tensor_tensor(out=s32[b][:], in0=s32[b][:], scalar=eb[b][:, c0 + 31:c0 + 32],
                                                                         in1=psS[b][:, 0:128], op0=ALU.mult, op1=ALU.add),
                          reads=[s32B[b], ebB[b], psSB[b]], writes=[s32B[b]])
                    yield
            cx.op("act", lambda e: e.activation(out=osb[b][:], in_=psO[po][:], func=AF.Copy), reads=[psOB[po]], writes=[osbB[b]])
            cx.op("act", lambda e: e.activation(out=osq[b][:], in_=psO[po][:], func=AF.Square), reads=[psOB[po]], writes=[osqB[b]])
            cx.op("pe", lambda e: e.matmul(psN[:], lhsT=ones_bf[:], rhs=osq[b][:], start=True, stop=True),
                  reads=[onesB, osqB[b]], writes=[psNB])
            cx.op("act", lambda e: e.activation(out=tmp[b][:], in_=psN[:], func=AF.Ln, scale=1.0 / 128, bias=eps_sb[:]),
                  reads=[psNB, epsB], writes=[tmpB[b]])
            cx.op("act", lambda e: e.activation(out=rstd[b][:], in_=tmp[b][:], func=AF.Exp, scale=-0.5), reads=[tmpB[b]], writes=[rstdB[b]])
            cx.op("dve", lambda e: e.tensor_tensor(out=osb[b][:], in0=osb[b][:], in1=rstd[b][:], op=ALU.mult),
                  reads=[osbB[b], rstdB[b]], writes=[osbB[b]])
            cx.op("dve", lambda e: e.scalar_tensor_tensor(out=ogb[b][:], in0=osb[b][:], scalar=hg[:, j:j + 1], in1=gate[b][:],
                                                          op0=ALU.mult, op1=ALU.mult),
                  reads=[osbB[b], hgB, gateB[b]], writes=[ogB[b]])
            cx.dma("sp", lambda e, j=j, t0=t0: e.dma_start(out=ogT[j * 128:(j + 1) * 128, t0:t0 + 512], in_=ogb[b][:]),
                   ogB[b], reads=[ogB[b]])


        def chain_gen(ch):
            hs = [j for j in range(NH) if j % NCH == ch]
            blks = [(j, tb) for j in hs for tb in range(NBLK)]
            issue_in(blks[0][0], blks[0][1], ch)
            for bi, (j, tb) in enumerate(blks):
                nxt = blks[bi + 1] if bi + 1 < len(blks) else None
                yield from block_gen(j, tb, ch, nxt)

        gens = [chain_gen(ch) for ch in range(NCH)]
        while gens:
            for g in list(gens):
                try:
                    next(g)
                except StopIteration:
                    gens.remove(g)


def moba_tables(heads):
    NH = len(heads)
    H = 16
    slopes = np.array([2.0 ** (-8.0 * (h + 1) / H) for h in heads], np.float64)
    ALA = np.zeros((2, NH, 128), np.float32)
    ALB = np.zeros((2, NH, 256), np.float32)
    bt = np.zeros((128, NH, 16), np.float32)
    for j in range(NH):
        ALA[0, j, :] = 1.0
        ALA[1, j, :] = slopes[j] / ATT_SCALE * np.arange(128)
        ALB[0, j, :] = -slopes[j] / ATT_SCALE * np.arange(256)
        ALB[1, j, :] = 1.0
        for d in range(16):
            bt[:, j, d] = -slopes[j] * 128.0 * (d - 1)
    return ALA, ALB, bt


def emit_moba(cx, qT, kvT, qgT, kgT, alaT, albT, btT, ogT, NH, S, consts, tag="moba"):
    nc = cx.nc
    NT = S // 128
    NQB = S // 256
    assert NQB == 8
    ones_bf, onesB = consts["ones_bf"]
    ident, identB = consts["ident_bf"]
    eps_sb, epsB = consts["eps"]
    pastm, pastB = consts["pastmask"]
    E_sb, EB = consts["E"]
    CM0, CMB = consts["CM0"]
    with cx.scope() as sc:
        raw = [sc.sb(f"raw{i}", [128, S], F32) for i in range(2)]
        rawB = [cx.dmabuf(f"{tag}_raw{i}") for i in range(2)]
        sq = sc.sb("sq", [128, S], BF16); sqB = Buf("sq")
        tmp = sc.sb("tmp", [128, 512], F32); tmpB = Buf("tmp")
        rstd = sc.sb("rstd", [128, S], F32); rstdB = Buf("rstd")
        kn32 = sc.sb("kn32", [128, S], F32); kn32B = Buf("kn32")
        knb = sc.sb("knb", [128, S], BF16); knbB = Buf("knb")
        qnb = sc.sb("qnb", [128, S], BF16); qnbB = Buf("qnb")
        vb = sc.sb("vb", [128, S], BF16); vbB = Buf("vb")
        vsb = sc.sb("vsb", [128, NT, 128], BF16); vsbB = Buf("vsb")
        kms = sc.sb("kms", [128, 8], F32); kmsB = Buf("kms")
        kmb = sc.sb("kmb", [128, 8], BF16); kmbB = Buf("kmb")
        gm = sc.sb("gm", [128, NT, 8], F32); gmB = Buf("gm")
        top8 = sc.sb("top8", [128, NT, 8], F32); top8B = Buf("top8")
        bb = sc.sb("bb", [128, NT, 8], BF16); bbB = Buf("bb")
        bbT = sc.sb("bbT", [8, S], BF16); bbTB = Buf("bbT")
        NSB = 3
        pT = [sc.sb(f"pT{i}", [128, 256], BF16) for i in range(NSB)]
        pTB = [Buf(f"pT{i}") for i in range(NSB)]
        rec = sc.sb("rec", [128, 256], F32); recB = Buf("rec")
        ogb = [sc.sb(f"ogb{i}", [128, 256], BF16) for i in range(2)]
        ogB = [cx.dmabuf(f"{tag}_og{i}") for i in range(2)]
        qg = sc.sb("qg", [128, 1], F32); kg = sc.sb("kg", [128, 1], F32)
        ala = sc.sb("ala", [2, NH, 128], F32); alb = sc.sb("alb", [2, NH, 256], F32)
        bt = sc.sb("bt", [128, NH, 16], F32)
        tabB = cx.dmabuf(f"{tag}_tab")
        for (t_, d_) in ((qg, qgT), (kg, kgT), (ala, alaT), (alb, albT), (bt, btT)):
            cx.dma("sp", lambda e, t_=t_, d_=d_: e.dma_start(out=t_[:], in_=d_), tabB, writes=[tabB])
        psN = sc.ps("psN", [128, 512], F32); psNB = Buf("psN")
        NTR = 1
        psTr = [sc.ps(f"psTr{i}", [128, 1024], BF16) for i in range(NTR)]
        psTrB = [Buf(f"psTr{i}") for i in range(NTR)]
        psG = sc.ps("psG", [128, 512], F32); psGB = Buf("psG")
        psS = [sc.ps(f"psS{i}", [128, 512], F32) for i in range(NSB)]
        psSB = [Buf(f"psS{i}") for i in range(NSB)]
        psO = sc.ps("psO", [128, 512], F32); psOB = Buf("psO")
        psR = sc.ps("psR", [128, 512], F32); psRB = Buf("psR")

        rc = 0
        trc = 0
        sc_i = 0
        oc = 0

        def load_raw(src_rows):
            nonlocal rc
            r = rc % 2
            rc += 1
            cx.dma("sp", lambda e, r=r: e.dma_start(out=raw[r][:], in_=src_rows), rawB[r], writes=[rawB[r]])
            return r

        def rms_rstd(r):
            for n in range(S // 512):
                cs = slice(n * 512, (n + 1) * 512)
                cx.op("act", lambda e, cs=cs: e.activation(out=sq[:, cs], in_=raw[r][:, cs], func=AF.Square),
                      reads=[rawB[r]], writes=[sqB] if n == 0 else [])
                sqB.w = {id(cx.esem["act"]): (cx.esem["act"], cx.ecnt["act"])}
                cx.op("pe", lambda e, cs=cs: e.matmul(psN[:], lhsT=ones_bf[:], rhs=sq[:, cs], start=True, stop=True),
                      reads=[sqB, onesB], writes=[psNB])
                cx.op("act", lambda e: e.activation(out=tmp[:], in_=psN[:], func=AF.Ln, scale=1.0 / 128, bias=eps_sb[:]),
                      reads=[psNB, epsB], writes=[tmpB])
                cx.op("act", lambda e, cs=cs: e.activation(out=rstd[:, cs], in_=tmp[:], func=AF.Exp, scale=-0.5),
                      reads=[tmpB], writes=[rstdB] if n == 0 else [])
                rstdB.w = {id(cx.esem["act"]): (cx.esem["act"], cx.ecnt["act"])}

        for j in range(NH):
            r = load_raw(kvT[j * 128:(j + 1) * 128, :])
            rms_rstd(r)
            cx.op("dve", lambda e: e.scalar_tensor_tensor(out=kn32[:], in0=raw[r][:], scalar=kg[:, 0:1], in1=rstd[:],
                                                          op0=ALU.mult, op1=ALU.mult),
                  reads=[rawB[r], tabB, rstdB], writes=[kn32B])
            cx.op("pool", lambda e: e.tensor_copy(out=knb[:], in_=kn32[:]), reads=[kn32B], writes=[knbB])
            cx.op("dve", lambda e: e.tensor_reduce(out=kms[:], in_=kn32[:].rearrange("p (n k) -> p n k", k=256),
                                                   axis=AX.X, op=ALU.add), reads=[kn32B], writes=[kmsB])
            cx.op("dve", lambda e: e.tensor_scalar(out=kmb[:], in0=kms[:], scalar1=1.0 / 256, scalar2=None, op0=ALU.mult),
                  reads=[kmsB], writes=[kmbB])
            r = load_raw(kvT[(NH + j) * 128:(NH + j + 1) * 128, :])
            cx.op("act", lambda e: e.activation(out=vb[:], in_=raw[r][:], func=AF.Copy), reads=[rawB[r]], writes=[vbB])
            for h in range(NT // 8):
                tr = trc % NTR
                trc += 1
                for i in range(8):
                    tt = h * 8 + i
                    cx.op("pe", lambda e, tr=tr, i=i, tt=tt: e.transpose(out=psTr[tr][:, i * 128:(i + 1) * 128],
                                                                        in_=vb[:, tt * 128:(tt + 1) * 128], identity=ident[:]),
                          reads=[vbB, identB], writes=[psTrB[tr]])
                cx.op("dve", lambda e, tr=tr, h=h: e.tensor_copy(out=vsb[:, h * 8:(h + 1) * 8, :].rearrange("p a b -> p (a b)"),
                                                                 in_=psTr[tr][:, :]),
                      reads=[psTrB[tr]], writes=[vsbB] if h == 0 else [])
                vsbB.w = {id(cx.esem["dve"]): (cx.esem["dve"], cx.ecnt["dve"])}
            r = load_raw(qT[j * 128:(j + 1) * 128, :])
            rms_rstd(r)
            cx.op("dve", lambda e: e.scalar_tensor_tensor(out=qnb[:], in0=raw[r][:], scalar=qg[:, 0:1], in1=rstd[:],
                                                          op0=ALU.mult, op1=ALU.mult),
                  reads=[rawB[r], tabB, rstdB], writes=[qnbB])
            for tt in range(NT):
                cx.op("pe", lambda e, tt=tt: e.matmul(psG[:, tt * 8:(tt + 1) * 8], lhsT=qnb[:, tt * 128:(tt + 1) * 128], rhs=kmb[:],
                                                      start=True, stop=True),
                      reads=[qnbB, kmbB], writes=[psGB])
            cx.op("dve", lambda e: e.tensor_tensor(out=gm[:].rearrange("p a b -> p (a b)"), in0=psG[:, 0:NT * 8],
                                                   in1=pastm[:].rearrange("p a b -> p (a b)"), op=ALU.add),
                  reads=[psGB, pastB], writes=[gmB])
            for tt in range(NT):
                cx.op("dve", lambda e, tt=tt: e.max(out=top8[:, tt, :], in_=gm[:, tt, :]), reads=[gmB],
                      writes=[top8B] if tt == 0 else [])
            top8B.w = {id(cx.esem["dve"]): (cx.esem["dve"], cx.ecnt["dve"])}
            for tt in range(NT):
                cx.op("dve", lambda e, tt=tt: e.tensor_scalar(out=bb[:, tt, :], in0=gm[:, tt, :], scalar1=top8[:, tt, 2:3],
                                                              scalar2=BIGNEG, op0=ALU.is_lt, op1=ALU.mult),
                      reads=[gmB, top8B], writes=[bbB] if tt == 0 else [])
            bbB.w = {id(cx.esem["dve"]): (cx.esem["dve"], cx.ecnt["dve"])}
            for h in range(NT // 8):
                tr = trc % NTR
                trc += 1
                for i in range(8):
                    tt = h * 8 + i
                    cx.op("pe", lambda e, tr=tr, i=i, tt=tt: e.transpose(out=psTr[tr][0:8, i * 128:(i + 1) * 128],
                                                                        in_=bb[:, tt, :], identity=ident[:]),
                          reads=[bbB, identB], writes=[psTrB[tr]])
                cx.op("dve", lambda e, tr=tr, h=h: e.tensor_copy(out=bbT[:, h * 1024:(h + 1) * 1024], in_=psTr[tr][0:8, :]),
                      reads=[psTrB[tr]], writes=[bbTB] if h == 0 else [])
                bbTB.w = {id(cx.esem["dve"]): (cx.esem["dve"], cx.ecnt["dve"])}
            items = []
            for qb in range(NQB):
                subs = [(2 * qb, 0, 256, True), (2 * qb + 1, 128, 128, True)] + [(ks, 0, 256, False) for ks in range(2 * qb)]
                for si, sub in enumerate(subs):
                    items.append((qb, si, len(subs)) + sub)

            def emit_scores(idx):
                qb, si, nsub, ks, lo, N, own = items[idx]
                s_ = idx % NSB
                q0 = qb * 256
                qc = slice(q0 + lo, q0 + lo + N)
                cx.op("pe", lambda e, s_=s_, ks=ks, qc=qc, N=N: e.matmul(psS[s_][:, 0:N], lhsT=knb[:, ks * 128:(ks + 1) * 128],
                                                                         rhs=qnb[:, qc], start=True, stop=False),
                      reads=[knbB, qnbB], writes=[psSB[s_]])
                cx.op("pe", lambda e, s_=s_, lo=lo, N=N: e.matmul(psS[s_][:, 0:N], lhsT=ala[0:2, j, :], rhs=alb[0:2, j, lo:lo + N],
                                                                  start=False, stop=False),
                      reads=[tabB], writes=[psSB[s_]])
                if own:
                    cx.op("pe", lambda e, s_=s_, N=N: e.matmul(psS[s_][:, 0:N], lhsT=ident[:], rhs=CM0[:, 0:N],
                                                               start=False, stop=True),
                          reads=[identB, CMB], writes=[psSB[s_]])
                else:
                    n = ks // 2
                    cx.op("pe", lambda e, s_=s_, n=n, qc=qc, N=N: e.matmul(psS[s_][:, 0:N], lhsT=E_sb[0:8, n, :], rhs=bbT[0:8, qc],
                                                                           start=False, stop=True),
                          reads=[EB, bbTB], writes=[psSB[s_]])
                didx = 2 * qb - ks + 1
                cx.op("act", lambda e, s_=s_, N=N, didx=didx: e.activation(out=pT[s_][:, 0:N], in_=psS[s_][:, 0:N], func=AF.Exp,
                                                                           scale=ATT_SCALE, bias=bt[:, j, didx:didx + 1]),
                      reads=[psSB[s_], tabB], writes=[pTB[s_]])

            def emit_pv(idx):
                nonlocal oc
                qb, si, nsub, ks, lo, N, own = items[idx]
                s_ = idx % NSB
                q0 = qb * 256
                last = (si == nsub - 1)
                cx.op("pe", lambda e, s_=s_, ks=ks, lo=lo, N=N, si=si, last=last: e.matmul(
                    psO[:, lo:lo + N], lhsT=vsb[:, ks, :], rhs=pT[s_][:, 0:N], start=(si == 0), stop=last),
                    reads=[vsbB, pTB[s_]], writes=[psOB])
                cx.op("pe", lambda e, s_=s_, lo=lo, N=N, si=si, last=last: e.matmul(
                    psR[:, lo:lo + N], lhsT=ones_bf[:], rhs=pT[s_][:, 0:N], start=(si == 0), stop=last),
                    reads=[onesB, pTB[s_]], writes=[psRB])
                if last:
                    o_ = oc % 2
                    oc += 1
                    cx.op("dve", lambda e: e.reciprocal(out=rec[:], in_=psR[:, 0:256]), reads=[psRB], writes=[recB])
                    cx.op("dve", lambda e, o_=o_: e.tensor_tensor(out=ogb[o_][:], in0=psO[:, 0:256], in1=rec[:], op=ALU.mult),
                          reads=[psOB, recB], writes=[ogB[o_]])
                    cx.dma("sp", lambda e, o_=o_, q0=q0: e.dma_start(out=ogT[j * 128:(j + 1) * 128, q0:q0 + 256], in_=ogb[o_][:]),
                           ogB[o_], reads=[ogB[o_]])

            LOOK = NSB - 1
            for idx in range(min(LOOK, len(items))):
                emit_scores(idx)
            for idx in range(len(items)):
                if idx + LOOK < len(items):
                    emit_scores(idx + LOOK)
                emit_pv(idx)

from concourse.bass_utils import run_bass_kernel_spmd

D_MODEL = 2048
SEQ = 2048
BATCH = 4
NHL = 8
D_FF = 8192
TOWN = 1024
NCORES = 8
_PROGS = {}


_DECL = {}


def _dram(nc, name, shape, dt, kind="ExternalInput"):
    if kind == "ExternalInput":
        _DECL[name] = (tuple(shape), dt)
    return nc.dram_tensor(name, list(shape), dt, kind=kind).ap()


def build_A(layer):
    nc = bass.Bass("TRN2", target_bir_lowering=False)
    cx = Ctx(nc)
    consts = setup_consts(cx)
    hT = _dram(nc, "hT", [D_MODEL, SEQ], F32)
    Wt = _dram(nc, "Wt", [4 * NHL, 128, D_MODEL], F32)
    gT = _dram(nc, "gT", [128, 16], F32)
    lbT = _dram(nc, "lbT", [128, 2, NHL], F32)
    hgT = _dram(nc, "hgT", [128, NHL], F32)
    projT = _dram(nc, "projT", [4 * NHL * 128, SEQ], F32, "Internal")
    ogT = _dram(nc, "ogT", [NHL * 128, SEQ], BF16, "ExternalOutput")
    emit_linear(cx, hT, Wt, projT, SEQ, D_MODEL, 4 * NHL * 128, F32, F32, consts, gainT=gT, tag="win")
    emit_gla(cx, projT, lbT, hgT, ogT, NHL, SEQ, layer, consts)
    cx.finish()
    return nc


def build_C(with_kv):
    nc = bass.Bass("TRN2", target_bir_lowering=False)
    cx = Ctx(nc)
    consts = setup_consts(cx)
    hT = _dram(nc, "hT", [D_MODEL, SEQ], F32)
    Wq = _dram(nc, "Wq", [NHL, 128, D_MODEL], F32)
    gT = _dram(nc, "gT", [128, 16], F32)
    qgT = _dram(nc, "qgT", [128, 1], F32)
    kgT = _dram(nc, "kgT", [128, 1], F32)
    alaT = _dram(nc, "alaT", [2, NHL, 128], F32)
    albT = _dram(nc, "albT", [2, NHL, 256], F32)
    btT = _dram(nc, "btT", [128, NHL, 16], F32)
    qT = _dram(nc, "qT", [NHL * 128, SEQ], F32, "Internal")
    ogT = _dram(nc, "ogT", [NHL * 128, SEQ], BF16, "ExternalOutput")
    if with_kv:
        Wkv = _dram(nc, "Wkv", [2 * NHL, 128, D_MODEL], F32)
        gkvT = _dram(nc, "gkvT", [128, 16], F32)
        kvT = _dram(nc, "kvT", [2 * NHL * 128, SEQ], F32, "ExternalOutput")
        emit_linear(cx, hT, Wkv, kvT, SEQ, D_MODEL, 2 * NHL * 128, F32, F32, consts, gainT=gkvT, tag="wkv")
    else:
        kvT = _dram(nc, "kvT", [2 * NHL * 128, SEQ], F32)
    emit_linear(cx, hT, Wq, qT, SEQ, D_MODEL, NHL * 128, F32, F32, consts, gainT=gT, tag="wq")
    emit_moba(cx, qT, kvT, qgT, kgT, alaT, albT, btT, ogT, NHL, SEQ, consts)
    cx.finish()
    return nc


def build_B():
    nc = bass.Bass("TRN2", target_bir_lowering=False)
    cx = Ctx(nc)
    consts = setup_consts(cx)
    ogT = _dram(nc, "ogT", [D_MODEL, TOWN], BF16)
    hT = _dram(nc, "hT", [D_MODEL, TOWN], F32)
    Wo = _dram(nc, "Wo", [16, 128, D_MODEL], F32)
    gT = _dram(nc, "gT", [128, 16], F32)
    W1 = _dram(nc, "W1", [64, 128, D_MODEL], F32)
    W2 = _dram(nc, "W2", [16, 128, D_FF], F32)
    h1T = _dram(nc, "h1T", [D_MODEL, TOWN], F32, "Internal")
    aT = _dram(nc, "aT", [D_FF, TOWN], BF16, "Internal")
    h2T = _dram(nc, "h2T", [D_MODEL, TOWN], F32, "ExternalOutput")
    emit_linear(cx, ogT, Wo, h1T, TOWN, D_MODEL, D_MODEL, BF16, F32, consts, resT=hT, tag="wo")
    emit_linear(cx, h1T, W1, aT, TOWN, D_MODEL, D_FF, F32, BF16, consts, gainT=gT, act="relu2", tag="w1")
    emit_linear(cx, aT, W2, h2T, TOWN, D_FF, D_MODEL, BF16, F32, consts, resT=h1T, tag="w2")
    cx.finish()
    return nc


PAIRS = [[0, 1], [2, 3], [4, 5], [6, 7]]


def build_fused(layers=(0, 1, 2, 3), info=None):
    nc = bass.Bass("TRN2", target_bir_lowering=False)
    cx = Ctx(nc)
    consts = setup_consts(cx)
    I = lambda n, s, dt=F32: _dram(nc, n, s, dt)
    N = lambda n, s, dt=F32: _dram(nc, n, s, dt, "Internal")
    rsel = I("rsel", [128, 2])
    hall0 = I("hall0", [2 * D_MODEL, TOWN])
    hT0 = I("hT0", [D_MODEL, TOWN])
    hall = N("hall", [2 * D_MODEL, TOWN])
    ogT = N("ogT", [NHL * 128, SEQ], BF16)
    ogall = N("ogall", [2 * NHL * 128, SEQ], BF16)
    projT = N("projT", [4 * NHL * 128, SEQ])
    qT = N("qT", [NHL * 128, SEQ])
    kvT = N("kvT", [2 * NHL * 128, SEQ])
    h1T = N("h1T", [D_MODEL, TOWN])
    aT = N("aT", [D_FF, TOWN], BF16)
    hbuf = [N("hA", [D_MODEL, TOWN]), N("hB", [D_MODEL, TOWN])]
    hout = _dram(nc, "hout", [D_MODEL, TOWN], F32, "ExternalOutput")
    agB = cx.dmabuf("ag")
    qgT = [None] * 2
    ogv = ogall.rearrange("(i r cc p) t -> r i p cc t", i=4, r=2, cc=2, p=128)

    def ogsrc(off):
        def f(c0, c1, t0, n):
            assert c0 == 0 and c1 == 16
            return [(8 * rr + 2 * i, 8 * rr + 2 * i + 2, ogv[rr, i, :, :, off + t0:off + t0 + n])
                    for rr in range(2) for i in range(4)]
        return f
    ogA = ogsrc(0)
    ogBf = ogsrc(TOWN)
    kgT = I("kgT", [128, 1])
    alaT = I("alaT", [2, NHL, 128])
    albT = I("albT", [2, NHL, 256])
    btT = I("btT", [128, NHL, 16])
    lbT = I("lbT", [128, 2, NHL])
    for l in layers:
        if l != layers[0]:
            cx.renew_engine_sems()
        h_cur = hT0 if l == layers[0] else hbuf[(l - 1) % 2]
        if l == layers[0]:
            src_all = hall0
        else:
            for i in range(8):
                cx.coll(lambda e, h_cur=h_cur, i=i: e.collective_compute(
                    "AllGather", ALU.bypass, replica_groups=PAIRS,
                    ins=[h_cur[256 * i:256 * (i + 1), :].opt()], outs=[hall[512 * i:512 * (i + 1), :].opt()]), agB)
            cx.barrier_on(agB)
            src_all = hall
        if l == layers[0]:
            sv0 = src_all.rearrange("(r c p) t -> r p c t", r=2, p=128)
            xs = lambda c0, c1, t0, n, sv0=sv0: [(c0, c1, sv0[t0 // TOWN, :, c0:c1, (t0 % TOWN):(t0 % TOWN) + n])]
        else:
            sv = src_all.rearrange("(i r cc p) t -> r i p cc t", i=8, r=2, cc=2, p=128)
            xs = lambda c0, c1, t0, n, sv=sv: [(2 * i, 2 * i + 2, sv[t0 // TOWN, i, :, :, (t0 % TOWN):(t0 % TOWN) + n])
                                               for i in range(c0 // 2, c1 // 2)]
        gT = I(f"gmix{l}", [128, 16])
        if l < 2:
            Wt = I(f"Win{l}", [4 * NHL, 128, D_MODEL])
            hgT = I(f"hg{l}", [128, NHL])
            emit_linear(cx, None, Wt, projT, SEQ, D_MODEL, 4 * NHL * 128, F32, F32, consts, gainT=gT, tag=f"win{l}", xsrc=xs)
            emit_gla(cx, projT, lbT, hgT, ogT, NHL, SEQ, l, consts, tag=f"gla{l}")
        else:
            Wq = I(f"Wq{l}", [NHL, 128, D_MODEL])
            qg = I(f"qg{l}", [128, 1])
            if l == 2:
                Wkv = I("Wkv", [2 * NHL, 128, D_MODEL])
                gkvT = I("gkv", [128, 16])
                emit_linear(cx, None, Wkv, kvT, SEQ, D_MODEL, 2 * NHL * 128, F32, F32, consts, gainT=gkvT, tag="wkv", xsrc=xs)
            emit_linear(cx, None, Wq, qT, SEQ, D_MODEL, NHL * 128, F32, F32, consts, gainT=gT, tag=f"wq{l}", xsrc=xs)
            emit_moba(cx, qT, kvT, qg, kgT, alaT, albT, btT, ogT, NHL, SEQ, consts, tag=f"moba{l}")
        for i in range(4):
            cx.coll(lambda e, i=i: e.collective_compute(
                "AllGather", ALU.bypass, replica_groups=PAIRS,
                ins=[ogT[256 * i:256 * (i + 1), :].opt()], outs=[ogall[512 * i:512 * (i + 1), :].opt()]), agB)
        cx.barrier_on(agB)
        Wo = I(f"Wo{l}", [16, 128, D_MODEL])
        g2 = I(f"gmlp{l}", [128, 16])
        W1 = I(f"W1_{l}", [64, 128, D_MODEL])
        W2 = I(f"W2_{l}", [16, 128, D_FF])
        h_next = hout if l == layers[-1] else hbuf[l % 2]
        emit_linear(cx, None, Wo, h1T, TOWN, D_MODEL, D_MODEL, BF16, F32, consts, resT=h_cur, tag=f"wo{l}",
                    xsrc=ogA, xsrcB=ogBf, selT=rsel)
        emit_linear(cx, h1T, W1, aT, TOWN, D_MODEL, D_FF, F32, BF16, consts, gainT=g2, act="relu2", tag=f"w1{l}")
        emit_linear(cx, aT, W2, h_next, TOWN, D_FF, D_MODEL, BF16, F32, consts, resT=h1T, tag=f"w2{l}", TBLK=1024, NW=2)
    if info is not None:
        info['nsem'] = cx.nsem
        info['ecnt'] = dict(cx.ecnt)
        info['maxcnt'] = getattr(cx, 'maxcnt', 0)
        info['maxdma'] = max([v for (_, v) in cx.final] + [b.dval for b in cx.alldma])
    cx.finish()
    return nc


def _head_cols(r, nparts):
    cols = []
    for a in range(nparts):
        for j in range(NHL):
            h = NHL * r + j
            cols.append(np.arange(a * D_MODEL + h * 128, a * D_MODEL + (h + 1) * 128))
    return np.concatenate(cols)


def kernel(x, a_norm, a_w_in, a_head_norm, a_w_out, lower_bounds, kv_norm, w_kv, k_norm,
           b_norm, b_w_q, b_q_norm, b_w_o, mlp_norm, mlp_w1, mlp_w2):
    f32 = lambda a: np.asarray(a, dtype=np.float32)
    x = f32(x)
    shared = dict(const_inputs())
    shared["kgT"] = f32(k_norm)[:, None].copy()
    for l in range(4):
        shared[f"gmlp{l}"] = fm(f32(mlp_norm[l]))
        shared[f"W1_{l}"] = tile_w(f32(mlp_w1[l]))
        shared[f"W2_{l}"] = tile_w(f32(mlp_w2[l]))
        shared[f"Wo{l}"] = tile_w(f32(a_w_out[l] if l < 2 else b_w_o[l - 2]))
        shared[f"gmix{l}"] = fm(f32(a_norm[l] if l < 2 else b_norm[l - 2]))
        if l >= 2:
            shared[f"qg{l}"] = f32(b_q_norm[l - 2])[:, None].copy()
    shared["gkv"] = fm(f32(kv_norm))
    per_rank = []
    for r in range(2):
        d = {}
        hs = slice(NHL * r * 128, NHL * (r + 1) * 128)
        for l in range(2):
            d[f"Win{l}"] = tile_w(f32(a_w_in[l])[:, _head_cols(r, 4)])
            d[f"hg{l}"] = np.ascontiguousarray(f32(a_head_norm[l])[hs].reshape(NHL, 128).T)
        d["lbT"] = np.ascontiguousarray(f32(lower_bounds)[:, hs].reshape(2, NHL, 128).transpose(2, 0, 1))
        for l in (2, 3):
            d[f"Wq{l}"] = tile_w(f32(b_w_q[l - 2])[:, _head_cols(r, 1)])
        d["Wkv"] = tile_w(f32(w_kv)[:, _head_cols(r, 2)])
        ala, alb, bt = moba_tables(list(range(NHL * r, NHL * (r + 1))))
        d["alaT"], d["albT"], d["btT"] = ala, alb, bt
        rs = np.zeros((128, 2), np.float32)
        rs[:, r] = 1.0
        d["rsel"] = rs
        per_rank.append(d)
    maps = []
    for c in range(NCORES):
        b, r = divmod(c, 2)
        xb = x[b]
        hall0 = np.ascontiguousarray(xb.reshape(2, TOWN, D_MODEL).transpose(0, 2, 1)).reshape(2 * D_MODEL, TOWN)
        m = dict(shared)
        m.update(per_rank[r])
        m["hall0"] = hall0
        m["hT0"] = np.ascontiguousarray(hall0[r * D_MODEL:(r + 1) * D_MODEL])
        maps.append(m)
    nc = build_fused()
    res = run_bass_kernel_spmd(nc, maps, core_ids=list(range(NCORES))).results
    out = np.empty((BATCH, SEQ, D_MODEL), np.float32)
    for c in range(NCORES):
        b, r = divmod(c, 2)
        out[b, r * TOWN:(r + 1) * TOWN, :] = np.asarray(res[c]["hout"]).T
    return out
```

```python
from contextlib import ExitStack
import numpy as np
import concourse.bass as bass
import concourse.mybir as mybir

F32 = mybir.dt.float32
BF16 = mybir.dt.bfloat16
AF = mybir.ActivationFunctionType
ALU = mybir.AluOpType
AX = mybir.AxisListType

ENGS = ("pe", "act", "dve", "pool", "sp")


class Buf:
    __slots__ = ("name", "w", "r", "dsem", "dval")

    def __init__(self, name):
        self.name = name
        self.w = {}
        self.r = {}
        self.dsem = None
        self.dval = 0


class Scope:
    uid = 0

    def __init__(self, cx):
        self.cx = cx
        self.es = ExitStack()

    def __enter__(self):
        self.mark = len(self.cx.scope_bufs)
        return self

    def __exit__(self, *a):
        self.cx.barrier()
        for b in self.cx.scope_bufs[self.mark:]:
            self.cx.sempool.append((b.dsem, b.dval))
            self.cx.alldma.remove(b)
            self.cx.final.append((b.dsem, b.dval))
        del self.cx.scope_bufs[self.mark:]
        self.es.close()
        return False

    def sb(self, name, shape, dt):
        Scope.uid += 1
        return self.es.enter_context(self.cx.nc.sbuf_tensor(f"{name}_{Scope.uid}", list(shape), dt))

    def ps(self, name, shape, dt):
        Scope.uid += 1
        return self.es.enter_context(self.cx.nc.psum_tensor(f"{name}_{Scope.uid}", list(shape), dt))


class Ctx:
    def __init__(self, nc):
        self.nc = nc
        self.es = ExitStack()
        self.eng = {"pe": nc.tensor, "act": nc.scalar, "dve": nc.vector, "pool": nc.gpsimd, "sp": nc.sync}
        self.esem = {}
        self.ecnt = {e: 0 for e in ENGS}
        for e in ENGS:
            self.esem[e] = self.es.enter_context(nc.semaphore("es_" + e))
        self.known = {e: {} for e in ENGS}
        self.nsem = 0
        self.final = []
        self.alldma = []
        self.sempool = []
        self.scope_bufs = []

    def scope(self):
        return Scope(self)

    def barrier(self):
        deps = {}
        for e in ENGS:
            if self.ecnt[e] > 0:
                deps[id(self.esem[e])] = (self.esem[e], self.ecnt[e])
        for b in self.alldma:
            if b.dval > 0:
                deps[id(b.dsem)] = (b.dsem, b.dval)
        for e in ENGS:
            self._emit_waits(e, deps)

    def newsem(self, name):
        self.nsem += 1
        return self.es.enter_context(self.nc.semaphore(name))

    def dmabuf(self, name):
        b = Buf(name)
        if self.sempool:
            b.dsem, b.dval = self.sempool.pop()
        else:
            b.dsem = self.newsem("d_%d" % self.nsem)
        self.alldma.append(b)
        self.scope_bufs.append(b)
        return b

    def _collect(self, reads, writes):
        deps = {}
        for b in reads:
            for k, (s, v) in b.w.items():
                if k not in deps or deps[k][1] < v:
                    deps[k] = (s, v)
        for b in writes:
            for d in (b.w, b.r):
                for k, (s, v) in d.items():
                    if k not in deps or deps[k][1] < v:
                        deps[k] = (s, v)
        return deps

    def _emit_waits(self, e, deps, skip_self=False):
        kn = self.known[e]
        for k, (s, v) in deps.items():
            if skip_self and k == id(self.esem[e]):
                continue
            if kn.get(k, 0) >= v:
                continue
            kn[k] = v
            self.eng[e].wait_ge(s, v)

    def op(self, e, fn, reads=(), writes=()):
        deps = self._collect(reads, writes)
        self._emit_waits(e, deps, skip_self=(e == "pe"))
        self.ecnt[e] += 1
        tok = (self.esem[e], self.ecnt[e])
        k = id(self.esem[e])
        fn(self.eng[e]).then_inc(self.esem[e], 1)
        for b in reads:
            b.r[k] = tok
        for b in writes:
            b.w = {k: tok}
            b.r = {}

    def dma(self, q, fn, owner, reads=(), writes=(), serial=True):
        deps = self._collect(reads, writes)
        if owner.dval > 0 and serial:
            deps[id(owner.dsem)] = (owner.dsem, max(owner.dval, deps.get(id(owner.dsem), (None, 0))[1]))
        self._emit_waits(q, deps)
        owner.dval += 16
        tok = (owner.dsem, owner.dval)
        k = id(owner.dsem)
        fn(self.eng[q]).then_inc(owner.dsem, 16)
        for b in reads:
            b.r[k] = tok
        for b in writes:
            b.w = {k: tok}
            b.r = {}
        return tok

    def renew_engine_sems(self):
        self.barrier()
        for e in ENGS:
            self.maxcnt = max(getattr(self, "maxcnt", 0), self.ecnt[e])
            if self.ecnt[e] > 0:
                self.esem[e] = self.es.enter_context(self.nc.semaphore("es%d_%s" % (self.nsem, e)))
                self.nsem += 1
                self.ecnt[e] = 0

    def barrier_on(self, owner):
        deps = {id(owner.dsem): (owner.dsem, owner.dval)}
        for e in ENGS:
            self._emit_waits(e, deps)

    def coll(self, fn, owner):
        deps = {}
        if owner.dval > 0:
            deps[id(owner.dsem)] = (owner.dsem, owner.dval)
        self._emit_waits("pool", deps)
        owner.dval += 1
        fn(self.eng["pool"]).then_inc(owner.dsem)

    def dma_multi(self, q, fn, owner, reads=(), writes_acc=()):
        deps = self._collect(reads, ())
        for b in writes_acc:
            for kk, (s, v) in b.r.items():
                if kk not in deps or deps[kk][1] < v:
                    deps[kk] = (s, v)
        if owner.dval > 0:
            deps[id(owner.dsem)] = (owner.dsem, max(owner.dval, deps.get(id(owner.dsem), (None, 0))[1]))
        self._emit_waits(q, deps)
        owner.dval += 16
        tok = (owner.dsem, owner.dval)
        k = id(owner.dsem)
        fn(self.eng[q]).then_inc(owner.dsem, 16)
        for b in reads:
            b.r[k] = tok
        for b in writes_acc:
            b.w[k] = tok
        return tok

    def finish(self):
        deps = {}
        for e in ENGS:
            if self.ecnt[e] > 0:
                deps[id(self.esem[e])] = (self.esem[e], self.ecnt[e])
        for b in self.alldma:
            if b.dval > 0:
                deps[id(b.dsem)] = (b.dsem, b.dval)
        for (s_, v) in self.final:
            if id(s_) not in deps or deps[id(s_)][1] < v:
                deps[id(s_)] = (s_, v)
        for k, (s, v) in deps.items():
            self.eng["sp"].wait_ge(s, v)
        self.es.close()


EPS = 1e-6


def _load_pieces(cx, pieces, dst_of, owner, buf):
    first = True
    for (cs, ce, sap) in pieces:
        cx.dma("sp", lambda e, dst=dst_of(cs, ce), sap=sap: e.dma_start(out=dst, in_=sap), owner,
               writes=[buf] if first else [], serial=first)
        first = False
    buf.w = {id(owner.dsem): (owner.dsem, owner.dval)}
    buf.r = {}


def emit_linear(cx, xT, Wt, outT, T, Fin, Fout, x_dt, out_dt, consts, gainT=None, resT=None, act=None,
                TBLK=None, tag="lin", xsrc=None, xsrcB=None, selT=None, NW=3):
    nc = cx.nc
    KC = Fin // 128
    MC = Fout // 128
    if TBLK is None:
        TBLK = min(T, 1024 if KC <= 16 else 512)
    NB = TBLK // 512
    G = max(1, 64 // KC)
    assert MC % G == 0
    if xsrc is None:
        xTv = xT.rearrange("(c p) t -> p c t", p=128)
        xsrc = lambda c0, c1, t0, n: [(c0, c1, xTv[:, c0:c1, t0:t0 + n])]
    with cx.scope() as sc:
        xn = sc.sb("xn", [128, KC, TBLK], BF16)
        xnB = Buf("xn")
        if xsrcB is not None:
            xtm = sc.sb("xtm", [128, KC, TBLK], BF16)
            xtmB = Buf("xtm")
            xtmD = cx.dmabuf(f"{tag}_xtm")
            sel = sc.sb("sel", [128, 2], F32)
            selB = cx.dmabuf(f"{tag}_sel")
            cx.dma("sp", lambda e: e.dma_start(out=sel[:], in_=selT), selB, writes=[selB])
        wb = [sc.sb(f"w{i}", [128, G, KC * 128], BF16) for i in range(NW)]
        wB = [cx.dmabuf(f"{tag}_w{i}") for i in range(NW)]
        NPS = 4
        pss = [sc.ps(f"ps{i}", [128, 512], F32) for i in range(NPS)]
        psB = [Buf(f"ps{i}") for i in range(NPS)]
        NO = 3
        ost = [sc.sb(f"o{i}", [128, 512], out_dt) for i in range(NO)]
        ostB = [cx.dmabuf(f"{tag}_o{i}") for i in range(NO)]
        if act == "relu2":
            rst = [sc.sb(f"r{i}", [128, 512], F32) for i in range(2)]
            rstB = [Buf(f"r{i}") for i in range(2)]
        if resT is not None:
            rsd = [sc.sb(f"rs{i}", [128, 512], F32) for i in range(3)]
            rsdB = [cx.dmabuf(f"{tag}_rs{i}") for i in range(3)]
        if gainT is not None:
            xs = sc.sb("xs", [128, KC, 512], F32)
            xsB = cx.dmabuf(f"{tag}_xs")
            sqb = sc.sb("sqb", [128, KC, 512], BF16)
            sqB = Buf("sqb")
            gsb = sc.sb("gsb", [128, KC], F32)
            gB = cx.dmabuf(f"{tag}_g")
            cx.dma("sp", lambda e: e.dma_start(out=gsb[:], in_=gainT), gB, writes=[gB])
            pssum = sc.ps("pssum", [128, 512], F32)
            pssB = Buf("pssum")
            tmp = sc.sb("tmp", [128, 512], F32)
            tmpB = Buf("tmp")
            rstd = sc.sb("rstd", [128, 512], F32)
            rstdB = Buf("rstd")
        else:
            xnD = cx.dmabuf(f"{tag}_xn")
        ones_bf, onesB = consts["ones_bf"]
        eps_sb, epsB = consts["eps"]
        cnt = 0
        wcnt = 0
        for tb in range(T // TBLK):
            t0 = tb * TBLK
            if gainT is None:
                for c0 in range(0, KC, 16):
                    _load_pieces(cx, xsrc(c0, c0 + 16, t0, TBLK), lambda cs, ce: xn[:, cs:ce, :], xnD, xnB)
                    if xsrcB is not None:
                        _load_pieces(cx, xsrcB(c0, c0 + 16, t0, TBLK), lambda cs, ce: xtm[:, cs:ce, :], xtmD, xtmB)
                        cx.op("dve", lambda e, c0=c0: e.tensor_scalar(out=xn[:, c0:c0 + 16, :], in0=xn[:, c0:c0 + 16, :],
                                                                      scalar1=sel[:, 0:1], scalar2=None, op0=ALU.mult),
                              reads=[xnB, selB], writes=[xnB])
                        cx.op("dve", lambda e, c0=c0: e.scalar_tensor_tensor(out=xn[:, c0:c0 + 16, :], in0=xtm[:, c0:c0 + 16, :],
                                                                             scalar=sel[:, 1:2], in1=xn[:, c0:c0 + 16, :],
                                                                             op0=ALU.mult, op1=ALU.add),
                              reads=[xtmB, xnB, selB], writes=[xnB])
            else:
                for n in range(NB):
                    ts = t0 + n * 512
                    _load_pieces(cx, xsrc(0, KC, ts, 512), lambda cs, ce: xs[:, cs:ce, :], xsB, xsB)
                    for c in range(KC):
                        cx.op("act", lambda e, c=c: e.activation(out=sqb[:, c, :], in_=xs[:, c, :], func=AF.Square),
                              reads=[xsB], writes=[sqB] if c == 0 else [])
                        if c > 0:
                            sqB.w = {id(cx.esem["act"]): (cx.esem["act"], cx.ecnt["act"])}
                    for c in range(KC):
                        cx.op("pe", lambda e, c=c: e.matmul(pssum[:], lhsT=ones_bf[:], rhs=sqb[:, c, :],
                                                            start=(c == 0), stop=(c == KC - 1)),
                              reads=[sqB, onesB], writes=[pssB])
                    cx.op("act", lambda e: e.activation(out=tmp[:], in_=pssum[:], func=AF.Ln, scale=1.0 / Fin, bias=eps_sb[:]),
                          reads=[pssB, epsB], writes=[tmpB])
                    cx.op("act", lambda e: e.activation(out=rstd[:], in_=tmp[:], func=AF.Exp, scale=-0.5),
                          reads=[tmpB], writes=[rstdB])
                    for c in range(KC):
                        cx.op("dve", lambda e, c=c, n=n: e.scalar_tensor_tensor(
                            out=xn[:, c, n * 512:(n + 1) * 512], in0=xs[:, c, :], scalar=gsb[:, c:c + 1], in1=rstd[:],
                            op0=ALU.mult, op1=ALU.mult),
                            reads=[xsB, gB, rstdB], writes=[xnB] if (c == 0 and n == 0) else [])
                        xnB.w = {id(cx.esem["dve"]): (cx.esem["dve"], cx.ecnt["dve"])}
            NMG = MC // G

            def issue_w(mg):
                wi = (wbase + mg) % NW
                cx.dma("pool", lambda e, wi=wi, mg=mg: e.dma_start(
                    out=wb[wi][:], in_=Wt[mg * G:(mg + 1) * G].rearrange("g p f -> p g f"), max_dma_last_dim=8192),
                    wB[wi], writes=[wB[wi]])

            wbase = wcnt
            wcnt += NMG
            iters = [(m, n) for m in range(MC) for n in range(NB)]

            def issue_res(i):
                m, n = iters[i]
                ri = i % 3
                ts = t0 + n * 512
                cx.dma("sp", lambda e, ri=ri, m=m, ts=ts: e.dma_start(
                    out=rsd[ri][:], in_=resT[m * 128:(m + 1) * 128, ts:ts + 512]), rsdB[ri], writes=[rsdB[ri]])

            for mg in range(min(NW - 1, NMG)):
                issue_w(mg)
            if resT is not None:
                for i in range(min(2, len(iters))):
                    issue_res(i)
            it = 0
            for mg in range(NMG):
                if mg + NW - 1 < NMG:
                    issue_w(mg + NW - 1)
                wi = (wbase + mg) % NW
                for g in range(G):
                    m = mg * G + g
                    for n in range(NB):
                        pi = cnt % NPS
                        oi = cnt % NO
                        cnt += 1
                        for c in range(KC):
                            cx.op("pe", lambda e, pi=pi, wi=wi, g=g, c=c, n=n: e.matmul(
                                pss[pi][:], lhsT=wb[wi][:, g, c * 128:(c + 1) * 128], rhs=xn[:, c, n * 512:(n + 1) * 512],
                                start=(c == 0), stop=(c == KC - 1)),
                                reads=[wB[wi], xnB], writes=[psB[pi]])
                        ts = t0 + n * 512
                        dst = outT[m * 128:(m + 1) * 128, ts:ts + 512]
                        if act == "relu2":
                            ri = cnt % 2
                            cx.op("act", lambda e, pi=pi, ri=ri: e.activation(out=rst[ri][:], in_=pss[pi][:], func=AF.Relu),
                                  reads=[psB[pi]], writes=[rstB[ri]])
                            cx.op("dve", lambda e, ri=ri, oi=oi: e.tensor_tensor(out=ost[oi][:], in0=rst[ri][:], in1=rst[ri][:],
                                                                                 op=ALU.mult),
                                  reads=[rstB[ri]], writes=[ostB[oi]])
                        elif resT is not None:
                            ri = it % 3
                            if it + 2 < len(iters):
                                issue_res(it + 2)
                            cx.op("dve", lambda e, pi=pi, ri=ri, oi=oi: e.tensor_tensor(
                                out=ost[oi][:], in0=pss[pi][:], in1=rsd[ri][:], op=ALU.add),
                                reads=[psB[pi], rsdB[ri]], writes=[ostB[oi]])
                        else:
                            if cnt % 2 == 0:
                                cx.op("act", lambda e, pi=pi, oi=oi: e.activation(out=ost[oi][:], in_=pss[pi][:], func=AF.Copy),
                                      reads=[psB[pi]], writes=[ostB[oi]])
                            else:
                                cx.op("dve", lambda e, pi=pi, oi=oi: e.tensor_copy(out=ost[oi][:], in_=pss[pi][:]),
                                      reads=[psB[pi]], writes=[ostB[oi]])
                        cx.dma("sp", lambda e, oi=oi, dst=dst: e.dma_start(out=dst, in_=ost[oi][:]), ostB[oi], reads=[ostB[oi]])
                        it += 1


import ml_dtypes

NPBF = ml_dtypes.bfloat16
BIGNEG = -30000.0
ATT_SCALE = 128 ** -0.5


def tile_w(W):
    Fin, Fout = W.shape
    KC, MC = Fin // 128, Fout // 128
    return np.ascontiguousarray(W.reshape(KC, 128, MC, 128).transpose(2, 1, 0, 3)).reshape(MC, 128, KC * 128)


def fm(v):
    return np.ascontiguousarray(v.reshape(-1, 128).T)


def _const_tables():
    c = {}
    c["c_ones_bf"] = np.ones((128, 128), NPBF)
    c["c_ident_bf"] = np.eye(128, dtype=np.float32).astype(NPBF)
    c["c_eps"] = np.full((128, 1), EPS, np.float32)
    seg = np.ones((128, 512), np.float32)
    seg[:, ::32] = 0.0
    c["c_segmask"] = seg
    s = np.arange(128)[:, None]
    t = np.arange(128)[None, :]
    c["c_gmaskT"] = ((s <= t) & (s // 32 == t // 32)).astype(np.float32)
    pm = np.zeros((128, 16, 8), np.float32)
    for tile in range(16):
        qb = tile // 2
        pm[:, tile, qb:] = -1e30
    c["c_pastmask"] = pm
    E = np.zeros((8, 8, 128), np.float32)
    for n in range(8):
        E[n, n, :] = 1.0
    c["c_E"] = E.astype(NPBF)
    s = np.arange(128)[:, None]
    t = np.arange(256)[None, :]
    c["c_CM0"] = np.where(s > t, BIGNEG, 0.0).astype(np.float32).astype(NPBF)
    return c


CONST_DT = {"c_ones_bf": BF16, "c_ident_bf": BF16, "c_eps": F32, "c_segmask": F32, "c_gmaskT": F32,
            "c_pastmask": F32, "c_E": BF16, "c_CM0": BF16}


def const_inputs():
    return _const_tables()


def setup_consts(cx, names=None):
    nc = cx.nc
    tabs = _const_tables()
    out = {}
    ld = cx.dmabuf("constld")
    for k, arr in tabs.items():
        d = nc.dram_tensor(k, list(arr.shape), CONST_DT[k], kind="ExternalInput").ap()
        if names is not None and k not in names:
            continue
        t = cx.es.enter_context(nc.sbuf_tensor("sb_" + k, list(arr.shape), CONST_DT[k]))
        b = Buf(k)
        cx.dma("sp", lambda e, t=t, d=d: e.dma_start(out=t[:], in_=d), ld, writes=[b])
        out[k[2:]] = (t, b)
    return out


def emit_gla(cx, projT, lbT, hgT, ogT, NH, S, layer, consts, tag="gla"):
    nc = cx.nc
    NBLK = S // 512
    pv = projT.rearrange("(a j p) t -> p a j t", a=4, j=NH, p=128)
    ones_bf, onesB = consts["ones_bf"]
    ident, identB = consts["ident_bf"]
    eps_sb, epsB = consts["eps"]
    segm, segB = consts["segmask"]
    gmask, gmB = consts["gmaskT"]
    with cx.scope() as sc:
        def T2(name, dt=F32, shape=(128, 512)):
            return [sc.sb(f"{name}{i}", list(shape), dt) for i in range(2)], [Buf(f"{name}{i}") for i in range(2)]
        inb = [sc.sb(f"in{i}", [128, 4, 512], F32) for i in range(2)]
        inB = [cx.dmabuf(f"{tag}_in{i}") for i in range(2)]
        qs, qsB = T2("qs")
        gate, gateB = T2("gate")
        ee, eeB = T2("ee")
        rr, rrB = T2("rr")
        ff, ffB = T2("ff")
        kk, kkB = T2("kk")
        lf, lfB = T2("lf")
        bc, bcB = T2("bc")
        eb, ebB = T2("eb")
        einv, einvB = T2("einv")
        qtb, qtbB = T2("qtb", BF16)
        ktb, ktbB = T2("ktb", BF16)
        khb, khbB = T2("khb", BF16)
        ib, ibB = T2("ib", BF16)
        vsb, vsbB = T2("vsb", BF16, (128, 4, 128))
        v32, v32B = T2("v32", BF16, (32, 16, 128))
        kh32, kh32B = T2("kh32", BF16, (32, 16, 128))
        amb, ambB = T2("amb", BF16, (128, 128))
        sbf = [[sc.sb(f"sbf{c_}{i}", [128, 128], BF16) for i in range(2)] for c_ in range(2)]
        sbfB = [[Buf(f"sbf{c_}{i}") for i in range(2)] for c_ in range(2)]
        osb, osbB = T2("osb")
        osq, osqB = T2("osq", BF16)
        tmp, tmpB = T2("tmp")
        rstd, rstdB = T2("rstd")
        ogb = [sc.sb(f"ogb{i}", [128, 512], BF16) for i in range(2)]
        ogB = [cx.dmabuf(f"{tag}_og{i}") for i in range(2)]
        s32 = [sc.sb(f"s32_{i}", [128, 128], F32) for i in range(2)]
        s32B = [Buf(f"s32_{i}") for i in range(2)]
        lbr = sc.sb("lbr", [128, 2, NH], F32)
        lbB = cx.dmabuf(f"{tag}_lb")
        hg = sc.sb("hg", [128, NH], F32)
        hgB = cx.dmabuf(f"{tag}_hg")
        lb = sc.sb("lb", [128, NH], F32)
        oml = sc.sb("oml", [128, NH], F32)
        lbcB = Buf("lbc")
        psTr = [sc.ps(f"psTr{i}", [128, 1024], BF16) for i in range(2)]
        psTrB = [Buf(f"psTr{i}") for i in range(2)]
        psA = [sc.ps(f"psA{i}", [128, 512], F32) for i in range(1)]
        psAB = [Buf(f"psA{i}") for i in range(1)]
        psO = [sc.ps(f"psO{i}", [128, 512], F32) for i in range(2)]
        psOB = [Buf(f"psO{i}") for i in range(2)]
        psS = [sc.ps(f"psS{i}", [128, 512], F32) for i in range(2)]
        psSB = [Buf(f"psS{i}") for i in range(2)]
        psN = sc.ps("psN", [128, 512], F32)
        psNB = Buf("psN")

        cx.dma("sp", lambda e: e.dma_start(out=lbr[:], in_=lbT), lbB, writes=[lbB])
        cx.dma("sp", lambda e: e.dma_start(out=hg[:], in_=hgT), hgB, writes=[hgB])
        if layer == 0:
            cx.op("dve", lambda e: e.memset(lb[:], 0.0), writes=[lbcB])
            cx.op("dve", lambda e: e.memset(oml[:], 1.0), writes=[])
        else:
            cx.op("dve", lambda e: e.tensor_tensor(out=lb[:], in0=lbr[:, 0, :], in1=lbr[:, 1, :], op=ALU.subtract),
                  reads=[lbB], writes=[lbcB])
            cx.op("act", lambda e: e.activation(out=lb[:], in_=lb[:], func=AF.Exp), reads=[lbcB], writes=[lbcB])
            cx.op("dve", lambda e: e.tensor_scalar(out=lb[:], in0=lb[:], scalar1=1.0, scalar2=None, op0=ALU.add),
                  reads=[lbcB], writes=[lbcB])
            cx.op("dve", lambda e: e.reciprocal(out=lb[:], in_=lb[:]), reads=[lbcB], writes=[lbcB])
            cx.op("dve", lambda e: e.tensor_scalar(out=oml[:], in0=lb[:], scalar1=-1.0, scalar2=1.0, op0=ALU.mult, op1=ALU.add),
                  reads=[lbcB], writes=[])
        lbcB.w = {id(cx.esem["dve"]): (cx.esem["dve"], cx.ecnt["dve"])}

        NCH = 2 if NH % 2 == 0 else 1
        trc_ = [0]
        sci = [0, 0]

        def issue_in(j, tb, b):
            cx.dma("sp", lambda e, b=b, j=j, tb=tb: e.dma_start(out=inb[b][:], in_=pv[:, :, j, tb * 512:(tb + 1) * 512]),
                   inB[b], writes=[inB[b]])

        def block_gen(j, tb, b, nxt):
            po = b
            X = inb[b]
            if tb == 0:
                cx.op("dve", lambda e: e.memset(s32[b][:], 0.0), writes=[s32B[b]])
                cx.op("dve", lambda e, k=sci[b] % 2: e.memset(sbf[b][k][:], 0.0), writes=[sbfB[b][sci[b] % 2]])
            t0 = tb * 512
            cx.op("act", lambda e: e.activation(out=qs[b][:], in_=X[:, 0, :], func=AF.Silu), reads=[inB[b]], writes=[qsB[b]])
            cx.op("act", lambda e: e.activation(out=gate[b][:], in_=X[:, 3, :], func=AF.Silu), reads=[inB[b]], writes=[gateB[b]])
            cx.op("act", lambda e: e.activation(out=ee[b][:], in_=X[:, 1, :], func=AF.Exp, scale=-1.0), reads=[inB[b]], writes=[eeB[b]])
            cx.op("act", lambda e: e.activation(out=ib[b][:], in_=X[:, 2, :], func=AF.Copy), reads=[inB[b]], writes=[ibB[b]])
            cx.op("dve", lambda e: e.tensor_scalar(out=ee[b][:], in0=ee[b][:], scalar1=1.0, scalar2=None, op0=ALU.add),
                  reads=[eeB[b]], writes=[eeB[b]])
            cx.op("dve", lambda e: e.reciprocal(out=rr[b][:], in_=ee[b][:]), reads=[eeB[b]], writes=[rrB[b]])
            cx.op("dve", lambda e: e.tensor_scalar(out=ff[b][:], in0=rr[b][:], scalar1=oml[:, j:j + 1], scalar2=lb[:, j:j + 1],
                                                   op0=ALU.mult, op1=ALU.add), reads=[rrB[b], lbcB], writes=[ffB[b]])
            cx.op("pool", lambda e: e.tensor_scalar(out=kk[b][:], in0=ff[b][:], scalar1=-1.0, scalar2=1.0, op0=ALU.mult, op1=ALU.add),
                  reads=[ffB[b]], writes=[kkB[b]])
            yield
            cx.op("act", lambda e: e.activation(out=lf[b][:], in_=ff[b][:], func=AF.Ln), reads=[ffB[b]], writes=[lfB[b]])
            cx.op("dve", lambda e: e.tensor_tensor_scan(out=bc[b][:], data0=segm[:], data1=lf[b][:], initial=0.0,
                                                        op0=ALU.mult, op1=ALU.add), reads=[lfB[b], segB], writes=[bcB[b]])
            yield
            cx.op("act", lambda e: e.activation(out=eb[b][:], in_=bc[b][:], func=AF.Exp), reads=[bcB[b]], writes=[ebB[b]])
            cx.op("dve", lambda e: e.tensor_tensor(out=qtb[b][:], in0=qs[b][:], in1=eb[b][:], op=ALU.mult),
                  reads=[qsB[b], ebB[b]], writes=[qtbB[b]])
            yield
            cx.op("dve", lambda e: e.reciprocal(out=einv[b][:], in_=eb[b][:]), reads=[ebB[b]], writes=[einvB[b]])
            cx.op("dve", lambda e: e.tensor_tensor(out=ktb[b][:], in0=kk[b][:], in1=einv[b][:], op=ALU.mult),
                  reads=[kkB[b], einvB[b]], writes=[ktbB[b]])
            yield
            for c in range(16):
                cx.op("act", lambda e, c=c: e.activation(out=khb[b][:, 32 * c:32 * c + 32], in_=ktb[b][:, 32 * c:32 * c + 32],
                                                         func=AF.Copy, scale=eb[b][:, 32 * c + 31:32 * c + 32]),
                      reads=[ktbB[b], ebB[b]], writes=[khbB[b]] if c == 0 else [])
                if c == 7:
                    khbB[b].w = {id(cx.esem["act"]): (cx.esem["act"], cx.ecnt["act"])}
                    yield
            khbB[b].w = {id(cx.esem["act"]): (cx.esem["act"], cx.ecnt["act"])}
            yield
            tr = trc_[0] % 2
            trc_[0] += 1
            for i in range(4):
                cx.op("pe", lambda e, tr=tr, i=i: e.transpose(out=psTr[tr][:, i * 128:(i + 1) * 128],
                                                              in_=ib[b][:, i * 128:(i + 1) * 128], identity=ident[:]),
                      reads=[ibB[b], identB], writes=[psTrB[tr]])
            cx.op("act", lambda e, tr=tr: e.activation(out=vsb[b][:].rearrange("p a b -> p (a b)"), in_=psTr[tr][:, 0:512], func=AF.Copy),
                  reads=[psTrB[tr]], writes=[vsbB[b]])
            yield
            for (src_, srcB, dst, dstB) in ((ib[b], ibB[b], v32[b], v32B[b]), (khb[b], khbB[b], kh32[b], kh32B[b])):
                for h in range(2):
                    tr = trc_[0] % 2
                    trc_[0] += 1
                    for i in range(8):
                        cc = h * 8 + i
                        cx.op("pe", lambda e, tr=tr, i=i, cc=cc, src_=src_: e.transpose(
                            out=psTr[tr][0:32, i * 128:(i + 1) * 128], in_=src_[:, cc * 32:(cc + 1) * 32], identity=ident[:]),
                            reads=[srcB, identB], writes=[psTrB[tr]])
                    cx.op("act" if h == 0 else "dve", lambda e, tr=tr, dst=dst, h=h: (e.activation(
                        out=dst[:, h * 8:(h + 1) * 8, :].rearrange("p a b -> p (a b)"), in_=psTr[tr][0:32, :], func=AF.Copy) if h == 0 else
                        e.tensor_copy(out=dst[:, h * 8:(h + 1) * 8, :].rearrange("p a b -> p (a b)"), in_=psTr[tr][0:32, :])),
                        reads=[psTrB[tr]], writes=[dstB] if h == 0 else [])
                    if h == 1:
                        dstB.w[id(cx.esem["dve"])] = (cx.esem["dve"], cx.ecnt["dve"])
                    yield
            if nxt is not None:
                issue_in(nxt[0], nxt[1], b)
            yield
            for i in range(4):
                a = 0
                cs = slice(i * 128, (i + 1) * 128)
                cx.op("pe", lambda e, a=a, cs=cs: e.matmul(psA[a][:, 0:128], lhsT=ktb[b][:, cs], rhs=qtb[b][:, cs], start=True, stop=True),
                      reads=[ktbB[b], qtbB[b]], writes=[psAB[a]])
                cx.op("dve", lambda e, a=a: e.tensor_tensor(out=amb[a][:], in0=psA[a][:, 0:128], in1=gmask[:], op=ALU.mult),
                      reads=[psAB[a], gmB], writes=[ambB[a]])
                cx.op("pe", lambda e, a=a, i=i, cs=cs: e.matmul(psO[po][:, cs], lhsT=vsb[b][:, i, :], rhs=amb[a][:], start=True, stop=False),
                      reads=[vsbB[b], ambB[a]], writes=[psOB[po]])
                for c in range(4):
                    k = sci[b] % 2
                    c0 = i * 128 + 32 * c
                    cx.op("pe", lambda e, k=k, c0=c0: e.matmul(psO[po][:, c0:c0 + 32], lhsT=sbf[b][k][:], rhs=qtb[b][:, c0:c0 + 32],
                                                               start=False, stop=(c0 % 128 == 96)),
                          reads=[sbfB[b][k], qtbB[b]], writes=[psOB[po]])
                    cx.op("pe", lambda e, i=i, c=c: e.matmul(psS[b][:, 0:128], lhsT=kh32[b][:, 4 * i + c, :],
                                                             rhs=v32[b][:, 4 * i + c, :], start=True, stop=True),
                          reads=[kh32B[b], v32B[b]], writes=[psSB[b]])
                    sci[b] += 1
                    k2 = sci[b] % 2
                    cx.op("dve", lambda e, c0=c0, k2=k2: e.scalar_tensor_tensor(out=sbf[b][k2][:], in0=s32[b][:], scalar=eb[b][:, c0 + 31:c0 + 32],
                                                                               in1=psS[b][:, 0:128], op0=ALU.mult, op1=ALU.add),
                          reads=[s32B[b], ebB[b], psSB[b]], writes=[sbfB[b][k2]])
                    cx.op("dve", lambda e, c0=c0: e.scalar_tensor_tensor(out=s32[b][:], in0=s32[b][:], scalar=eb[b][:, c0 + 31:c0 + 32],
                                                                         in1=psS[b][:, 0:128], op0=ALU.mult, op1=ALU.add),
                          reads=[s32B[b], ebB[b], psSB[b]], writes=[s32B[b]])
                    yield
            yield
            cx.op("act", lambda e: e.activation(out=osb[b][:], in_=psO[po][:], func=AF.Copy), reads=[psOB[po]], writes=[osbB[b]])
            cx.op("act", lambda e: e.activation(out=osq[b][:], in_=psO[po][:], func=AF.Square), reads=[psOB[po]], writes=[osqB[b]])
            cx.op("pe", lambda e: e.matmul(psN[:], lhsT=ones_bf[:], rhs=osq[b][:], start=True, stop=True),
                  reads=[onesB, osqB[b]], writes=[psNB])
            cx.op("act", lambda e: e.activation(out=tmp[b][:], in_=psN[:], func=AF.Ln, scale=1.0 / 128, bias=eps_sb[:]),
                  reads=[psNB, epsB], writes=[tmpB[b]])
            cx.op("act", lambda e: e.activation(out=rstd[b][:], in_=tmp[b][:], func=AF.Exp, scale=-0.5), reads=[tmpB[b]], writes=[rstdB[b]])
            cx.op("dve", lambda e: e.tensor_tensor(out=osb[b][:], in0=osb[b][:], in1=rstd[b][:], op=ALU.mult),
                  reads=[osbB[b], rstdB[b]], writes=[osbB[b]])
            cx.op("dve", lambda e: e.scalar_tensor_tensor(out=ogb[b][:], in0=osb[b][:], scalar=hg[:, j:j + 1], in1=gate[b][:],
                                                          op0=ALU.mult, op1=ALU.mult),
                  reads=[osbB[b], hgB, gateB[b]], writes=[ogB[b]])
            cx.dma("sp", lambda e, j=j, t0=t0: e.dma_start(out=ogT[j * 128:(j + 1) * 128, t0:t0 + 512], in_=ogb[b][:]),
                   ogB[b], reads=[ogB[b]])


        def chain_gen(ch):
            hs = [j for j in range(NH) if j % NCH == ch]
            blks = [(j, tb) for j in hs for tb in range(NBLK)]
            issue_in(blks[0][0], blks[0][1], ch)
            for bi, (j, tb) in enumerate(blks):
                nxt = blks[bi + 1] if bi + 1 < len(blks) else None
                yield from block_gen(j, tb, ch, nxt)

        gens = [chain_gen(ch) for ch in range(NCH)]
        if NCH == 2:
            for _ in range(14):
                next(gens[0])
        while gens:
            for g in list(gens):
                try:
                    next(g)
                except StopIteration:
                    gens.remove(g)


def _bf3(x):
    x = np.float32(x)
    a = np.float32(np.asarray(x, np.float32).astype(NPBF))
    r = np.float32(x - a)
    b = np.float32(np.asarray(r, np.float32).astype(NPBF))
    c = np.float32(np.asarray(np.float32(r - b), np.float32).astype(NPBF))
    return a, b, c


def moba_tables(heads):
    NH = len(heads)
    H = 16
    slopes = np.array([2.0 ** (-8.0 * (h + 1) / H) for h in heads], np.float64)
    ALA = np.zeros((6, NH, 128), np.float32)
    ALB = np.zeros((6, NH, 256), np.float32)
    bt = np.zeros((128, NH, 16), np.float32)
    for j in range(NH):
        cs = _bf3(slopes[j] / ATT_SCALE)
        for i in range(3):
            ALA[i, j, :] = np.arange(128)
            ALB[i, j, :] = cs[i]
            ALA[3 + i, j, :] = -cs[i]
            ALB[3 + i, j, :] = np.arange(256)
        for d in range(16):
            bt[:, j, d] = -slopes[j] * 128.0 * (d - 1)
    return ALA.astype(NPBF), ALB.astype(NPBF), bt


def emit_moba(cx, qT, kvT, qgT, kgT, alaT, albT, btT, ogT, NH, S, consts, tag="moba"):
    nc = cx.nc
    NT = S // 128
    NQB = S // 256
    assert NQB == 8
    ones_bf, onesB = consts["ones_bf"]
    ident, identB = consts["ident_bf"]
    eps_sb, epsB = consts["eps"]
    pastm, pastB = consts["pastmask"]
    E_sb, EB = consts["E"]
    CM0, CMB = consts["CM0"]
    with cx.scope() as sc:
        raw = [sc.sb(f"raw{i}", [128, S], F32) for i in range(2)]
        rawB = [cx.dmabuf(f"{tag}_raw{i}") for i in range(2)]
        sq = sc.sb("sq", [128, S], BF16); sqB = Buf("sq")
        tmp = sc.sb("tmp", [128, 512], F32); tmpB = Buf("tmp")
        rstd = sc.sb("rstd", [128, S], F32); rstdB = Buf("rstd")
        kn32 = sc.sb("kn32", [128, S], F32); kn32B = Buf("kn32")
        knb = sc.sb("knb", [128, S], BF16); knbB = Buf("knb")
        qnb = sc.sb("qnb", [128, S], BF16); qnbB = Buf("qnb")
        vb = sc.sb("vb", [128, S], BF16); vbB = Buf("vb")
        vsb = sc.sb("vsb", [128, NT, 128], BF16); vsbB = Buf("vsb")
        kms = sc.sb("kms", [128, 8], F32); kmsB = Buf("kms")
        kmb = sc.sb("kmb", [128, 8], BF16); kmbB = Buf("kmb")
        gm = sc.sb("gm", [128, NT, 8], F32); gmB = Buf("gm")
        top8 = sc.sb("top8", [128, NT, 8], F32); top8B = Buf("top8")
        bb = sc.sb("bb", [128, NT, 8], BF16); bbB = Buf("bb")
        bbT = sc.sb("bbT", [8, S], BF16); bbTB = Buf("bbT")
        NSB = 3
        pT = [sc.sb(f"pT{i}", [128, 256], BF16) for i in range(NSB)]
        pTB = [Buf(f"pT{i}") for i in range(NSB)]
        rec = sc.sb("rec", [128, 256], F32); recB = Buf("rec")
        ogb = [sc.sb(f"ogb{i}", [128, 256], BF16) for i in range(2)]
        ogB = [cx.dmabuf(f"{tag}_og{i}") for i in range(2)]
        qg = sc.sb("qg", [128, 1], F32); kg = sc.sb("kg", [128, 1], F32)
        ala = sc.sb("ala", [6, NH, 128], BF16); alb = sc.sb("alb", [6, NH, 256], BF16)
        bt = sc.sb("bt", [128, NH, 16], F32)
        tabB = cx.dmabuf(f"{tag}_tab")
        for (t_, d_) in ((qg, qgT), (kg, kgT), (ala, alaT), (alb, albT), (bt, btT)):
            cx.dma("sp", lambda e, t_=t_, d_=d_: e.dma_start(out=t_[:], in_=d_), tabB, writes=[tabB])
        psN = sc.ps("psN", [128, 512], F32); psNB = Buf("psN")
        NTR = 1
        psTr = [sc.ps(f"psTr{i}", [128, 1024], BF16) for i in range(NTR)]
        psTrB = [Buf(f"psTr{i}") for i in range(NTR)]
        psG = sc.ps("psG", [128, 512], F32); psGB = Buf("psG")
        psS = [sc.ps(f"psS{i}", [128, 512], F32) for i in range(NSB)]
        psSB = [Buf(f"psS{i}") for i in range(NSB)]
        psO = sc.ps("psO", [128, 512], F32); psOB = Buf("psO")
        psR = sc.ps("psR", [128, 512], F32); psRB = Buf("psR")

        rc = 0
        trc = 0
        sc_i = 0
        oc = 0

        def load_raw(src_rows):
            nonlocal rc
            r = rc % 2
            rc += 1
            cx.dma("sp", lambda e, r=r: e.dma_start(out=raw[r][:], in_=src_rows), rawB[r], writes=[rawB[r]])
            return r

        def rms_rstd(r):
            for n in range(S // 512):
                cs = slice(n * 512, (n + 1) * 512)
                cx.op("act", lambda e, cs=cs: e.activation(out=sq[:, cs], in_=raw[r][:, cs], func=AF.Square),
                      reads=[rawB[r]], writes=[sqB] if n == 0 else [])
                sqB.w = {id(cx.esem["act"]): (cx.esem["act"], cx.ecnt["act"])}
                cx.op("pe", lambda e, cs=cs: e.matmul(psN[:], lhsT=ones_bf[:], rhs=sq[:, cs], start=True, stop=True),
                      reads=[sqB, onesB], writes=[psNB])
                cx.op("act", lambda e: e.activation(out=tmp[:], in_=psN[:], func=AF.Ln, scale=1.0 / 128, bias=eps_sb[:]),
                      reads=[psNB, epsB], writes=[tmpB])
                cx.op("act", lambda e, cs=cs: e.activation(out=rstd[:, cs], in_=tmp[:], func=AF.Exp, scale=-0.5),
                      reads=[tmpB], writes=[rstdB] if n == 0 else [])
                rstdB.w = {id(cx.esem["act"]): (cx.esem["act"], cx.ecnt["act"])}

        for j in range(NH):
            r = load_raw(kvT[j * 128:(j + 1) * 128, :])
            rms_rstd(r)
            cx.op("dve", lambda e: e.scalar_tensor_tensor(out=kn32[:], in0=raw[r][:], scalar=kg[:, 0:1], in1=rstd[:],
                                                          op0=ALU.mult, op1=ALU.mult),
                  reads=[rawB[r], tabB, rstdB], writes=[kn32B])
            cx.op("pool", lambda e: e.tensor_copy(out=knb[:], in_=kn32[:]), reads=[kn32B], writes=[knbB])
            cx.op("dve", lambda e: e.tensor_reduce(out=kms[:], in_=kn32[:].rearrange("p (n k) -> p n k", k=256),
                                                   axis=AX.X, op=ALU.add), reads=[kn32B], writes=[kmsB])
            cx.op("dve", lambda e: e.tensor_scalar(out=kmb[:], in0=kms[:], scalar1=1.0 / 256, scalar2=None, op0=ALU.mult),
                  reads=[kmsB], writes=[kmbB])
            r = load_raw(kvT[(NH + j) * 128:(NH + j + 1) * 128, :])
            cx.op("act", lambda e: e.activation(out=vb[:], in_=raw[r][:], func=AF.Copy), reads=[rawB[r]], writes=[vbB])
            for h in range(NT // 8):
                tr = trc % NTR
                trc += 1
                for i in range(8):
                    tt = h * 8 + i
                    cx.op("pe", lambda e, tr=tr, i=i, tt=tt: e.transpose(out=psTr[tr][:, i * 128:(i + 1) * 128],
                                                                        in_=vb[:, tt * 128:(tt + 1) * 128], identity=ident[:]),
                          reads=[vbB, identB], writes=[psTrB[tr]])
                cx.op("dve", lambda e, tr=tr, h=h: e.tensor_copy(out=vsb[:, h * 8:(h + 1) * 8, :].rearrange("p a b -> p (a b)"),
                                                                 in_=psTr[tr][:, :]),
                      reads=[psTrB[tr]], writes=[vsbB] if h == 0 else [])
                vsbB.w = {id(cx.esem["dve"]): (cx.esem["dve"], cx.ecnt["dve"])}
            r = load_raw(qT[j * 128:(j + 1) * 128, :])
            rms_rstd(r)
            cx.op("dve", lambda e: e.scalar_tensor_tensor(out=qnb[:], in0=raw[r][:], scalar=qg[:, 0:1], in1=rstd[:],
                                                          op0=ALU.mult, op1=ALU.mult),
                  reads=[rawB[r], tabB, rstdB], writes=[qnbB])
            for tt in range(NT):
                cx.op("pe", lambda e, tt=tt: e.matmul(psG[:, tt * 8:(tt + 1) * 8], lhsT=qnb[:, tt * 128:(tt + 1) * 128], rhs=kmb[:],
                                                      start=True, stop=True),
                      reads=[qnbB, kmbB], writes=[psGB])
            cx.op("dve", lambda e: e.tensor_tensor(out=gm[:].rearrange("p a b -> p (a b)"), in0=psG[:, 0:NT * 8],
                                                   in1=pastm[:].rearrange("p a b -> p (a b)"), op=ALU.add),
                  reads=[psGB, pastB], writes=[gmB])
            for tt in range(NT):
                cx.op("dve", lambda e, tt=tt: e.max(out=top8[:, tt, :], in_=gm[:, tt, :]), reads=[gmB],
                      writes=[top8B] if tt == 0 else [])
            top8B.w = {id(cx.esem["dve"]): (cx.esem["dve"], cx.ecnt["dve"])}
            for tt in range(NT):
                cx.op("dve", lambda e, tt=tt: e.tensor_scalar(out=bb[:, tt, :], in0=gm[:, tt, :], scalar1=top8[:, tt, 2:3],
                                                              scalar2=BIGNEG, op0=ALU.is_lt, op1=ALU.mult),
                      reads=[gmB, top8B], writes=[bbB] if tt == 0 else [])
            bbB.w = {id(cx.esem["dve"]): (cx.esem["dve"], cx.ecnt["dve"])}
            for h in range(NT // 8):
                tr = trc % NTR
                trc += 1
                for i in range(8):
                    tt = h * 8 + i
                    cx.op("pe", lambda e, tr=tr, i=i, tt=tt: e.transpose(out=psTr[tr][0:8, i * 128:(i + 1) * 128],
                                                                        in_=bb[:, tt, :], identity=ident[:]),
                          reads=[bbB, identB], writes=[psTrB[tr]])
                cx.op("dve", lambda e, tr=tr, h=h: e.tensor_copy(out=bbT[:, h * 1024:(h + 1) * 1024], in_=psTr[tr][0:8, :]),
                      reads=[psTrB[tr]], writes=[bbTB] if h == 0 else [])
                bbTB.w = {id(cx.esem["dve"]): (cx.esem["dve"], cx.ecnt["dve"])}
            items = []
            for qb in range(NQB):
                subs = [(2 * qb, 0, 256, True), (2 * qb + 1, 128, 128, True)] + [(ks, 0, 256, False) for ks in range(2 * qb)]
                for si, sub in enumerate(subs):
                    items.append((qb, si, len(subs)) + sub)

            def emit_scores(idx):
                qb, si, nsub, ks, lo, N, own = items[idx]
                s_ = idx % NSB
                q0 = qb * 256
                qc = slice(q0 + lo, q0 + lo + N)
                cx.op("pe", lambda e, s_=s_, ks=ks, qc=qc, N=N: e.matmul(psS[s_][:, 0:N], lhsT=knb[:, ks * 128:(ks + 1) * 128],
                                                                         rhs=qnb[:, qc], start=True, stop=False),
                      reads=[knbB, qnbB], writes=[psSB[s_]])
                cx.op("pe", lambda e, s_=s_, lo=lo, N=N: e.matmul(psS[s_][:, 0:N], lhsT=ala[0:6, j, :], rhs=alb[0:6, j, lo:lo + N],
                                                                  start=False, stop=False),
                      reads=[tabB], writes=[psSB[s_]])
                if own:
                    cx.op("pe", lambda e, s_=s_, N=N: e.matmul(psS[s_][:, 0:N], lhsT=ident[:], rhs=CM0[:, 0:N],
                                                               start=False, stop=True),
                          reads=[identB, CMB], writes=[psSB[s_]])
                else:
                    n = ks // 2
                    cx.op("pe", lambda e, s_=s_, n=n, qc=qc, N=N: e.matmul(psS[s_][:, 0:N], lhsT=E_sb[0:8, n, :], rhs=bbT[0:8, qc],
                                                                           start=False, stop=True),
                          reads=[EB, bbTB], writes=[psSB[s_]])
                didx = 2 * qb - ks + 1
                cx.op("act", lambda e, s_=s_, N=N, didx=didx: e.activation(out=pT[s_][:, 0:N], in_=psS[s_][:, 0:N], func=AF.Exp,
                                                                           scale=ATT_SCALE, bias=bt[:, j, didx:didx + 1]),
                      reads=[psSB[s_], tabB], writes=[pTB[s_]])

            def emit_pv(idx):
                nonlocal oc
                qb, si, nsub, ks, lo, N, own = items[idx]
                s_ = idx % NSB
                q0 = qb * 256
                last = (si == nsub - 1)
                cx.op("pe", lambda e, s_=s_, ks=ks, lo=lo, N=N, si=si, last=last: e.matmul(
                    psO[:, lo:lo + N], lhsT=vsb[:, ks, :], rhs=pT[s_][:, 0:N], start=(si == 0), stop=last),
                    reads=[vsbB, pTB[s_]], writes=[psOB])
                cx.op("pe", lambda e, s_=s_, lo=lo, N=N, si=si, last=last: e.matmul(
                    psR[:, lo:lo + N], lhsT=ones_bf[:], rhs=pT[s_][:, 0:N], start=(si == 0), stop=last),
                    reads=[onesB, pTB[s_]], writes=[psRB])
                if last:
                    o_ = oc % 2
                    oc += 1
                    cx.op("dve", lambda e: e.reciprocal(out=rec[:], in_=psR[:, 0:256]), reads=[psRB], writes=[recB])
                    cx.op("dve", lambda e, o_=o_: e.tensor_tensor(out=ogb[o_][:], in0=psO[:, 0:256], in1=rec[:], op=ALU.mult),
                          reads=[psOB, recB], writes=[ogB[o_]])
                    cx.dma("sp", lambda e, o_=o_, q0=q0: e.dma_start(out=ogT[j * 128:(j + 1) * 128, q0:q0 + 256], in_=ogb[o_][:]),
                           ogB[o_], reads=[ogB[o_]])

            LOOK = NSB - 1
            for idx in range(min(LOOK, len(items))):
                emit_scores(idx)
            for idx in range(len(items)):
                if idx + LOOK < len(items):
                    emit_scores(idx + LOOK)
                emit_pv(idx)

from concourse.bass_utils import run_bass_kernel_spmd

D_MODEL = 2048
SEQ = 2048
BATCH = 4
NHL = 8
D_FF = 8192
TOWN = 1024
NCORES = 8
_PROGS = {}


_DECL = {}


def _dram(nc, name, shape, dt, kind="ExternalInput"):
    if kind == "ExternalInput":
        _DECL[name] = (tuple(shape), dt)
    return nc.dram_tensor(name, list(shape), dt, kind=kind).ap()


def build_A(layer):
    nc = bass.Bass("TRN2", target_bir_lowering=False)
    cx = Ctx(nc)
    consts = setup_consts(cx)
    hT = _dram(nc, "hT", [D_MODEL, SEQ], F32)
    Wt = _dram(nc, "Wt", [4 * NHL, 128, D_MODEL], F32)
    gT = _dram(nc, "gT", [128, 16], F32)
    lbT = _dram(nc, "lbT", [128, 2, NHL], F32)
    hgT = _dram(nc, "hgT", [128, NHL], F32)
    projT = _dram(nc, "projT", [4 * NHL * 128, SEQ], F32, "Internal")
    ogT = _dram(nc, "ogT", [NHL * 128, SEQ], BF16, "ExternalOutput")
    emit_linear(cx, hT, Wt, projT, SEQ, D_MODEL, 4 * NHL * 128, F32, F32, consts, gainT=gT, tag="win")
    emit_gla(cx, projT, lbT, hgT, ogT, NHL, SEQ, layer, consts)
    cx.finish()
    return nc


def build_C(with_kv):
    nc = bass.Bass("TRN2", target_bir_lowering=False)
    cx = Ctx(nc)
    consts = setup_consts(cx)
    hT = _dram(nc, "hT", [D_MODEL, SEQ], F32)
    Wq = _dram(nc, "Wq", [NHL, 128, D_MODEL], F32)
    gT = _dram(nc, "gT", [128, 16], F32)
    qgT = _dram(nc, "qgT", [128, 1], F32)
    kgT = _dram(nc, "kgT", [128, 1], F32)
    alaT = _dram(nc, "alaT", [6, NHL, 128], BF16)
    albT = _dram(nc, "albT", [6, NHL, 256], BF16)
    btT = _dram(nc, "btT", [128, NHL, 16], F32)
    qT = _dram(nc, "qT", [NHL * 128, SEQ], F32, "Internal")
    ogT = _dram(nc, "ogT", [NHL * 128, SEQ], BF16, "ExternalOutput")
    if with_kv:
        Wkv = _dram(nc, "Wkv", [2 * NHL, 128, D_MODEL], F32)
        gkvT = _dram(nc, "gkvT", [128, 16], F32)
        kvT = _dram(nc, "kvT", [2 * NHL * 128, SEQ], F32, "ExternalOutput")
        emit_linear(cx, hT, Wkv, kvT, SEQ, D_MODEL, 2 * NHL * 128, F32, F32, consts, gainT=gkvT, tag="wkv")
    else:
        kvT = _dram(nc, "kvT", [2 * NHL * 128, SEQ], F32)
    emit_linear(cx, hT, Wq, qT, SEQ, D_MODEL, NHL * 128, F32, F32, consts, gainT=gT, tag="wq")
    emit_moba(cx, qT, kvT, qgT, kgT, alaT, albT, btT, ogT, NHL, SEQ, consts)
    cx.finish()
    return nc


def build_B():
    nc = bass.Bass("TRN2", target_bir_lowering=False)
    cx = Ctx(nc)
    consts = setup_consts(cx)
    ogT = _dram(nc, "ogT", [D_MODEL, TOWN], BF16)
    hT = _dram(nc, "hT", [D_MODEL, TOWN], F32)
    Wo = _dram(nc, "Wo", [16, 128, D_MODEL], F32)
    gT = _dram(nc, "gT", [128, 16], F32)
    W1 = _dram(nc, "W1", [64, 128, D_MODEL], F32)
    W2 = _dram(nc, "W2", [16, 128, D_FF], F32)
    h1T = _dram(nc, "h1T", [D_MODEL, TOWN], F32, "Internal")
    aT = _dram(nc, "aT", [D_FF, TOWN], BF16, "Internal")
    h2T = _dram(nc, "h2T", [D_MODEL, TOWN], F32, "ExternalOutput")
    emit_linear(cx, ogT, Wo, h1T, TOWN, D_MODEL, D_MODEL, BF16, F32, consts, resT=hT, tag="wo")
    emit_linear(cx, h1T, W1, aT, TOWN, D_MODEL, D_FF, F32, BF16, consts, gainT=gT, act="relu2", tag="w1")
    emit_linear(cx, aT, W2, h2T, TOWN, D_FF, D_MODEL, BF16, F32, consts, resT=h1T, tag="w2")
    cx.finish()
    return nc


PAIRS = [[0, 1], [2, 3], [4, 5], [6, 7]]


def build_fused(layers=(0, 1, 2, 3), info=None):
    nc = bass.Bass("TRN2", target_bir_lowering=False)
    cx = Ctx(nc)
    consts = setup_consts(cx)
    I = lambda n, s, dt=F32: _dram(nc, n, s, dt)
    N = lambda n, s, dt=F32: _dram(nc, n, s, dt, "Internal")
    rsel = I("rsel", [128, 2])
    hall0 = I("hall0", [2 * D_MODEL, TOWN])
    hT0 = I("hT0", [D_MODEL, TOWN])
    hall = N("hall", [2 * D_MODEL, TOWN])
    ogT = N("ogT", [NHL * 128, SEQ], BF16)
    ogall = N("ogall", [2 * NHL * 128, SEQ], BF16)
    projT = N("projT", [4 * NHL * 128, SEQ])
    qT = N("qT", [NHL * 128, SEQ])
    kvT = N("kvT", [2 * NHL * 128, SEQ])
    h1T = N("h1T", [D_MODEL, TOWN])
    aT = N("aT", [D_FF, TOWN], BF16)
    hbuf = [N("hA", [D_MODEL, TOWN]), N("hB", [D_MODEL, TOWN])]
    hout = _dram(nc, "hout", [D_MODEL, TOWN], F32, "ExternalOutput")
    agB = cx.dmabuf("ag")
    qgT = [None] * 2
    ogv = ogall.rearrange("(i r cc p) t -> r i p cc t", i=4, r=2, cc=2, p=128)

    def ogsrc(off):
        def f(c0, c1, t0, n):
            assert c0 == 0 and c1 == 16
            return [(8 * rr + 2 * i, 8 * rr + 2 * i + 2, ogv[rr, i, :, :, off + t0:off + t0 + n])
                    for rr in range(2) for i in range(4)]
        return f
    ogA = ogsrc(0)
    ogBf = ogsrc(TOWN)
    kgT = I("kgT", [128, 1])
    alaT = I("alaT", [6, NHL, 128], BF16)
    albT = I("albT", [6, NHL, 256], BF16)
    btT = I("btT", [128, NHL, 16])
    lbT = I("lbT", [128, 2, NHL])
    for l in layers:
        if l != layers[0]:
            cx.renew_engine_sems()
        h_cur = hT0 if l == layers[0] else hbuf[(l - 1) % 2]
        if l == layers[0]:
            src_all = hall0
        else:
            for i in range(8):
                cx.coll(lambda e, h_cur=h_cur, i=i: e.collective_compute(
                    "AllGather", ALU.bypass, replica_groups=PAIRS,
                    ins=[h_cur[256 * i:256 * (i + 1), :].opt()], outs=[hall[512 * i:512 * (i + 1), :].opt()]), agB)
            cx.barrier_on(agB)
            src_all = hall
        if l == layers[0]:
            sv0 = src_all.rearrange("(r c p) t -> r p c t", r=2, p=128)
            xs = lambda c0, c1, t0, n, sv0=sv0: [(c0, c1, sv0[t0 // TOWN, :, c0:c1, (t0 % TOWN):(t0 % TOWN) + n])]
        else:
            sv = src_all.rearrange("(i r cc p) t -> r i p cc t", i=8, r=2, cc=2, p=128)
            xs = lambda c0, c1, t0, n, sv=sv: [(2 * i, 2 * i + 2, sv[t0 // TOWN, i, :, :, (t0 % TOWN):(t0 % TOWN) + n])
                                               for i in range(c0 // 2, c1 // 2)]
        gT = I(f"gmix{l}", [128, 16])
        if l < 2:
            Wt = I(f"Win{l}", [4 * NHL, 128, D_MODEL])
            hgT = I(f"hg{l}", [128, NHL])
            emit_linear(cx, None, Wt, projT, SEQ, D_MODEL, 4 * NHL * 128, F32, F32, consts, gainT=gT, tag=f"win{l}", xsrc=xs)
            emit_gla(cx, projT, lbT, hgT, ogT, NHL, SEQ, l, consts, tag=f"gla{l}")
        else:
            Wq = I(f"Wq{l}", [NHL, 128, D_MODEL])
            qg = I(f"qg{l}", [128, 1])
            if l == 2:
                Wkv = I("Wkv", [2 * NHL, 128, D_MODEL])
                gkvT = I("gkv", [128, 16])
                emit_linear(cx, None, Wkv, kvT, SEQ, D_MODEL, 2 * NHL * 128, F32, F32, consts, gainT=gkvT, tag="wkv", xsrc=xs)
            emit_linear(cx, None, Wq, qT, SEQ, D_MODEL, NHL * 128, F32, F32, consts, gainT=gT, tag=f"wq{l}", xsrc=xs)
            emit_moba(cx, qT, kvT, qg, kgT, alaT, albT, btT, ogT, NHL, SEQ, consts, tag=f"moba{l}")
        for i in range(4):
            cx.coll(lambda e, i=i: e.collective_compute(
                "AllGather", ALU.bypass, replica_groups=PAIRS,
                ins=[ogT[256 * i:256 * (i + 1), :].opt()], outs=[ogall[512 * i:512 * (i + 1), :].opt()]), agB)
        cx.barrier_on(agB)
        Wo = I(f"Wo{l}", [16, 128, D_MODEL])
        g2 = I(f"gmlp{l}", [128, 16])
        W1 = I(f"W1_{l}", [64, 128, D_MODEL])
        W2 = I(f"W2_{l}", [16, 128, D_FF])
        h_next = hout if l == layers[-1] else hbuf[l % 2]
        emit_linear(cx, None, Wo, h1T, TOWN, D_MODEL, D_MODEL, BF16, F32, consts, resT=h_cur, tag=f"wo{l}",
                    xsrc=ogA, xsrcB=ogBf, selT=rsel)
        emit_linear(cx, h1T, W1, aT, TOWN, D_MODEL, D_FF, F32, BF16, consts, gainT=g2, act="relu2", tag=f"w1{l}")
        emit_linear(cx, aT, W2, h_next, TOWN, D_FF, D_MODEL, BF16, F32, consts, resT=h1T, tag=f"w2{l}", TBLK=1024, NW=2)
    if info is not None:
        info['nsem'] = cx.nsem
        info['ecnt'] = dict(cx.ecnt)
        info['maxcnt'] = getattr(cx, 'maxcnt', 0)
        info['maxdma'] = max([v for (_, v) in cx.final] + [b.dval for b in cx.alldma])
    cx.finish()
    return nc


def _head_cols(r, nparts):
    cols = []
    for a in range(nparts):
        for j in range(NHL):
            h = NHL * r + j
            cols.append(np.arange(a * D_MODEL + h * 128, a * D_MODEL + (h + 1) * 128))
    return np.concatenate(cols)


def kernel(x, a_norm, a_w_in, a_head_norm, a_w_out, lower_bounds, kv_norm, w_kv, k_norm,
           b_norm, b_w_q, b_q_norm, b_w_o, mlp_norm, mlp_w1, mlp_w2):
    f32 = lambda a: np.asarray(a, dtype=np.float32)
    x = f32(x)
    shared = dict(const_inputs())
    shared["kgT"] = f32(k_norm)[:, None].copy()
    for l in range(4):
        shared[f"gmlp{l}"] = fm(f32(mlp_norm[l]))
        shared[f"W1_{l}"] = tile_w(f32(mlp_w1[l]))
        shared[f"W2_{l}"] = tile_w(f32(mlp_w2[l]))
        shared[f"Wo{l}"] = tile_w(f32(a_w_out[l] if l < 2 else b_w_o[l - 2]))
        shared[f"gmix{l}"] = fm(f32(a_norm[l] if l < 2 else b_norm[l - 2]))
        if l >= 2:
            shared[f"qg{l}"] = f32(b_q_norm[l - 2])[:, None].copy()
    shared["gkv"] = fm(f32(kv_norm))
    per_rank = []
    for r in range(2):
        d = {}
        hs = slice(NHL * r * 128, NHL * (r + 1) * 128)
        for l in range(2):
            d[f"Win{l}"] = tile_w(f32(a_w_in[l])[:, _head_cols(r, 4)])
            d[f"hg{l}"] = np.ascontiguousarray(f32(a_head_norm[l])[hs].reshape(NHL, 128).T)
        d["lbT"] = np.ascontiguousarray(f32(lower_bounds)[:, hs].reshape(2, NHL, 128).transpose(2, 0, 1))
        for l in (2, 3):
            d[f"Wq{l}"] = tile_w(f32(b_w_q[l - 2])[:, _head_cols(r, 1)])
        d["Wkv"] = tile_w(f32(w_kv)[:, _head_cols(r, 2)])
        ala, alb, bt = moba_tables(list(range(NHL * r, NHL * (r + 1))))
        d["alaT"], d["albT"], d["btT"] = ala, alb, bt
        rs = np.zeros((128, 2), np.float32)
        rs[:, r] = 1.0
        d["rsel"] = rs
        per_rank.append(d)
    maps = []
    for c in range(NCORES):
        b, r = divmod(c, 2)
        xb = x[b]
        hall0 = np.ascontiguousarray(xb.reshape(2, TOWN, D_MODEL).transpose(0, 2, 1)).reshape(2 * D_MODEL, TOWN)
        m = dict(shared)
        m.update(per_rank[r])
        m["hall0"] = hall0
        m["hT0"] = np.ascontiguousarray(hall0[r * D_MODEL:(r + 1) * D_MODEL])
        maps.append(m)
    nc = build_fused()
    res = run_bass_kernel_spmd(nc, maps, core_ids=list(range(NCORES))).results
    out = np.empty((BATCH, SEQ, D_MODEL), np.float32)
    for c in range(NCORES):
        b, r = divmod(c, 2)
        out[b, r * TOWN:(r + 1) * TOWN, :] = np.asarray(res[c]["hout"]).T
    return out
```

```python
from contextlib import ExitStack
import numpy as np
import concourse.bass as bass
import concourse.mybir as mybir

F32 = mybir.dt.float32
BF16 = mybir.dt.bfloat16
AF = mybir.ActivationFunctionType
ALU = mybir.AluOpType
AX = mybir.AxisListType

ENGS = ("pe", "act", "dve", "pool", "sp")


class Buf:
    __slots__ = ("name", "w", "r", "dsem", "dval")

    def __init__(self, name):
        self.name = name
        self.w = {}
        self.r = {}
        self.dsem = None
        self.dval = 0


class Scope:
    uid = 0

    def __init__(self, cx):
        self.cx = cx
        self.es = ExitStack()

    def __enter__(self):
        self.mark = len(self.cx.scope_bufs)
        return self

    def __exit__(self, *a):
        self.cx.barrier()
        for b in self.cx.scope_bufs[self.mark:]:
            self.cx.sempool.append((b.dsem, b.dval))
            self.cx.alldma.remove(b)
            self.cx.final.append((b.dsem, b.dval))
        del self.cx.scope_bufs[self.mark:]
        self.es.close()
        return False

    def sb(self, name, shape, dt):
        Scope.uid += 1
        return self.es.enter_context(self.cx.nc.sbuf_tensor(f"{name}_{Scope.uid}", list(shape), dt))

    def ps(self, name, shape, dt):
        Scope.uid += 1
        return self.es.enter_context(self.cx.nc.psum_tensor(f"{name}_{Scope.uid}", list(shape), dt))


class Ctx:
    def __init__(self, nc):
        self.nc = nc
        self.es = ExitStack()
        self.eng = {"pe": nc.tensor, "act": nc.scalar, "dve": nc.vector, "pool": nc.gpsimd, "sp": nc.sync}
        self.esem = {}
        self.ecnt = {e: 0 for e in ENGS}
        for e in ENGS:
            self.esem[e] = self.es.enter_context(nc.semaphore("es_" + e))
        self.known = {e: {} for e in ENGS}
        self.nsem = 0
        self.final = []
        self.alldma = []
        self.sempool = []
        self.scope_bufs = []

    def scope(self):
        return Scope(self)

    def barrier(self):
        deps = {}
        for e in ENGS:
            if self.ecnt[e] > 0:
                deps[id(self.esem[e])] = (self.esem[e], self.ecnt[e])
        for b in self.alldma:
            if b.dval > 0:
                deps[id(b.dsem)] = (b.dsem, b.dval)
        for e in ENGS:
            self._emit_waits(e, deps)

    def newsem(self, name):
        self.nsem += 1
        return self.es.enter_context(self.nc.semaphore(name))

    def dmabuf(self, name):
        b = Buf(name)
        if self.sempool:
            b.dsem, b.dval = self.sempool.pop()
        else:
            b.dsem = self.newsem("d_%d" % self.nsem)
        self.alldma.append(b)
        self.scope_bufs.append(b)
        return b

    def _collect(self, reads, writes):
        deps = {}
        for b in reads:
            for k, (s, v) in b.w.items():
                if k not in deps or deps[k][1] < v:
                    deps[k] = (s, v)
        for b in writes:
            for d in (b.w, b.r):
                for k, (s, v) in d.items():
                    if k not in deps or deps[k][1] < v:
                        deps[k] = (s, v)
        return deps

    def _emit_waits(self, e, deps, skip_self=False):
        kn = self.known[e]
        for k, (s, v) in deps.items():
            if skip_self and k == id(self.esem[e]):
                continue
            if kn.get(k, 0) >= v:
                continue
            kn[k] = v
            self.eng[e].wait_ge(s, v)

    def op(self, e, fn, reads=(), writes=()):
        deps = self._collect(reads, writes)
        self._emit_waits(e, deps, skip_self=(e == "pe"))
        self.ecnt[e] += 1
        tok = (self.esem[e], self.ecnt[e])
        k = id(self.esem[e])
        fn(self.eng[e]).then_inc(self.esem[e], 1)
        for b in reads:
            b.r[k] = tok
        for b in writes:
            b.w = {k: tok}
            b.r = {}

    def dma(self, q, fn, owner, reads=(), writes=(), serial=True):
        deps = self._collect(reads, writes)
        if owner.dval > 0 and serial:
            deps[id(owner.dsem)] = (owner.dsem, max(owner.dval, deps.get(id(owner.dsem), (None, 0))[1]))
        self._emit_waits(q, deps)
        owner.dval += 16
        tok = (owner.dsem, owner.dval)
        k = id(owner.dsem)
        fn(self.eng[q]).then_inc(owner.dsem, 16)
        for b in reads:
            b.r[k] = tok
        for b in writes:
            b.w = {k: tok}
            b.r = {}
        return tok

    def renew_engine_sems(self):
        self.barrier()
        for e in ENGS:
            self.maxcnt = max(getattr(self, "maxcnt", 0), self.ecnt[e])
            if self.ecnt[e] > 0:
                self.esem[e] = self.es.enter_context(self.nc.semaphore("es%d_%s" % (self.nsem, e)))
                self.nsem += 1
                self.ecnt[e] = 0

    def barrier_on(self, owner):
        deps = {id(owner.dsem): (owner.dsem, owner.dval)}
        for e in ENGS:
            self._emit_waits(e, deps)

    def coll(self, fn, owner, serial=True):
        deps = {}
        if owner.dval > 0 and serial:
            deps[id(owner.dsem)] = (owner.dsem, owner.dval)
        self._emit_waits("pool", deps)
        owner.dval += 1
        fn(self.eng["pool"]).then_inc(owner.dsem)

    def dma_multi(self, q, fn, owner, reads=(), writes_acc=()):
        deps = self._collect(reads, ())
        for b in writes_acc:
            for kk, (s, v) in b.r.items():
                if kk not in deps or deps[kk][1] < v:
                    deps[kk] = (s, v)
        if owner.dval > 0:
            deps[id(owner.dsem)] = (owner.dsem, max(owner.dval, deps.get(id(owner.dsem), (None, 0))[1]))
        self._emit_waits(q, deps)
        owner.dval += 16
        tok = (owner.dsem, owner.dval)
        k = id(owner.dsem)
        fn(self.eng[q]).then_inc(owner.dsem, 16)
        for b in reads:
            b.r[k] = tok
        for b in writes_acc:
            b.w[k] = tok
        return tok

    def finish(self):
        deps = {}
        for e in ENGS:
            if self.ecnt[e] > 0:
                deps[id(self.esem[e])] = (self.esem[e], self.ecnt[e])
        for b in self.alldma:
            if b.dval > 0:
                deps[id(b.dsem)] = (b.dsem, b.dval)
        for (s_, v) in self.final:
            if id(s_) not in deps or deps[id(s_)][1] < v:
                deps[id(s_)] = (s_, v)
        for k, (s, v) in deps.items():
            self.eng["sp"].wait_ge(s, v)
        self.es.close()


EPS = 1e-6


def _load_pieces(cx, pieces, dst_of, owner, buf):
    first = True
    for (cs, ce, sap) in pieces:
        cx.dma("sp", lambda e, dst=dst_of(cs, ce), sap=sap: e.dma_start(out=dst, in_=sap), owner,
               writes=[buf] if first else [], serial=first)
        first = False
    buf.w = {id(owner.dsem): (owner.dsem, owner.dval)}
    buf.r = {}


def emit_linear(cx, xT, Wt, outT, T, Fin, Fout, x_dt, out_dt, consts, gainT=None, resT=None, act=None,
                TBLK=None, tag="lin", xsrc=None, xsrcB=None, selT=None, NW=3):
    nc = cx.nc
    KC = Fin // 128
    MC = Fout // 128
    if TBLK is None:
        TBLK = min(T, 1024 if KC <= 16 else 512)
    NB = TBLK // 512
    G = max(1, 64 // KC)
    assert MC % G == 0
    if xsrc is None:
        xTv = xT.rearrange("(c p) t -> p c t", p=128)
        xsrc = lambda c0, c1, t0, n: [(c0, c1, xTv[:, c0:c1, t0:t0 + n])]
    with cx.scope() as sc:
        xn = sc.sb("xn", [128, KC, TBLK], BF16)
        xnB = Buf("xn")
        if xsrcB is not None:
            xtm = sc.sb("xtm", [128, KC, TBLK], BF16)
            xtmB = Buf("xtm")
            xtmD = cx.dmabuf(f"{tag}_xtm")
            sel = sc.sb("sel", [128, 2], F32)
            selB = cx.dmabuf(f"{tag}_sel")
            cx.dma("sp", lambda e: e.dma_start(out=sel[:], in_=selT), selB, writes=[selB])
        wb = [sc.sb(f"w{i}", [128, G, KC * 128], BF16) for i in range(NW)]
        wB = [cx.dmabuf(f"{tag}_w{i}") for i in range(NW)]
        NPS = 4
        pss = [sc.ps(f"ps{i}", [128, 512], F32) for i in range(NPS)]
        psB = [Buf(f"ps{i}") for i in range(NPS)]
        NO = 3
        ost = [sc.sb(f"o{i}", [128, 512], out_dt) for i in range(NO)]
        ostB = [cx.dmabuf(f"{tag}_o{i}") for i in range(NO)]
        if act == "relu2":
            rst = [sc.sb(f"r{i}", [128, 512], F32) for i in range(2)]
            rstB = [Buf(f"r{i}") for i in range(2)]
        if resT is not None:
            rsd = [sc.sb(f"rs{i}", [128, 512], F32) for i in range(3)]
            rsdB = [cx.dmabuf(f"{tag}_rs{i}") for i in range(3)]
        if gainT is not None:
            xs = sc.sb("xs", [128, KC, 512], F32)
            xsB = cx.dmabuf(f"{tag}_xs")
            sqb = sc.sb("sqb", [128, KC, 512], BF16)
            sqB = Buf("sqb")
            gsb = sc.sb("gsb", [128, KC], F32)
            gB = cx.dmabuf(f"{tag}_g")
            cx.dma("sp", lambda e: e.dma_start(out=gsb[:], in_=gainT), gB, writes=[gB])
            pssum = sc.ps("pssum", [128, 512], F32)
            pssB = Buf("pssum")
            tmp = sc.sb("tmp", [128, 512], F32)
            tmpB = Buf("tmp")
            rstd = sc.sb("rstd", [128, 512], F32)
            rstdB = Buf("rstd")
        else:
            xnD = cx.dmabuf(f"{tag}_xn")
        ones_bf, onesB = consts["ones_bf"]
        eps_sb, epsB = consts["eps"]
        cnt = 0
        wcnt = 0
        for tb in range(T // TBLK):
            t0 = tb * TBLK
            if gainT is None:
                for c0 in range(0, KC, 16):
                    _load_pieces(cx, xsrc(c0, c0 + 16, t0, TBLK), lambda cs, ce: xn[:, cs:ce, :], xnD, xnB)
                    if xsrcB is not None:
                        _load_pieces(cx, xsrcB(c0, c0 + 16, t0, TBLK), lambda cs, ce: xtm[:, cs:ce, :], xtmD, xtmB)
                        cx.op("dve", lambda e, c0=c0: e.tensor_scalar(out=xn[:, c0:c0 + 16, :], in0=xn[:, c0:c0 + 16, :],
                                                                      scalar1=sel[:, 0:1], scalar2=None, op0=ALU.mult),
                              reads=[xnB, selB], writes=[xnB])
                        cx.op("dve", lambda e, c0=c0: e.scalar_tensor_tensor(out=xn[:, c0:c0 + 16, :], in0=xtm[:, c0:c0 + 16, :],
                                                                             scalar=sel[:, 1:2], in1=xn[:, c0:c0 + 16, :],
                                                                             op0=ALU.mult, op1=ALU.add),
                              reads=[xtmB, xnB, selB], writes=[xnB])
            else:
                for n in range(NB):
                    ts = t0 + n * 512
                    _load_pieces(cx, xsrc(0, KC, ts, 512), lambda cs, ce: xs[:, cs:ce, :], xsB, xsB)
                    for c in range(KC):
                        cx.op("act", lambda e, c=c: e.activation(out=sqb[:, c, :], in_=xs[:, c, :], func=AF.Square),
                              reads=[xsB], writes=[sqB] if c == 0 else [])
                        if c > 0:
                            sqB.w = {id(cx.esem["act"]): (cx.esem["act"], cx.ecnt["act"])}
                    for c in range(KC):
                        cx.op("pe", lambda e, c=c: e.matmul(pssum[:], lhsT=ones_bf[:], rhs=sqb[:, c, :],
                                                            start=(c == 0), stop=(c == KC - 1)),
                              reads=[sqB, onesB], writes=[pssB])
                    cx.op("act", lambda e: e.activation(out=tmp[:], in_=pssum[:], func=AF.Ln, scale=1.0 / Fin, bias=eps_sb[:]),
                          reads=[pssB, epsB], writes=[tmpB])
                    cx.op("act", lambda e: e.activation(out=rstd[:], in_=tmp[:], func=AF.Exp, scale=-0.5),
                          reads=[tmpB], writes=[rstdB])
                    for c in range(KC):
                        cx.op("dve", lambda e, c=c, n=n: e.scalar_tensor_tensor(
                            out=xn[:, c, n * 512:(n + 1) * 512], in0=xs[:, c, :], scalar=gsb[:, c:c + 1], in1=rstd[:],
                            op0=ALU.mult, op1=ALU.mult),
                            reads=[xsB, gB, rstdB], writes=[xnB] if (c == 0 and n == 0) else [])
                        xnB.w = {id(cx.esem["dve"]): (cx.esem["dve"], cx.ecnt["dve"])}
            NMG = MC // G

            def issue_w(mg):
                wi = (wbase + mg) % NW
                cx.dma("pool", lambda e, wi=wi, mg=mg: e.dma_start(
                    out=wb[wi][:], in_=Wt[mg * G:(mg + 1) * G].rearrange("g p f -> p g f"), max_dma_last_dim=8192),
                    wB[wi], writes=[wB[wi]])

            wbase = wcnt
            wcnt += NMG
            iters = [(m, n) for m in range(MC) for n in range(NB)]

            def issue_res(i):
                m, n = iters[i]
                ri = i % 3
                ts = t0 + n * 512
                cx.dma("sp", lambda e, ri=ri, m=m, ts=ts: e.dma_start(
                    out=rsd[ri][:], in_=resT[m * 128:(m + 1) * 128, ts:ts + 512]), rsdB[ri], writes=[rsdB[ri]])

            for mg in range(min(NW - 1, NMG)):
                issue_w(mg)
            if resT is not None:
                for i in range(min(2, len(iters))):
                    issue_res(i)
            it = 0
            for mg in range(NMG):
                if mg + NW - 1 < NMG:
                    issue_w(mg + NW - 1)
                wi = (wbase + mg) % NW
                for g in range(G):
                    m = mg * G + g
                    for n in range(NB):
                        pi = cnt % NPS
                        oi = cnt % NO
                        cnt += 1
                        for c in range(KC):
                            cx.op("pe", lambda e, pi=pi, wi=wi, g=g, c=c, n=n: e.matmul(
                                pss[pi][:], lhsT=wb[wi][:, g, c * 128:(c + 1) * 128], rhs=xn[:, c, n * 512:(n + 1) * 512],
                                start=(c == 0), stop=(c == KC - 1)),
                                reads=[wB[wi], xnB], writes=[psB[pi]])
                        ts = t0 + n * 512
                        dst = outT[m * 128:(m + 1) * 128, ts:ts + 512]
                        if act == "relu2":
                            ri = cnt % 2
                            cx.op("act", lambda e, pi=pi, ri=ri: e.activation(out=rst[ri][:], in_=pss[pi][:], func=AF.Relu),
                                  reads=[psB[pi]], writes=[rstB[ri]])
                            cx.op("dve", lambda e, ri=ri, oi=oi: e.tensor_tensor(out=ost[oi][:], in0=rst[ri][:], in1=rst[ri][:],
                                                                                 op=ALU.mult),
                                  reads=[rstB[ri]], writes=[ostB[oi]])
                        elif resT is not None:
                            ri = it % 3
                            if it + 2 < len(iters):
                                issue_res(it + 2)
                            cx.op("dve", lambda e, pi=pi, ri=ri, oi=oi: e.tensor_tensor(
                                out=ost[oi][:], in0=pss[pi][:], in1=rsd[ri][:], op=ALU.add),
                                reads=[psB[pi], rsdB[ri]], writes=[ostB[oi]])
                        else:
                            if cnt % 2 == 0:
                                cx.op("act", lambda e, pi=pi, oi=oi: e.activation(out=ost[oi][:], in_=pss[pi][:], func=AF.Copy),
                                      reads=[psB[pi]], writes=[ostB[oi]])
                            else:
                                cx.op("dve", lambda e, pi=pi, oi=oi: e.tensor_copy(out=ost[oi][:], in_=pss[pi][:]),
                                      reads=[psB[pi]], writes=[ostB[oi]])
                        cx.dma("sp", lambda e, oi=oi, dst=dst: e.dma_start(out=dst, in_=ost[oi][:]), ostB[oi], reads=[ostB[oi]])
                        it += 1


import ml_dtypes

NPBF = ml_dtypes.bfloat16
BIGNEG = -30000.0
ATT_SCALE = 128 ** -0.5


def tile_w(W):
    Fin, Fout = W.shape
    KC, MC = Fin // 128, Fout // 128
    return np.ascontiguousarray(W.reshape(KC, 128, MC, 128).transpose(2, 1, 0, 3)).reshape(MC, 128, KC * 128)


def fm(v):
    return np.ascontiguousarray(v.reshape(-1, 128).T)


def _const_tables():
    c = {}
    c["c_ones_bf"] = np.ones((128, 128), NPBF)
    c["c_ident_bf"] = np.eye(128, dtype=np.float32).astype(NPBF)
    c["c_eps"] = np.full((128, 1), EPS, np.float32)
    seg = np.ones((128, 512), np.float32)
    seg[:, ::32] = 0.0
    c["c_segmask"] = seg
    s = np.arange(128)[:, None]
    t = np.arange(128)[None, :]
    c["c_gmaskT"] = ((s <= t) & (s // 32 == t // 32)).astype(np.float32)
    pm = np.zeros((128, 16, 8), np.float32)
    for tile in range(16):
        qb = tile // 2
        pm[:, tile, qb:] = -1e30
    c["c_pastmask"] = pm
    E = np.zeros((8, 8, 128), np.float32)
    for n in range(8):
        E[n, n, :] = 1.0
    c["c_E"] = E.astype(NPBF)
    s = np.arange(128)[:, None]
    t = np.arange(256)[None, :]
    c["c_CM0"] = np.where(s > t, BIGNEG, 0.0).astype(np.float32).astype(NPBF)
    return c


CONST_DT = {"c_ones_bf": BF16, "c_ident_bf": BF16, "c_eps": F32, "c_segmask": F32, "c_gmaskT": F32,
            "c_pastmask": F32, "c_E": BF16, "c_CM0": BF16}


def const_inputs():
    return _const_tables()


def setup_consts(cx, names=None):
    nc = cx.nc
    tabs = _const_tables()
    out = {}
    ld = cx.dmabuf("constld")
    for k, arr in tabs.items():
        d = nc.dram_tensor(k, list(arr.shape), CONST_DT[k], kind="ExternalInput").ap()
        if names is not None and k not in names:
            continue
        t = cx.es.enter_context(nc.sbuf_tensor("sb_" + k, list(arr.shape), CONST_DT[k]))
        b = Buf(k)
        cx.dma("sp", lambda e, t=t, d=d: e.dma_start(out=t[:], in_=d), ld, writes=[b])
        out[k[2:]] = (t, b)
    return out


def emit_gla(cx, projT, lbT, hgT, ogT, NH, S, layer, consts, tag="gla"):
    nc = cx.nc
    NBLK = S // 512
    pv = projT.rearrange("(a j p) t -> p a j t", a=4, j=NH, p=128)
    ones_bf, onesB = consts["ones_bf"]
    ident, identB = consts["ident_bf"]
    eps_sb, epsB = consts["eps"]
    segm, segB = consts["segmask"]
    gmask, gmB = consts["gmaskT"]
    with cx.scope() as sc:
        def T2(name, dt=F32, shape=(128, 512)):
            return [sc.sb(f"{name}{i}", list(shape), dt) for i in range(2)], [Buf(f"{name}{i}") for i in range(2)]
        inb = [sc.sb(f"in{i}", [128, 4, 512], F32) for i in range(2)]
        inB = [cx.dmabuf(f"{tag}_in{i}") for i in range(2)]
        qs, qsB = T2("qs")
        gate, gateB = T2("gate")
        ee, eeB = T2("ee")
        rr, rrB = T2("rr")
        ff, ffB = T2("ff")
        kk, kkB = T2("kk")
        lf, lfB = T2("lf")
        bc, bcB = T2("bc")
        eb, ebB = T2("eb")
        einv, einvB = T2("einv")
        qtb, qtbB = T2("qtb", BF16)
        ktb, ktbB = T2("ktb", BF16)
        khb, khbB = T2("khb", BF16)
        ib, ibB = T2("ib", BF16)
        vsb, vsbB = T2("vsb", BF16, (128, 4, 128))
        v32, v32B = T2("v32", BF16, (32, 16, 128))
        kh32, kh32B = T2("kh32", BF16, (32, 16, 128))
        amb, ambB = T2("amb", BF16, (128, 128))
        sbf = [[sc.sb(f"sbf{c_}{i}", [128, 128], BF16) for i in range(2)] for c_ in range(2)]
        sbfB = [[Buf(f"sbf{c_}{i}") for i in range(2)] for c_ in range(2)]
        osb, osbB = T2("osb")
        osq, osqB = T2("osq", BF16)
        tmp, tmpB = T2("tmp")
        rstd, rstdB = T2("rstd")
        ogb = [sc.sb(f"ogb{i}", [128, 512], BF16) for i in range(2)]
        ogB = [cx.dmabuf(f"{tag}_og{i}") for i in range(2)]
        s32 = [sc.sb(f"s32_{i}", [128, 128], F32) for i in range(2)]
        s32B = [Buf(f"s32_{i}") for i in range(2)]
        lbr = sc.sb("lbr", [128, 2, NH], F32)
        lbB = cx.dmabuf(f"{tag}_lb")
        hg = sc.sb("hg", [128, NH], F32)
        hgB = cx.dmabuf(f"{tag}_hg")
        lb = sc.sb("lb", [128, NH], F32)
        oml = sc.sb("oml", [128, NH], F32)
        lbcB = Buf("lbc")
        psTr = [sc.ps(f"psTr{i}", [128, 1024], BF16) for i in range(2)]
        psTrB = [Buf(f"psTr{i}") for i in range(2)]
        psA = [sc.ps(f"psA{i}", [128, 512], F32) for i in range(1)]
        psAB = [Buf(f"psA{i}") for i in range(1)]
        psO = [sc.ps(f"psO{i}", [128, 512], F32) for i in range(2)]
        psOB = [Buf(f"psO{i}") for i in range(2)]
        psS = [sc.ps(f"psS{i}", [128, 512], F32) for i in range(2)]
        psSB = [Buf(f"psS{i}") for i in range(2)]
        psN = sc.ps("psN", [128, 512], F32)
        psNB = Buf("psN")

        cx.dma("sp", lambda e: e.dma_start(out=lbr[:], in_=lbT), lbB, writes=[lbB])
        cx.dma("sp", lambda e: e.dma_start(out=hg[:], in_=hgT), hgB, writes=[hgB])
        if layer == 0:
            cx.op("dve", lambda e: e.memset(lb[:], 0.0), writes=[lbcB])
            cx.op("dve", lambda e: e.memset(oml[:], 1.0), writes=[])
        else:
            cx.op("dve", lambda e: e.tensor_tensor(out=lb[:], in0=lbr[:, 0, :], in1=lbr[:, 1, :], op=ALU.subtract),
                  reads=[lbB], writes=[lbcB])
            cx.op("act", lambda e: e.activation(out=lb[:], in_=lb[:], func=AF.Exp), reads=[lbcB], writes=[lbcB])
            cx.op("dve", lambda e: e.tensor_scalar(out=lb[:], in0=lb[:], scalar1=1.0, scalar2=None, op0=ALU.add),
                  reads=[lbcB], writes=[lbcB])
            cx.op("dve", lambda e: e.reciprocal(out=lb[:], in_=lb[:]), reads=[lbcB], writes=[lbcB])
            cx.op("dve", lambda e: e.tensor_scalar(out=oml[:], in0=lb[:], scalar1=-1.0, scalar2=1.0, op0=ALU.mult, op1=ALU.add),
                  reads=[lbcB], writes=[])
        lbcB.w = {id(cx.esem["dve"]): (cx.esem["dve"], cx.ecnt["dve"])}

        NCH = 2 if NH % 2 == 0 else 1
        trc_ = [0]
        sci = [0, 0]

        def issue_in(j, tb, b):
            cx.dma("sp", lambda e, b=b, j=j, tb=tb: e.dma_start(out=inb[b][:], in_=pv[:, :, j, tb * 512:(tb + 1) * 512]),
                   inB[b], writes=[inB[b]])

        def block_gen(j, tb, b, nxt):
            po = b
            X = inb[b]
            if tb == 0:
                cx.op("dve", lambda e: e.memset(s32[b][:], 0.0), writes=[s32B[b]])
                cx.op("dve", lambda e, k=sci[b] % 2: e.memset(sbf[b][k][:], 0.0), writes=[sbfB[b][sci[b] % 2]])
            t0 = tb * 512
            cx.op("act", lambda e: e.activation(out=qs[b][:], in_=X[:, 0, :], func=AF.Silu), reads=[inB[b]], writes=[qsB[b]])
            cx.op("act", lambda e: e.activation(out=gate[b][:], in_=X[:, 3, :], func=AF.Silu), reads=[inB[b]], writes=[gateB[b]])
            cx.op("act", lambda e: e.activation(out=ee[b][:], in_=X[:, 1, :], func=AF.Exp, scale=-1.0), reads=[inB[b]], writes=[eeB[b]])
            cx.op("act", lambda e: e.activation(out=ib[b][:], in_=X[:, 2, :], func=AF.Copy), reads=[inB[b]], writes=[ibB[b]])
            cx.op("pool", lambda e: e.tensor_scalar(out=ee[b][:], in0=ee[b][:], scalar1=1.0, scalar2=1.0, op0=ALU.add, op1=ALU.mult),
                  reads=[eeB[b]], writes=[eeB[b]])
            cx.op("dve", lambda e: e.reciprocal(out=rr[b][:], in_=ee[b][:]), reads=[eeB[b]], writes=[rrB[b]])
            cx.op("pool", lambda e: e.tensor_scalar(out=ff[b][:], in0=rr[b][:], scalar1=oml[:, j:j + 1], scalar2=lb[:, j:j + 1],
                                                    op0=ALU.mult, op1=ALU.add), reads=[rrB[b], lbcB], writes=[ffB[b]])
            cx.op("pool", lambda e: e.tensor_scalar(out=kk[b][:], in0=ff[b][:], scalar1=-1.0, scalar2=1.0, op0=ALU.mult, op1=ALU.add),
                  reads=[ffB[b]], writes=[kkB[b]])
            yield
            cx.op("act", lambda e: e.activation(out=lf[b][:], in_=ff[b][:], func=AF.Ln), reads=[ffB[b]], writes=[lfB[b]])
            cx.op("dve", lambda e: e.tensor_tensor_scan(out=bc[b][:], data0=segm[:], data1=lf[b][:], initial=0.0,
                                                        op0=ALU.mult, op1=ALU.add), reads=[lfB[b], segB], writes=[bcB[b]])
            yield
            cx.op("act", lambda e: e.activation(out=eb[b][:], in_=bc[b][:], func=AF.Exp), reads=[bcB[b]], writes=[ebB[b]])
            cx.op("pool", lambda e: e.tensor_tensor(out=qtb[b][:], in0=qs[b][:], in1=eb[b][:], op=ALU.mult),
                  reads=[qsB[b], ebB[b]], writes=[qtbB[b]])
            yield
            cx.op("dve", lambda e: e.reciprocal(out=einv[b][:], in_=eb[b][:]), reads=[ebB[b]], writes=[einvB[b]])
            cx.op("pool", lambda e: e.tensor_tensor(out=ktb[b][:], in0=kk[b][:], in1=einv[b][:], op=ALU.mult),
                  reads=[kkB[b], einvB[b]], writes=[ktbB[b]])
            yield
            for c in range(16):
                cx.op("act", lambda e, c=c: e.activation(out=khb[b][:, 32 * c:32 * c + 32], in_=ktb[b][:, 32 * c:32 * c + 32],
                                                         func=AF.Copy, scale=eb[b][:, 32 * c + 31:32 * c + 32]),
                      reads=[ktbB[b], ebB[b]], writes=[khbB[b]] if c == 0 else [])
                if c == 7:
                    khbB[b].w = {id(cx.esem["act"]): (cx.esem["act"], cx.ecnt["act"])}
                    yield
            khbB[b].w = {id(cx.esem["act"]): (cx.esem["act"], cx.ecnt["act"])}
            yield
            tr = trc_[0] % 2
            trc_[0] += 1
            for i in range(4):
                cx.op("pe", lambda e, tr=tr, i=i: e.transpose(out=psTr[tr][:, i * 128:(i + 1) * 128],
                                                              in_=ib[b][:, i * 128:(i + 1) * 128], identity=ident[:]),
                      reads=[ibB[b], identB], writes=[psTrB[tr]])
            cx.op("act", lambda e, tr=tr: e.activation(out=vsb[b][:].rearrange("p a b -> p (a b)"), in_=psTr[tr][:, 0:512], func=AF.Copy),
                  reads=[psTrB[tr]], writes=[vsbB[b]])
            yield
            for (src_, srcB, dst, dstB) in ((ib[b], ibB[b], v32[b], v32B[b]), (khb[b], khbB[b], kh32[b], kh32B[b])):
                for h in range(2):
                    tr = trc_[0] % 2
                    trc_[0] += 1
                    for i in range(8):
                        cc = h * 8 + i
                        cx.op("pe", lambda e, tr=tr, i=i, cc=cc, src_=src_: e.transpose(
                            out=psTr[tr][0:32, i * 128:(i + 1) * 128], in_=src_[:, cc * 32:(cc + 1) * 32], identity=ident[:]),
                            reads=[srcB, identB], writes=[psTrB[tr]])
                    cx.op("act", lambda e, tr=tr, dst=dst, h=h: e.activation(
                        out=dst[:, h * 8:(h + 1) * 8, :].rearrange("p a b -> p (a b)"), in_=psTr[tr][0:32, :], func=AF.Copy),
                        reads=[psTrB[tr]], writes=[dstB] if h == 0 else [])
                    if h == 1:
                        dstB.w[id(cx.esem["act"])] = (cx.esem["act"], cx.ecnt["act"])
                    yield
            if nxt is not None:
                issue_in(nxt[0], nxt[1], b)
            yield
            for i in range(4):
                a = 0
                cs = slice(i * 128, (i + 1) * 128)
                cx.op("pe", lambda e, a=a, cs=cs: e.matmul(psA[a][:, 0:128], lhsT=ktb[b][:, cs], rhs=qtb[b][:, cs], start=True, stop=True),
                      reads=[ktbB[b], qtbB[b]], writes=[psAB[a]])
                cx.op("dve", lambda e, a=a: e.tensor_tensor(out=amb[a][:], in0=psA[a][:, 0:128], in1=gmask[:], op=ALU.mult),
                      reads=[psAB[a], gmB], writes=[ambB[a]])
                cx.op("pe", lambda e, a=a, i=i, cs=cs: e.matmul(psO[po][:, cs], lhsT=vsb[b][:, i, :], rhs=amb[a][:], start=True, stop=False),
                      reads=[vsbB[b], ambB[a]], writes=[psOB[po]])
                for c in range(4):
                    k = sci[b] % 2
                    c0 = i * 128 + 32 * c
                    cx.op("pe", lambda e, k=k, c0=c0: e.matmul(psO[po][:, c0:c0 + 32], lhsT=sbf[b][k][:], rhs=qtb[b][:, c0:c0 + 32],
                                                               start=False, stop=(c0 % 128 == 96)),
                          reads=[sbfB[b][k], qtbB[b]], writes=[psOB[po]])
                    cx.op("pe", lambda e, i=i, c=c: e.matmul(psS[b][:, 0:128], lhsT=kh32[b][:, 4 * i + c, :],
                                                             rhs=v32[b][:, 4 * i + c, :], start=True, stop=True),
                          reads=[kh32B[b], v32B[b]], writes=[psSB[b]])
                    sci[b] += 1
                    k2 = sci[b] % 2
                    cx.op("dve", lambda e, c0=c0, k2=k2: e.scalar_tensor_tensor(out=sbf[b][k2][:], in0=s32[b][:], scalar=eb[b][:, c0 + 31:c0 + 32],
                                                                               in1=psS[b][:, 0:128], op0=ALU.mult, op1=ALU.add),
                          reads=[s32B[b], ebB[b], psSB[b]], writes=[sbfB[b][k2]])
                    cx.op("dve", lambda e, c0=c0: e.scalar_tensor_tensor(out=s32[b][:], in0=s32[b][:], scalar=eb[b][:, c0 + 31:c0 + 32],
                                                                         in1=psS[b][:, 0:128], op0=ALU.mult, op1=ALU.add),
                          reads=[s32B[b], ebB[b], psSB[b]], writes=[s32B[b]])
                    yield
            yield
            cx.op("act", lambda e: e.activation(out=osb[b][:], in_=psO[po][:], func=AF.Copy), reads=[psOB[po]], writes=[osbB[b]])
            cx.op("act", lambda e: e.activation(out=osq[b][:], in_=psO[po][:], func=AF.Square), reads=[psOB[po]], writes=[osqB[b]])
            cx.op("pe", lambda e: e.matmul(psN[:], lhsT=ones_bf[:], rhs=osq[b][:], start=True, stop=True),
                  reads=[onesB, osqB[b]], writes=[psNB])
            cx.op("act", lambda e: e.activation(out=tmp[b][:], in_=psN[:], func=AF.Ln, scale=1.0 / 128, bias=eps_sb[:]),
                  reads=[psNB, epsB], writes=[tmpB[b]])
            cx.op("act", lambda e: e.activation(out=rstd[b][:], in_=tmp[b][:], func=AF.Exp, scale=-0.5), reads=[tmpB[b]], writes=[rstdB[b]])
            cx.op("pool", lambda e: e.tensor_tensor(out=osb[b][:], in0=osb[b][:], in1=rstd[b][:], op=ALU.mult),
                  reads=[osbB[b], rstdB[b]], writes=[osbB[b]])
            cx.op("dve", lambda e: e.scalar_tensor_tensor(out=ogb[b][:], in0=osb[b][:], scalar=hg[:, j:j + 1], in1=gate[b][:],
                                                          op0=ALU.mult, op1=ALU.mult),
                  reads=[osbB[b], hgB, gateB[b]], writes=[ogB[b]])
            cx.dma("sp", lambda e, j=j, t0=t0: e.dma_start(out=ogT[j * 128:(j + 1) * 128, t0:t0 + 512], in_=ogb[b][:]),
                   ogB[b], reads=[ogB[b]])


        def chain_gen(ch):
            hs = [j for j in range(NH) if j % NCH == ch]
            blks = [(j, tb) for j in hs for tb in range(NBLK)]
            issue_in(blks[0][0], blks[0][1], ch)
            for bi, (j, tb) in enumerate(blks):
                nxt = blks[bi + 1] if bi + 1 < len(blks) else None
                yield from block_gen(j, tb, ch, nxt)

        gens = [chain_gen(ch) for ch in range(NCH)]
        if NCH == 2:
            for _ in range(14):
                next(gens[0])
        while gens:
            for g in list(gens):
                try:
                    next(g)
                except StopIteration:
                    gens.remove(g)


def _bf3(x):
    x = np.float32(x)
    a = np.float32(np.asarray(x, np.float32).astype(NPBF))
    r = np.float32(x - a)
    b = np.float32(np.asarray(r, np.float32).astype(NPBF))
    c = np.float32(np.asarray(np.float32(r - b), np.float32).astype(NPBF))
    return a, b, c


def moba_tables(heads):
    NH = len(heads)
    H = 16
    slopes = np.array([2.0 ** (-8.0 * (h + 1) / H) for h in heads], np.float64)
    ALA = np.zeros((6, NH, 128), np.float32)
    ALB = np.zeros((6, NH, 256), np.float32)
    bt = np.zeros((128, NH, 16), np.float32)
    for j in range(NH):
        cs = _bf3(slopes[j] / ATT_SCALE)
        for i in range(3):
            ALA[i, j, :] = np.arange(128)
            ALB[i, j, :] = cs[i]
            ALA[3 + i, j, :] = -cs[i]
            ALB[3 + i, j, :] = np.arange(256)
        for d in range(16):
            bt[:, j, d] = -slopes[j] * 128.0 * (d - 1)
    return ALA.astype(NPBF), ALB.astype(NPBF), bt


def emit_moba(cx, qT, kvT, qgT, kgT, alaT, albT, btT, ogT, NH, S, consts, tag="moba"):
    nc = cx.nc
    NT = S // 128
    NQB = S // 256
    assert NQB == 8
    ones_bf, onesB = consts["ones_bf"]
    ident, identB = consts["ident_bf"]
    eps_sb, epsB = consts["eps"]
    pastm, pastB = consts["pastmask"]
    E_sb, EB = consts["E"]
    CM0, CMB = consts["CM0"]
    with cx.scope() as sc:
        raw = [sc.sb(f"raw{i}", [128, S], F32) for i in range(2)]
        rawB = [cx.dmabuf(f"{tag}_raw{i}") for i in range(2)]
        sq = sc.sb("sq", [128, S], BF16); sqB = Buf("sq")
        tmp = sc.sb("tmp", [128, 512], F32); tmpB = Buf("tmp")
        rstd = sc.sb("rstd", [128, S], F32); rstdB = Buf("rstd")
        kn32 = sc.sb("kn32", [128, S], F32); kn32B = Buf("kn32")
        knb = sc.sb("knb", [128, S], BF16); knbB = Buf("knb")
        qnb = sc.sb("qnb", [128, S], BF16); qnbB = Buf("qnb")
        vb = sc.sb("vb", [128, S], BF16); vbB = Buf("vb")
        vsb = sc.sb("vsb", [128, NT, 128], BF16); vsbB = Buf("vsb")
        kms = sc.sb("kms", [128, 8], F32); kmsB = Buf("kms")
        kmb = sc.sb("kmb", [128, 8], BF16); kmbB = Buf("kmb")
        gm = sc.sb("gm", [128, NT, 8], F32); gmB = Buf("gm")
        top8 = sc.sb("top8", [128, NT, 8], F32); top8B = Buf("top8")
        bb = sc.sb("bb", [128, NT, 8], BF16); bbB = Buf("bb")
        bbT = sc.sb("bbT", [8, S], BF16); bbTB = Buf("bbT")
        NSB = 3
        pT = [sc.sb(f"pT{i}", [128, 256], BF16) for i in range(NSB)]
        pTB = [Buf(f"pT{i}") for i in range(NSB)]
        rec = sc.sb("rec", [128, 256], F32); recB = Buf("rec")
        ogb = [sc.sb(f"ogb{i}", [128, 256], BF16) for i in range(2)]
        ogB = [cx.dmabuf(f"{tag}_og{i}") for i in range(2)]
        qg = sc.sb("qg", [128, 1], F32); kg = sc.sb("kg", [128, 1], F32)
        ala = sc.sb("ala", [6, NH, 128], BF16); alb = sc.sb("alb", [6, NH, 256], BF16)
        bt = sc.sb("bt", [128, NH, 16], F32)
        tabB = cx.dmabuf(f"{tag}_tab")
        for (t_, d_) in ((qg, qgT), (kg, kgT), (ala, alaT), (alb, albT), (bt, btT)):
            cx.dma("sp", lambda e, t_=t_, d_=d_: e.dma_start(out=t_[:], in_=d_), tabB, writes=[tabB])
        psN = sc.ps("psN", [128, 512], F32); psNB = Buf("psN")
        NTR = 1
        psTr = [sc.ps(f"psTr{i}", [128, 1024], BF16) for i in range(NTR)]
        psTrB = [Buf(f"psTr{i}") for i in range(NTR)]
        psG = sc.ps("psG", [128, 512], F32); psGB = Buf("psG")
        psS = [sc.ps(f"psS{i}", [128, 512], F32) for i in range(NSB)]
        psSB = [Buf(f"psS{i}") for i in range(NSB)]
        psO = sc.ps("psO", [128, 512], F32); psOB = Buf("psO")
        psR = sc.ps("psR", [128, 512], F32); psRB = Buf("psR")

        rc = 0
        trc = 0
        sc_i = 0
        oc = 0

        def load_raw(src_rows):
            nonlocal rc
            r = rc % 2
            rc += 1
            cx.dma("sp", lambda e, r=r: e.dma_start(out=raw[r][:], in_=src_rows), rawB[r], writes=[rawB[r]])
            return r

        def rms_rstd(r):
            for n in range(S // 512):
                cs = slice(n * 512, (n + 1) * 512)
                cx.op("act", lambda e, cs=cs: e.activation(out=sq[:, cs], in_=raw[r][:, cs], func=AF.Square),
                      reads=[rawB[r]], writes=[sqB] if n == 0 else [])
                sqB.w = {id(cx.esem["act"]): (cx.esem["act"], cx.ecnt["act"])}
                cx.op("pe", lambda e, cs=cs: e.matmul(psN[:], lhsT=ones_bf[:], rhs=sq[:, cs], start=True, stop=True),
                      reads=[sqB, onesB], writes=[psNB])
                cx.op("act", lambda e: e.activation(out=tmp[:], in_=psN[:], func=AF.Ln, scale=1.0 / 128, bias=eps_sb[:]),
                      reads=[psNB, epsB], writes=[tmpB])
                cx.op("act", lambda e, cs=cs: e.activation(out=rstd[:, cs], in_=tmp[:], func=AF.Exp, scale=-0.5),
                      reads=[tmpB], writes=[rstdB] if n == 0 else [])
                rstdB.w = {id(cx.esem["act"]): (cx.esem["act"], cx.ecnt["act"])}

        for j in range(NH):
            r = load_raw(kvT[j * 128:(j + 1) * 128, :])
            rms_rstd(r)
            cx.op("dve", lambda e: e.scalar_tensor_tensor(out=kn32[:], in0=raw[r][:], scalar=kg[:, 0:1], in1=rstd[:],
                                                          op0=ALU.mult, op1=ALU.mult),
                  reads=[rawB[r], tabB, rstdB], writes=[kn32B])
            cx.op("pool", lambda e: e.tensor_copy(out=knb[:], in_=kn32[:]), reads=[kn32B], writes=[knbB])
            cx.op("dve", lambda e: e.tensor_reduce(out=kms[:], in_=kn32[:].rearrange("p (n k) -> p n k", k=256),
                                                   axis=AX.X, op=ALU.add), reads=[kn32B], writes=[kmsB])
            cx.op("dve", lambda e: e.tensor_scalar(out=kmb[:], in0=kms[:], scalar1=1.0 / 256, scalar2=None, op0=ALU.mult),
                  reads=[kmsB], writes=[kmbB])
            r = load_raw(kvT[(NH + j) * 128:(NH + j + 1) * 128, :])
            cx.op("act", lambda e: e.activation(out=vb[:], in_=raw[r][:], func=AF.Copy), reads=[rawB[r]], writes=[vbB])
            for h in range(NT // 8):
                tr = trc % NTR
                trc += 1
                for i in range(8):
                    tt = h * 8 + i
                    cx.op("pe", lambda e, tr=tr, i=i, tt=tt: e.transpose(out=psTr[tr][:, i * 128:(i + 1) * 128],
                                                                        in_=vb[:, tt * 128:(tt + 1) * 128], identity=ident[:]),
                          reads=[vbB, identB], writes=[psTrB[tr]])
                cx.op("dve", lambda e, tr=tr, h=h: e.tensor_copy(out=vsb[:, h * 8:(h + 1) * 8, :].rearrange("p a b -> p (a b)"),
                                                                 in_=psTr[tr][:, :]),
                      reads=[psTrB[tr]], writes=[vsbB] if h == 0 else [])
                vsbB.w = {id(cx.esem["dve"]): (cx.esem["dve"], cx.ecnt["dve"])}
            r = load_raw(qT[j * 128:(j + 1) * 128, :])
            rms_rstd(r)
            cx.op("dve", lambda e: e.scalar_tensor_tensor(out=qnb[:], in0=raw[r][:], scalar=qg[:, 0:1], in1=rstd[:],
                                                          op0=ALU.mult, op1=ALU.mult),
                  reads=[rawB[r], tabB, rstdB], writes=[qnbB])
            for tt in range(NT):
                cx.op("pe", lambda e, tt=tt: e.matmul(psG[:, tt * 8:(tt + 1) * 8], lhsT=qnb[:, tt * 128:(tt + 1) * 128], rhs=kmb[:],
                                                      start=True, stop=True),
                      reads=[qnbB, kmbB], writes=[psGB])
            cx.op("dve", lambda e: e.tensor_tensor(out=gm[:].rearrange("p a b -> p (a b)"), in0=psG[:, 0:NT * 8],
                                                   in1=pastm[:].rearrange("p a b -> p (a b)"), op=ALU.add),
                  reads=[psGB, pastB], writes=[gmB])
            for tt in range(NT):
                cx.op("dve", lambda e, tt=tt: e.max(out=top8[:, tt, :], in_=gm[:, tt, :]), reads=[gmB],
                      writes=[top8B] if tt == 0 else [])
            top8B.w = {id(cx.esem["dve"]): (cx.esem["dve"], cx.ecnt["dve"])}
            for tt in range(NT):
                cx.op("dve", lambda e, tt=tt: e.tensor_scalar(out=bb[:, tt, :], in0=gm[:, tt, :], scalar1=top8[:, tt, 2:3],
                                                              scalar2=BIGNEG, op0=ALU.is_lt, op1=ALU.mult),
                      reads=[gmB, top8B], writes=[bbB] if tt == 0 else [])
            bbB.w = {id(cx.esem["dve"]): (cx.esem["dve"], cx.ecnt["dve"])}
            for h in range(NT // 8):
                tr = trc % NTR
                trc += 1
                for i in range(8):
                    tt = h * 8 + i
                    cx.op("pe", lambda e, tr=tr, i=i, tt=tt: e.transpose(out=psTr[tr][0:8, i * 128:(i + 1) * 128],
                                                                        in_=bb[:, tt, :], identity=ident[:]),
                          reads=[bbB, identB], writes=[psTrB[tr]])
                cx.op("dve", lambda e, tr=tr, h=h: e.tensor_copy(out=bbT[:, h * 1024:(h + 1) * 1024], in_=psTr[tr][0:8, :]),
                      reads=[psTrB[tr]], writes=[bbTB] if h == 0 else [])
                bbTB.w = {id(cx.esem["dve"]): (cx.esem["dve"], cx.ecnt["dve"])}
            items = []
            for qb in range(NQB):
                subs = [(2 * qb, 0, 256, True), (2 * qb + 1, 128, 128, True)] + [(ks, 0, 256, False) for ks in range(2 * qb)]
                for si, sub in enumerate(subs):
                    items.append((qb, si, len(subs)) + sub)

            def emit_scores(idx):
                qb, si, nsub, ks, lo, N, own = items[idx]
                s_ = idx % NSB
                q0 = qb * 256
                qc = slice(q0 + lo, q0 + lo + N)
                cx.op("pe", lambda e, s_=s_, ks=ks, qc=qc, N=N: e.matmul(psS[s_][:, 0:N], lhsT=knb[:, ks * 128:(ks + 1) * 128],
                                                                         rhs=qnb[:, qc], start=True, stop=False),
                      reads=[knbB, qnbB], writes=[psSB[s_]])
                cx.op("pe", lambda e, s_=s_, lo=lo, N=N: e.matmul(psS[s_][:, 0:N], lhsT=ala[0:6, j, :], rhs=alb[0:6, j, lo:lo + N],
                                                                  start=False, stop=False),
                      reads=[tabB], writes=[psSB[s_]])
                if own:
                    cx.op("pe", lambda e, s_=s_, N=N: e.matmul(psS[s_][:, 0:N], lhsT=ident[:], rhs=CM0[:, 0:N],
                                                               start=False, stop=True),
                          reads=[identB, CMB], writes=[psSB[s_]])
                else:
                    n = ks // 2
                    cx.op("pe", lambda e, s_=s_, n=n, qc=qc, N=N: e.matmul(psS[s_][:, 0:N], lhsT=E_sb[0:8, n, :], rhs=bbT[0:8, qc],
                                                                           start=False, stop=True),
                          reads=[EB, bbTB], writes=[psSB[s_]])
                didx = 2 * qb - ks + 1
                cx.op("act", lambda e, s_=s_, N=N, didx=didx: e.activation(out=pT[s_][:, 0:N], in_=psS[s_][:, 0:N], func=AF.Exp,
                                                                           scale=ATT_SCALE, bias=bt[:, j, didx:didx + 1]),
                      reads=[psSB[s_], tabB], writes=[pTB[s_]])

            def emit_pv(idx):
                nonlocal oc
                qb, si, nsub, ks, lo, N, own = items[idx]
                s_ = idx % NSB
                q0 = qb * 256
                last = (si == nsub - 1)
                cx.op("pe", lambda e, s_=s_, ks=ks, lo=lo, N=N, si=si, last=last: e.matmul(
                    psO[:, lo:lo + N], lhsT=vsb[:, ks, :], rhs=pT[s_][:, 0:N], start=(si == 0), stop=last),
                    reads=[vsbB, pTB[s_]], writes=[psOB])
                cx.op("pe", lambda e, s_=s_, lo=lo, N=N, si=si, last=last: e.matmul(
                    psR[:, lo:lo + N], lhsT=ones_bf[:], rhs=pT[s_][:, 0:N], start=(si == 0), stop=last),
                    reads=[onesB, pTB[s_]], writes=[psRB])
                if last:
                    o_ = oc % 2
                    oc += 1
                    cx.op("dve", lambda e: e.reciprocal(out=rec[:], in_=psR[:, 0:256]), reads=[psRB], writes=[recB])
                    cx.op("dve", lambda e, o_=o_: e.tensor_tensor(out=ogb[o_][:], in0=psO[:, 0:256], in1=rec[:], op=ALU.mult),
                          reads=[psOB, recB], writes=[ogB[o_]])
                    cx.dma("sp", lambda e, o_=o_, q0=q0: e.dma_start(out=ogT[j * 128:(j + 1) * 128, q0:q0 + 256], in_=ogb[o_][:]),
                           ogB[o_], reads=[ogB[o_]])

            LOOK = NSB - 1
            for idx in range(min(LOOK, len(items))):
                emit_scores(idx)
            for idx in range(len(items)):
                if idx + LOOK < len(items):
                    emit_scores(idx + LOOK)
                emit_pv(idx)

from concourse.bass_utils import run_bass_kernel_spmd

D_MODEL = 2048
SEQ = 2048
BATCH = 4
NHL = 8
D_FF = 8192
TOWN = 1024
NCORES = 8
_PROGS = {}


_DECL = {}


def _dram(nc, name, shape, dt, kind="ExternalInput"):
    if kind == "ExternalInput":
        _DECL[name] = (tuple(shape), dt)
    return nc.dram_tensor(name, list(shape), dt, kind=kind).ap()


def build_A(layer):
    nc = bass.Bass("TRN2", target_bir_lowering=False)
    cx = Ctx(nc)
    consts = setup_consts(cx)
    hT = _dram(nc, "hT", [D_MODEL, SEQ], F32)
    Wt = _dram(nc, "Wt", [4 * NHL, 128, D_MODEL], F32)
    gT = _dram(nc, "gT", [128, 16], F32)
    lbT = _dram(nc, "lbT", [128, 2, NHL], F32)
    hgT = _dram(nc, "hgT", [128, NHL], F32)
    projT = _dram(nc, "projT", [4 * NHL * 128, SEQ], F32, "Internal")
    ogT = _dram(nc, "ogT", [NHL * 128, SEQ], BF16, "ExternalOutput")
    emit_linear(cx, hT, Wt, projT, SEQ, D_MODEL, 4 * NHL * 128, F32, F32, consts, gainT=gT, tag="win")
    emit_gla(cx, projT, lbT, hgT, ogT, NHL, SEQ, layer, consts)
    cx.finish()
    return nc


def build_C(with_kv):
    nc = bass.Bass("TRN2", target_bir_lowering=False)
    cx = Ctx(nc)
    consts = setup_consts(cx)
    hT = _dram(nc, "hT", [D_MODEL, SEQ], F32)
    Wq = _dram(nc, "Wq", [NHL, 128, D_MODEL], F32)
    gT = _dram(nc, "gT", [128, 16], F32)
    qgT = _dram(nc, "qgT", [128, 1], F32)
    kgT = _dram(nc, "kgT", [128, 1], F32)
    alaT = _dram(nc, "alaT", [6, NHL, 128], BF16)
    albT = _dram(nc, "albT", [6, NHL, 256], BF16)
    btT = _dram(nc, "btT", [128, NHL, 16], F32)
    qT = _dram(nc, "qT", [NHL * 128, SEQ], F32, "Internal")
    ogT = _dram(nc, "ogT", [NHL * 128, SEQ], BF16, "ExternalOutput")
    if with_kv:
        Wkv = _dram(nc, "Wkv", [2 * NHL, 128, D_MODEL], F32)
        gkvT = _dram(nc, "gkvT", [128, 16], F32)
        kvT = _dram(nc, "kvT", [2 * NHL * 128, SEQ], F32, "ExternalOutput")
        emit_linear(cx, hT, Wkv, kvT, SEQ, D_MODEL, 2 * NHL * 128, F32, F32, consts, gainT=gkvT, tag="wkv")
    else:
        kvT = _dram(nc, "kvT", [2 * NHL * 128, SEQ], F32)
    emit_linear(cx, hT, Wq, qT, SEQ, D_MODEL, NHL * 128, F32, F32, consts, gainT=gT, tag="wq")
    emit_moba(cx, qT, kvT, qgT, kgT, alaT, albT, btT, ogT, NHL, SEQ, consts)
    cx.finish()
    return nc


def build_B():
    nc = bass.Bass("TRN2", target_bir_lowering=False)
    cx = Ctx(nc)
    consts = setup_consts(cx)
    ogT = _dram(nc, "ogT", [D_MODEL, TOWN], BF16)
    hT = _dram(nc, "hT", [D_MODEL, TOWN], F32)
    Wo = _dram(nc, "Wo", [16, 128, D_MODEL], F32)
    gT = _dram(nc, "gT", [128, 16], F32)
    W1 = _dram(nc, "W1", [64, 128, D_MODEL], F32)
    W2 = _dram(nc, "W2", [16, 128, D_FF], F32)
    h1T = _dram(nc, "h1T", [D_MODEL, TOWN], F32, "Internal")
    aT = _dram(nc, "aT", [D_FF, TOWN], BF16, "Internal")
    h2T = _dram(nc, "h2T", [D_MODEL, TOWN], F32, "ExternalOutput")
    emit_linear(cx, ogT, Wo, h1T, TOWN, D_MODEL, D_MODEL, BF16, F32, consts, resT=hT, tag="wo")
    emit_linear(cx, h1T, W1, aT, TOWN, D_MODEL, D_FF, F32, BF16, consts, gainT=gT, act="relu2", tag="w1")
    emit_linear(cx, aT, W2, h2T, TOWN, D_FF, D_MODEL, BF16, F32, consts, resT=h1T, tag="w2")
    cx.finish()
    return nc


PAIRS = [[0, 1], [2, 3], [4, 5], [6, 7]]


def build_fused(layers=(0, 1, 2, 3), info=None):
    nc = bass.Bass("TRN2", target_bir_lowering=False)
    cx = Ctx(nc)
    consts = setup_consts(cx)
    I = lambda n, s, dt=F32: _dram(nc, n, s, dt)
    N = lambda n, s, dt=F32: _dram(nc, n, s, dt, "Internal")
    rsel = I("rsel", [128, 2])
    hall0 = I("hall0", [2 * D_MODEL, TOWN])
    hT0 = I("hT0", [D_MODEL, TOWN])
    hall = N("hall", [2 * D_MODEL, TOWN])
    ogT = N("ogT", [NHL * 128, SEQ], BF16)
    ogall = N("ogall", [2 * NHL * 128, SEQ], BF16)
    projT = N("projT", [4 * NHL * 128, SEQ])
    qT = N("qT", [NHL * 128, SEQ])
    kvT = N("kvT", [2 * NHL * 128, SEQ])
    h1T = N("h1T", [D_MODEL, TOWN])
    aT = N("aT", [D_FF, TOWN], BF16)
    hbuf = [N("hA", [D_MODEL, TOWN]), N("hB", [D_MODEL, TOWN])]
    hout = _dram(nc, "hout", [D_MODEL, TOWN], F32, "ExternalOutput")
    agB = cx.dmabuf("ag")
    qgT = [None] * 2
    ogv = ogall.rearrange("(i r cc p) t -> r i p cc t", i=4, r=2, cc=2, p=128)

    def ogsrc(off):
        def f(c0, c1, t0, n):
            assert c0 == 0 and c1 == 16
            return [(8 * rr + 2 * i, 8 * rr + 2 * i + 2, ogv[rr, i, :, :, off + t0:off + t0 + n])
                    for rr in range(2) for i in range(4)]
        return f
    ogA = ogsrc(0)
    ogBf = ogsrc(TOWN)
    kgT = I("kgT", [128, 1])
    alaT = I("alaT", [6, NHL, 128], BF16)
    albT = I("albT", [6, NHL, 256], BF16)
    btT = I("btT", [128, NHL, 16])
    lbT = I("lbT", [128, 2, NHL])
    for l in layers:
        if l != layers[0]:
            cx.renew_engine_sems()
        h_cur = hT0 if l == layers[0] else hbuf[(l - 1) % 2]
        if l == layers[0]:
            src_all = hall0
        else:
            for i in range(8):
                cx.coll(lambda e, h_cur=h_cur, i=i: e.collective_compute(
                    "AllGather", ALU.bypass, replica_groups=PAIRS,
                    ins=[h_cur[256 * i:256 * (i + 1), :].opt()], outs=[hall[512 * i:512 * (i + 1), :].opt()]), agB)
            cx.barrier_on(agB)
            src_all = hall
        if l == layers[0]:
            sv0 = src_all.rearrange("(r c p) t -> r p c t", r=2, p=128)
            xs = lambda c0, c1, t0, n, sv0=sv0: [(c0, c1, sv0[t0 // TOWN, :, c0:c1, (t0 % TOWN):(t0 % TOWN) + n])]
        else:
            sv = src_all.rearrange("(i r cc p) t -> r i p cc t", i=8, r=2, cc=2, p=128)
            xs = lambda c0, c1, t0, n, sv=sv: [(2 * i, 2 * i + 2, sv[t0 // TOWN, i, :, :, (t0 % TOWN):(t0 % TOWN) + n])
                                               for i in range(c0 // 2, c1 // 2)]
        gT = I(f"gmix{l}", [128, 16])
        if l < 2:
            Wt = I(f"Win{l}", [4 * NHL, 128, D_MODEL])
            hgT = I(f"hg{l}", [128, NHL])
            emit_linear(cx, None, Wt, projT, SEQ, D_MODEL, 4 * NHL * 128, F32, F32, consts, gainT=gT, tag=f"win{l}", xsrc=xs)
            emit_gla(cx, projT, lbT, hgT, ogT, NHL, SEQ, l, consts, tag=f"gla{l}")
        else:
            Wq = I(f"Wq{l}", [NHL, 128, D_MODEL])
            qg = I(f"qg{l}", [128, 1])
            if l == 2:
                Wkv = I("Wkv", [2 * NHL, 128, D_MODEL])
                gkvT = I("gkv", [128, 16])
                emit_linear(cx, None, Wkv, kvT, SEQ, D_MODEL, 2 * NHL * 128, F32, F32, consts, gainT=gkvT, tag="wkv", xsrc=xs)
            emit_linear(cx, None, Wq, qT, SEQ, D_MODEL, NHL * 128, F32, F32, consts, gainT=gT, tag=f"wq{l}", xsrc=xs)
            emit_moba(cx, qT, kvT, qg, kgT, alaT, albT, btT, ogT, NHL, SEQ, consts, tag=f"moba{l}")
        for i in range(4):
            cx.coll(lambda e, i=i: e.collective_compute(
                "AllGather", ALU.bypass, replica_groups=PAIRS,
                ins=[ogT[256 * i:256 * (i + 1), :].opt()], outs=[ogall[512 * i:512 * (i + 1), :].opt()]), agB)
        cx.barrier_on(agB)
        Wo = I(f"Wo{l}", [16, 128, D_MODEL])
        g2 = I(f"gmlp{l}", [128, 16])
        W1 = I(f"W1_{l}", [64, 128, D_MODEL])
        W2 = I(f"W2_{l}", [16, 128, D_FF])
        h_next = hout if l == layers[-1] else hbuf[l % 2]
        emit_linear(cx, None, Wo, h1T, TOWN, D_MODEL, D_MODEL, BF16, F32, consts, resT=h_cur, tag=f"wo{l}",
                    xsrc=ogA, xsrcB=ogBf, selT=rsel)
        emit_linear(cx, h1T, W1, aT, TOWN, D_MODEL, D_FF, F32, BF16, consts, gainT=g2, act="relu2", tag=f"w1{l}")
        emit_linear(cx, aT, W2, h_next, TOWN, D_FF, D_MODEL, BF16, F32, consts, resT=h1T, tag=f"w2{l}", TBLK=1024, NW=2)
    if info is not None:
        info['nsem'] = cx.nsem
        info['ecnt'] = dict(cx.ecnt)
        info['maxcnt'] = getattr(cx, 'maxcnt', 0)
        info['maxdma'] = max([v for (_, v) in cx.final] + [b.dval for b in cx.alldma])
    cx.finish()
    return nc


def _head_cols(r, nparts):
    cols = []
    for a in range(nparts):
        for j in range(NHL):
            h = NHL * r + j
            cols.append(np.arange(a * D_MODEL + h * 128, a * D_MODEL + (h + 1) * 128))
    return np.concatenate(cols)


def kernel(x, a_norm, a_w_in, a_head_norm, a_w_out, lower_bounds, kv_norm, w_kv, k_norm,
           b_norm, b_w_q, b_q_norm, b_w_o, mlp_norm, mlp_w1, mlp_w2):
    f32 = lambda a: np.asarray(a, dtype=np.float32)
    x = f32(x)
    shared = dict(const_inputs())
    shared["kgT"] = f32(k_norm)[:, None].copy()
    for l in range(4):
        shared[f"gmlp{l}"] = fm(f32(mlp_norm[l]))
        shared[f"W1_{l}"] = tile_w(f32(mlp_w1[l]))
        shared[f"W2_{l}"] = tile_w(f32(mlp_w2[l]))
        shared[f"Wo{l}"] = tile_w(f32(a_w_out[l] if l < 2 else b_w_o[l - 2]))
        shared[f"gmix{l}"] = fm(f32(a_norm[l] if l < 2 else b_norm[l - 2]))
        if l >= 2:
            shared[f"qg{l}"] = f32(b_q_norm[l - 2])[:, None].copy()
    shared["gkv"] = fm(f32(kv_norm))
    per_rank = []
    for r in range(2):
        d = {}
        hs = slice(NHL * r * 128, NHL * (r + 1) * 128)
        for l in range(2):
            d[f"Win{l}"] = tile_w(f32(a_w_in[l])[:, _head_cols(r, 4)])
            d[f"hg{l}"] = np.ascontiguousarray(f32(a_head_norm[l])[hs].reshape(NHL, 128).T)
        d["lbT"] = np.ascontiguousarray(f32(lower_bounds)[:, hs].reshape(2, NHL, 128).transpose(2, 0, 1))
        for l in (2, 3):
            d[f"Wq{l}"] = tile_w(f32(b_w_q[l - 2])[:, _head_cols(r, 1)])
        d["Wkv"] = tile_w(f32(w_kv)[:, _head_cols(r, 2)])
        ala, alb, bt = moba_tables(list(range(NHL * r, NHL * (r + 1))))
        d["alaT"], d["albT"], d["btT"] = ala, alb, bt
        rs = np.zeros((128, 2), np.float32)
        rs[:, r] = 1.0
        d["rsel"] = rs
        per_rank.append(d)
    maps = []
    for c in range(NCORES):
        b, r = divmod(c, 2)
        xb = x[b]
        hall0 = np.ascontiguousarray(xb.reshape(2, TOWN, D_MODEL).transpose(0, 2, 1)).reshape(2 * D_MODEL, TOWN)
        m = dict(shared)
        m.update(per_rank[r])
        m["hall0"] = hall0
        m["hT0"] = np.ascontiguousarray(hall0[r * D_MODEL:(r + 1) * D_MODEL])
        maps.append(m)
    nc = build_fused()
    res = run_bass_kernel_spmd(nc, maps, core_ids=list(range(NCORES))).results
    out = np.empty((BATCH, SEQ, D_MODEL), np.float32)
    for c in range(NCORES):
        b, r = divmod(c, 2)
        out[b, r * TOWN:(r + 1) * TOWN, :] = np.asarray(res[c]["hout"]).T
    return out
```
